# Optimizing a Trainium2 kernel written in Bass

```python
import jax, jax.numpy as jnp
from jax import lax
import numpy as np

D_MODEL = 1024
BATCH = 4
SEQ = 8192
DEPTH = 4

N_EVEN = (DEPTH + 1) // 2
N_ODD = DEPTH // 2
N_NORMS = 6

D_FF = ((8 * D_MODEL // 3 + 127) // 128) * 128
FFN_RES = 0.5

SB_HEADS = 8
SB_HEAD_DIM = 64
SB_WIDTH = SB_HEADS * SB_HEAD_DIM
SB_BLOCK = 128

CONV_CH = D_MODEL // 2
CONV_WIDTH = 31

EVEN_IN = 3 * SB_WIDTH + 2 * CONV_CH
EVEN_OUT = SB_WIDTH + CONV_CH

GLA_HEADS = 4
GLA_KEY = D_MODEL // 2
GLA_VAL = D_MODEL
GLA_DK = GLA_KEY // GLA_HEADS
GLA_DV = GLA_VAL // GLA_HEADS
GATE_RANK = 16
GATE_TAU = 16.0
GLA_CHUNK = 64
ODD_IN = 2 * GLA_KEY + 2 * GLA_VAL + GATE_RANK

NORM_EPS = 1e-6

kernel_name = "hybrid_sbattn_conformerconv_gla_macaron"


def rms_norm(x, g):
    xf = x.astype(jnp.float32)
    y = xf * lax.rsqrt(jnp.mean(xf * xf, axis=-1, keepdims=True) + NORM_EPS)
    return (y * g.astype(jnp.float32)).astype(x.dtype)


def layer_norm(x, g, b):
    xf = x.astype(jnp.float32)
    mu = jnp.mean(xf, axis=-1, keepdims=True)
    var = jnp.mean(jnp.square(xf - mu), axis=-1, keepdims=True)
    y = (xf - mu) * lax.rsqrt(var + NORM_EPS)
    return (y * g.astype(jnp.float32) + b.astype(jnp.float32)).astype(x.dtype)


def swiglu(x, w_in, w_out):
    a, b = jnp.split(x @ w_in, 2, axis=-1)
    return (jax.nn.silu(a) * b) @ w_out


def split_heads(t, n):
    B, S, W = t.shape
    return t.reshape(B, S, n, W // n).transpose(0, 2, 1, 3)


def merge_heads(t):
    B, H, S, d = t.shape
    return t.transpose(0, 2, 1, 3).reshape(B, S, H * d)


def stick_breaking_attention(q, k, v):
    B, H, S, Dh = q.shape
    scale = Dh ** -0.5
    outs = []
    for i in range(S // SB_BLOCK):
        t0, t1 = i * SB_BLOCK, (i + 1) * SB_BLOCK
        qb = q[:, :, t0:t1]
        kb = k[:, :, :t1]
        vb = v[:, :, :t1]
        z = jnp.einsum('bhqd,bhkd->bhqk', qb, kb).astype(jnp.float32) * scale
        mask = jnp.arange(t1)[None, :] < jnp.arange(t0, t1)[:, None]
        log_1m = jnp.where(mask, jax.nn.log_sigmoid(-z), 0.0)
        rest = lax.cumsum(log_1m, axis=3, reverse=True) - log_1m
        w = jnp.where(mask, jnp.exp(jax.nn.log_sigmoid(z) + rest), 0.0)
        outs.append(jnp.einsum('bhqk,bhkd->bhqd', w.astype(vb.dtype), vb))
    return jnp.concatenate(outs, axis=2)


def causal_depthwise_conv(x, w, b):
    K, C = w.shape
    xp = jnp.pad(x, ((0, 0), (K - 1, 0), (0, 0)))
    y = lax.conv_general_dilated(
        xp, w[:, None, :].astype(x.dtype), window_strides=(1,), padding='VALID',
        dimension_numbers=('NWC', 'WIO', 'NWC'), feature_group_count=C)
    return y + b.astype(x.dtype)


def even_mixer(h, w_in, w_out, dw_w, dw_b, ln_g, ln_b):
    proj = h @ w_in
    q, k, v, u, gate = jnp.split(
        proj, [SB_WIDTH, 2 * SB_WIDTH, 3 * SB_WIDTH, 3 * SB_WIDTH + CONV_CH], axis=-1)
    a_out = merge_heads(stick_breaking_attention(
        split_heads(q, SB_HEADS), split_heads(k, SB_HEADS), split_heads(v, SB_HEADS)))
    c = u * jax.nn.sigmoid(gate)
    c = causal_depthwise_conv(c, dw_w, dw_b)
    c = jax.nn.silu(layer_norm(c, ln_g, ln_b))
    return jnp.concatenate([a_out.astype(h.dtype), c], axis=-1) @ w_out


def gla_chunked(q, k, v, g):
    B, H, S, dk = q.shape
    dv = v.shape[-1]
    N = S // GLA_CHUNK

    def to_chunks(t):
        return t.reshape(B, H, N, GLA_CHUNK, t.shape[-1]).transpose(2, 0, 1, 3, 4)

    mask = jnp.tril(jnp.ones((GLA_CHUNK, GLA_CHUNK), dtype=bool))

    def step(state, inp):
        qc, kc, vc, gc = inp
        b = jnp.cumsum(gc, axis=2)
        inter = jnp.einsum('bhtk,bhkv->bhtv', qc * jnp.exp(b), state)
        diff = b[:, :, :, None, :] - b[:, :, None, :, :]
        decay = jnp.exp(jnp.where(mask[:, :, None], diff, -jnp.inf))
        scores = jnp.einsum('bhtk,bhsk,bhtsk->bhts', qc, kc, decay)
        out = inter + jnp.einsum('bhts,bhsv->bhtv', scores, vc)
        b_last = b[:, :, -1, :]
        state = jnp.exp(b_last)[..., None] * state + jnp.einsum(
            'bhsk,bhsv->bhkv', kc * jnp.exp(b_last[:, :, None, :] - b), vc)
        return state, out

    init = jnp.zeros((B, H, dk, dv), jnp.float32)
    _, o = lax.scan(step, init, (to_chunks(q), to_chunks(k), to_chunks(v), to_chunks(g)))
    return o.transpose(1, 2, 0, 3, 4).reshape(B, H, S, dv)


def odd_mixer(h, w_in, w_gate, b_gate, norm_g, w_out):
    proj = h @ w_in
    q, k, v, r, g_lr = jnp.split(
        proj, [GLA_KEY, 2 * GLA_KEY, 2 * GLA_KEY + GLA_VAL, 2 * GLA_KEY + 2 * GLA_VAL], axis=-1)
    g = jax.nn.log_sigmoid((g_lr @ w_gate + b_gate).astype(jnp.float32)) / GATE_TAU
    f32 = jnp.float32
    qh = split_heads(q, GLA_HEADS).astype(f32) * (GLA_DK ** -0.5)
    kh = split_heads(k, GLA_HEADS).astype(f32)
    vh = split_heads(v, GLA_HEADS).astype(f32)
    gh = split_heads(g, GLA_HEADS)
    o = gla_chunked(qh, kh, vh, gh)
    o = merge_heads(rms_norm(o, norm_g)).astype(h.dtype)
    return (o * jax.nn.silu(r)) @ w_out


def setup_inputs(seed: int = 0) -> dict:
    key = jax.random.key(seed)
    ks = jax.random.split(key, 20)
    f32 = jnp.float32

    def w(k, shape, fan_in):
        return jax.random.normal(k, shape, f32) * (fan_in ** -0.5)

    return {
        "x": jax.random.normal(ks[0], (BATCH, SEQ, D_MODEL), f32),
        "norm_g": 1.0 + 0.05 * jax.random.normal(ks[1], (DEPTH, N_NORMS, D_MODEL), f32),
        "ffn1_w_in": w(ks[2], (DEPTH, D_MODEL, 2 * D_FF), D_MODEL),
        "ffn1_w_out": w(ks[3], (DEPTH, D_FF, D_MODEL), D_FF),
        "ffn2_w_in": w(ks[4], (DEPTH, D_MODEL, 2 * D_FF), D_MODEL),
        "ffn2_w_out": w(ks[5], (DEPTH, D_FF, D_MODEL), D_FF),
        "hyb_w_in": w(ks[6], (N_EVEN, D_MODEL, EVEN_IN), D_MODEL),
        "hyb_w_out": w(ks[7], (N_EVEN, EVEN_OUT, D_MODEL), EVEN_OUT),
        "conv_dw_w": w(ks[8], (N_EVEN, CONV_WIDTH, CONV_CH), CONV_WIDTH),
        "conv_dw_b": 0.02 * jax.random.normal(ks[9], (N_EVEN, CONV_CH), f32),
        "conv_ln_g": 1.0 + 0.05 * jax.random.normal(ks[10], (N_EVEN, CONV_CH), f32),
        "conv_ln_b": 0.02 * jax.random.normal(ks[11], (N_EVEN, CONV_CH), f32),
        "gla_w_in": w(ks[12], (N_ODD, D_MODEL, ODD_IN), D_MODEL),
        "gla_w_gate": w(ks[13], (N_ODD, GATE_RANK, GLA_KEY), GATE_RANK),
        "gla_b_gate": 0.02 * jax.random.normal(ks[14], (N_ODD, GLA_KEY), f32),
        "gla_norm_g": 1.0 + 0.05 * jax.random.normal(ks[15], (N_ODD, GLA_DV), f32),
        "gla_w_out": w(ks[16], (N_ODD, GLA_VAL, D_MODEL), GLA_VAL),
    }


def reference(x, norm_g, ffn1_w_in, ffn1_w_out, ffn2_w_in, ffn2_w_out,
              hyb_w_in, hyb_w_out, conv_dw_w, conv_dw_b, conv_ln_g, conv_ln_b,
              gla_w_in, gla_w_gate, gla_b_gate, gla_norm_g, gla_w_out):
    h = x
    for l in range(DEPTH):
        g = norm_g[l]
        f = swiglu(rms_norm(h, g[0]), ffn1_w_in[l], ffn1_w_out[l])
        h = h + FFN_RES * rms_norm(f, g[1])
        hn = rms_norm(h, g[2])
        if l % 2 == 0:
            e = l // 2
            m = even_mixer(hn, hyb_w_in[e], hyb_w_out[e], conv_dw_w[e], conv_dw_b[e],
                           conv_ln_g[e], conv_ln_b[e])
        else:
            o = l // 2
            m = odd_mixer(hn, gla_w_in[o], gla_w_gate[o], gla_b_gate[o], gla_norm_g[o],
                          gla_w_out[o])
        h = h + rms_norm(m, g[3])
        f = swiglu(rms_norm(h, g[4]), ffn2_w_in[l], ffn2_w_out[l])
        h = h + FFN_RES * rms_norm(f, g[5])
    return h
```

```python
import os
import math
import numpy as np
from contextlib import ExitStack
import concourse.bass as bass
import concourse.mybir as mybir
from concourse.bass_utils import run_bass_kernel_spmd

F32 = mybir.dt.float32
BF16 = mybir.dt.bfloat16
AF = mybir.ActivationFunctionType
ALU = mybir.AluOpType

D = 1024
DFF = 2816
NFC = 22
TOK = 4096
TT = 512
NTT = TOK // TT
SEQ = 8192
EPS = 1e-6
NCORES = 8
ATT_NH, ATT_NG, GLA_NSEG = 4, 16, 8

C_ID, C_ONES, C_NEGU, C_TRIN, C_GMASK = 0, 128, 256, 384, 512
C_AMASK = 640
C_WGLR = C_AMASK + 2048
C_WGATE = C_WGLR + 256
NBF = C_WGATE + 1024
C_NORMG = NBF
C_GLANG = C_NORMG + 192
C_CONVW = C_GLANG + 4
C_CONVP = C_CONVW + 248
C_FLAG = C_CONVP + 24
NCONST = C_FLAG + 1
NF32 = NCONST - NBF

RE = 1544
BE = 772
RG = 2560
BG = 1280


class T:
    __slots__ = ("base", "w", "r", "name", "multi")

    def __init__(self, base, name, multi=False):
        self.base = base
        self.w = None
        self.r = {}
        self.name = name
        self.multi = multi

    def __getitem__(self, idx):
        return self.base[idx]


class Eng:
    def __init__(self, name, h, sem):
        self.name = name
        self.h = h
        self.sem = sem
        self.count = 0
        self.known = {}
        self.pend_r = []
        self.pend_w = []


class FW:
    def __init__(self, nc):
        self.nc = nc
        self.es = ExitStack()
        self.eng = {}
        for name, h in (("pe", nc.tensor), ("act", nc.scalar), ("dve", nc.vector),
                        ("pool", nc.gpsimd), ("sp", nc.sync)):
            sem = self.es.enter_context(nc.semaphore("s_" + name))
            self.eng[name] = Eng(name, h, sem)
        self.dq = {}
        for q in ("sp", "pool"):
            sems = [self.es.enter_context(nc.semaphore(f"dq_{q}_{i}")) for i in range(8)]
            self.dq[q] = {"sems": sems, "n": 0}
        self.nwaits = 0
        self.nins = 0
        self._uid = 0
        self.pending = set()

    def sb(self, es, name, shape, dtype=F32):
        self._uid += 1
        t = es.enter_context(self.nc.sbuf_tensor(f"{name}_{self._uid}", list(shape), dtype))
        return t

    def ps(self, es, name, shape, dtype=F32):
        self._uid += 1
        t = es.enter_context(self.nc.psum_tensor(f"{name}_{self._uid}", list(shape), dtype))
        return t

    def tile(self, es, name, shape, dtype=F32):
        return T(self.sb(es, name, shape, dtype), name)

    def tiles(self, es, name, n, shape, dtype=F32):
        t = self.sb(es, name, [shape[0], n] + list(shape[1:]), dtype)
        return t, [T(t[:, i], f"{name}{i}") for i in range(n)]

    def ptile(self, es, name, shape, dtype=F32):
        return T(self.ps(es, name, shape, dtype), name)

    def sem(self, name):
        return self.es.enter_context(self.nc.semaphore(name))

    def _wait(self, E, ev):
        sem, val = ev
        k = id(sem)
        if E.known.get(k, 0) >= val:
            return
        E.h.wait_ge(sem, val)
        E.known[k] = val
        self.nwaits += 1

    def _deps(self, E, reads, writes, skip_self=False):
        for b in list(reads) + list(writes):
            assert id(b) not in self.pending or b in E.pend_r or b in E.pend_w, \
                f"access to {b.name} with pending (un-incremented) accesses"
        for b in reads:
            if b.multi:
                for ev in b.r.values():
                    self._wait(E, ev)
                continue
            if b.w is not None and not (skip_self and b.w[0] is E.sem):
                self._wait(E, b.w)
        for b in writes:
            if b.multi:
                continue
            if b.w is not None and not (skip_self and b.w[0] is E.sem):
                self._wait(E, b.w)
            for ev in b.r.values():
                if not (skip_self and ev[0] is E.sem):
                    self._wait(E, ev)

    def _mark(self, ev, reads, writes):
        k = id(ev[0])
        for b in reads:
            if b.multi:
                continue
            b.r[k] = ev
        for b in writes:
            if b.multi:
                b.r[k] = ev
            else:
                b.w = ev
                b.r = {}

    def op(self, eng, reads, writes, fn, inc=True):
        E = self.eng[eng]
        self._deps(E, reads, writes, skip_self=(eng == "pe"))
        ins = fn(E.h)
        self.nins += 1
        if inc:
            E.count += 1
            ins.then_inc(E.sem, 1)
            ev = (E.sem, E.count)
            self._mark(ev, E.pend_r + list(reads), E.pend_w + list(writes))
            for b in E.pend_r + E.pend_w:
                self.pending.discard(id(b))
            E.pend_r = []
            E.pend_w = []
        else:
            E.pend_r += list(reads)
            E.pend_w += list(writes)
            for b in list(reads) + list(writes):
                self.pending.add(id(b))
        return ins

    def dma(self, q, out_ap, in_ap, reads, writes, **kw):
        E = self.eng[q]
        Dq = self.dq[q]
        i = Dq["n"]
        Dq["n"] += 1
        sem = Dq["sems"][i % 8]
        prev = 16 * (i // 8)
        if prev > 0:
            self._wait(E, (sem, prev))
        self._deps(E, reads, writes)
        ins = E.h.dma_start(out=out_ap, in_=in_ap, **kw)
        ins.then_inc(sem, 16)
        self.nins += 1
        ev = (sem, prev + 16)
        self._mark(ev, reads, writes)
        return ev

    def wait_all_dma(self, q):
        E = self.eng[q]
        Dq = self.dq[q]
        n = Dq["n"]
        for s in range(8):
            cnt = (n - s + 7) // 8
            if cnt > 0:
                self._wait(E, (Dq["sems"][s], 16 * cnt))

    def collective(self, sem, in_ap, out_ap, reads, writes):
        E = self.eng["pool"]
        self._deps(E, reads, writes)
        ins = E.h.collective_compute("AllGather", ALU.bypass,
                                     replica_groups=[list(range(NCORES))],
                                     ins=[in_ap], outs=[out_ap])
        ins.then_inc(sem)
        self.nins += 1
        self._mark((sem, 1), reads, writes)

    def barrier(self):
        for q in ("sp", "pool"):
            self.wait_all_dma(q)
        evs = []
        for name, E in self.eng.items():
            assert not E.pend_r and not E.pend_w
            if E.count > 0:
                self._wait(E, (E.sem, E.count))
            E.count += 1
            E.h.sem_inc(E.sem, 1)
            evs.append((E.sem, E.count))
        for name, E in self.eng.items():
            for ev in evs:
                self._wait(E, ev)


def build_program(stop_after=None, sim=False, test=None):
    nc = bass.Bass("TRN2", target_bir_lowering=False)
    fw = FW(nc)
    ges = fw.es

    xT_d = nc.dram_tensor("xT", [D, TOK], F32, kind="ExternalInput")
    wA_d = nc.dram_tensor("wA", [22 * 128, 2048], F32, kind="ExternalInput")
    wB_d = nc.dram_tensor("wB", [8 * 128, 2816], F32, kind="ExternalInput")
    wC_d = nc.dram_tensor("wC", [15 * 128, 1024], F32, kind="ExternalInput")
    cst_d = nc.dram_tensor("consts", [128, NCONST], F32, kind="ExternalInput")
    out_d = nc.dram_tensor("outT", [D, TOK], F32, kind="ExternalOutput")

    ext = {}
    if test == "attn":
        ext = {"locF0_0": "ExternalInput", "locF0_1": "ExternalInput", "sendR0": "ExternalOutput"}
    if test == "gla":
        ext = {"locF1_0": "ExternalInput", "locF1_1": "ExternalInput", "sendR1": "ExternalOutput"}
    if test == "loop1":
        ext = {"hT": "ExternalInput", "locR0_0": "ExternalInput", "locR0_1": "ExternalInput", "cdr0": "ExternalInput",
               "locF0_0": "ExternalInput", "sendF1": "ExternalOutput", "rdr1": "ExternalOutput"}
    if test == "loop2":
        ext = {"hT": "ExternalInput", "locR1_0": "ExternalInput", "locR1_1": "ExternalInput", "rdr1": "ExternalInput",
               "sendF0": "ExternalOutput", "cdr0": "ExternalOutput"}

    def idram(name, shape, dt):
        if name in ext:
            return nc.dram_tensor(name, list(shape), dt, kind=ext[name])
        return nc.dram_tensor(name, list(shape), dt)

    wsA = idram("wsA", [22 * 128, 2048], BF16); wsB = idram("wsB", [8 * 128, 2816], BF16); wsC = idram("wsC", [15 * 128, 1024], BF16)
    if sim:
        waA = nc.dram_tensor("waA", [176 * 128, 2048], BF16, kind="ExternalInput")
        waB = nc.dram_tensor("waB", [64 * 128, 2816], BF16, kind="ExternalInput")
        waC = nc.dram_tensor("waC", [120 * 128, 1024], BF16, kind="ExternalInput")
    else:
        waA = idram("waA", [176 * 128, 2048], BF16)
        waB = idram("waB", [64 * 128, 2816], BF16)
        waC = idram("waC", [120 * 128, 1024], BF16)
    hT_d = idram("hT", [D, TOK], F32)
    sendF = {}; gathF = {}; sendR = {}; gathR = {}; cdr = {}; rdr = {}
    for l in range(2):
        if l % 2 == 0:
            sendF[l] = idram(f"sendF{l}", [RE, TOK], BF16); gathF[l] = idram(f"gathF{l}", [8 * RE, TOK], BF16)
            sendR[l] = idram(f"sendR{l}", [512, TOK], BF16); gathR[l] = idram(f"gathR{l}", [8 * 512, TOK], BF16)
            cdr[l] = idram(f"cdr{l}", [512, TOK], BF16)
        else:
            sendF[l] = idram(f"sendF{l}", [RG, TOK], BF16); gathF[l] = idram(f"gathF{l}", [8 * RG, TOK], BF16)
            sendR[l] = idram(f"sendR{l}", [1024, TOK], BF16); gathR[l] = idram(f"gathR{l}", [8 * 1024, TOK], BF16)
            rdr[l] = idram(f"rdr{l}", [D, TOK], F32)
    for l in (2, 3):
        sendF[l] = sendF[l - 2]; gathF[l] = gathF[l - 2]; sendR[l] = sendR[l - 2]; gathR[l] = gathR[l - 2]
        if l == 2: cdr[l] = cdr[0]
        else: rdr[l] = rdr[1]
    T_wsA = T(wsA, "wsA", multi=True); T_waA = T(waA, "waA")
    T_wsB = T(wsB, "wsB", multi=True); T_waB = T(waB, "waB")
    T_wsC = T(wsC, "wsC", multi=True); T_waC = T(waC, "waC")
    T_h = [T(hT_d, f"hT{i}") for i in range(NTT)]
    T_sendF = {l: T(sendF[l], f"sendF{l}", multi=True) for l in range(2)}
    T_gathF = {l: T(gathF[l], f"gathF{l}") for l in range(2)}
    T_sendR = {l: T(sendR[l], f"sendR{l}", multi=True) for l in range(2)}
    T_gathR = {l: T(gathR[l], f"gathR{l}") for l in range(2)}
    T_cdr = {0: [T(cdr[0], f"cdr_{i}") for i in range(NTT)]}
    T_rdr = {1: [T(rdr[1], f"rdr_{i}") for i in range(NTT)]}
    for l in (2, 3):
        T_sendF[l] = T_sendF[l - 2]; T_gathF[l] = T_gathF[l - 2]; T_sendR[l] = T_sendR[l - 2]; T_gathR[l] = T_gathR[l - 2]
    T_cdr[2] = T_cdr[0]; T_rdr[3] = T_rdr[1]
    T_out = T(out_d, "out", multi=True)
    cc_sems = [fw.sem(f"cc{i}") for i in range(11)]
    cc_i = [0]

    def allgather(send, gath, Ts, Tg):
        fw.collective(cc_sems[cc_i[0]], send.ap().opt(), gath.ap().opt(), [Ts], [Tg])
        cc_i[0] += 1

    pid = nc.partition_id([mybir.EngineType.Pool])
    B0 = nc.gpsimd.snap(pid + (pid // 2) * 2)
    locF = {}; locR = {}; T_locF = {}; T_locR = {}
    for l in range(2):
        rf, rr = (BE, 256) if l % 2 == 0 else (BG, 512)
        locF[l] = [idram(f"locF{l}_{s_}", [rf, TOK], BF16) for s_ in range(2)]
        locR[l] = [idram(f"locR{l}_{s_}", [rr, TOK], BF16) for s_ in range(2)]
        T_locF[l] = [T(locF[l][s_], f"locF{l}_{s_}") for s_ in range(2)]
        T_locR[l] = [T(locR[l][s_], f"locR{l}_{s_}") for s_ in range(2)]
    for l in (2, 3):
        locF[l] = locF[l - 2]; locR[l] = locR[l - 2]; T_locF[l] = T_locF[l - 2]; T_locR[l] = T_locR[l - 2]

    def fetch_blocks(gath, Tg, loc, Tloc, rows):
        v3 = gath.ap().rearrange("(b r) t -> b r t", r=rows)
        for s_ in range(2):
            fw.dma("pool", loc[s_].ap(), v3[2 * s_:, :, :][bass.ds(B0, 1), :, :].rearrange("o r t -> (o r) t"),
                   [Tg], [Tloc[s_]])

    cb_t = fw.sb(ges, "cb", [128, NBF], BF16); cb = T(cb_t, "cb")
    cf_t = fw.sb(ges, "cf", [128, NF32], F32); cf = T(cf_t, "cf")
    glr_aug = fw.tile(ges, "glr_aug", [32, TT], BF16)

    def cfc(col, n=1):
        return cf[:, col - NBF: col - NBF + n]

    with ExitStack() as es:
        ctmp = fw.tile(es, "ctmp", [128, NCONST], F32)
        fw.dma("sp", ctmp[:], cst_d.ap(), [], [ctmp])
        fw.op("dve", [ctmp], [cb], lambda e: e.tensor_copy(cb[:, 0:2048], ctmp[:, 0:2048]))
        fw.op("pool", [ctmp], [cb], lambda e: e.tensor_copy(cb[:, 2048:NBF], ctmp[:, 2048:NBF]))
        fw.op("act", [ctmp], [cf], lambda e: e.activation(out=cf[:], in_=ctmp[:, NBF:NCONST], func=AF.Copy))
        fw.op("pool", [], [glr_aug], lambda e: e.memset(glr_aug[:], 1.0))
        wi = 0
        for (src, dst, Tdst, n, L) in (() if sim else ((wA_d, wsA, T_wsA, 22, 2048), (wB_d, wsB, T_wsB, 8, 2816),
                                       (wC_d, wsC, T_wsC, 15, 1024))):
            st32 = [fw.tile(es, f"wst32_{L}_{i}", [128, L], F32) for i in range(2)]
            st16 = [fw.tile(es, f"wst16_{L}_{i}", [128, L], BF16) for i in range(2)]
            for u in range(n):
                a, b_ = st32[u % 2], st16[u % 2]
                fw.dma("sp", a[:], src.ap()[u * 128:(u + 1) * 128, :], [], [a])
                eng = ("dve", "pool", "act")[wi % 3]; wi += 1
                if eng == "act":
                    fw.op("act", [a], [b_], lambda e, a=a, b_=b_: e.activation(out=b_[:], in_=a[:], func=AF.Copy))
                else:
                    fw.op(eng, [a], [b_], lambda e, a=a, b_=b_: e.tensor_copy(b_[:], a[:]))
                fw.dma("pool", dst.ap()[u * 128:(u + 1) * 128, :], b_[:], [b_], [Tdst])
        if not sim:
            allgather(wsA, waA, T_wsA, T_waA)
            allgather(wsB, waB, T_wsB, T_waB)
            allgather(wsC, waC, T_wsC, T_waC)
        fw.barrier()

    cb_id = lambda: cb[:, C_ID:C_ID + 128]
    cb_ones = lambda: cb[:, C_ONES:C_ONES + 128]

    def uA(l, which, fc): return (l * 2 + which) * 22 + fc
    def uB(l, which, dc): return (l * 2 + which) * 8 + dc
    UC_BASE = {0: 0, 2: 28, 1: 56, 3: 88}

    def token_loop(k):
        lb = k - 1 if k > 0 else None
        la = k if k < 4 else None
        with ExitStack() as es:
            P = [fw.ptile(es, f"P{i}", [128, TT], F32) for i in range(8)]
            h_t, h = fw.tiles(es, "h", 8, [128, TT], F32)
            xn_t, xn = fw.tiles(es, "xn", 8, [128, TT], BF16)
            sq_t, sq = fw.tiles(es, "sq", 8, [128, TT], BF16)
            f_t, f = fw.tiles(es, "f", 8, [128, TT], F32)
            hm_t, hm = fw.tiles(es, "hm", NFC, [128, TT], BF16)
            cat_t, cat = fw.tiles(es, "cat", 8, [128, TT], BF16)
            lnv = fw.tile(es, "lnv", [128, TT], F32)
            rstd = fw.tile(es, "rstd", [128, TT], F32)
            sa = [fw.tile(es, f"sa{i}", [128, TT], F32) for i in range(2)]
            tmp = [fw.tile(es, f"tmp{i}", [128, TT], F32) for i in range(3)]
            stg = [fw.tile(es, f"stg{i}", [128, TT], BF16) for i in range(3)]
            stg32 = [fw.tile(es, f"stg32_{i}", [128, TT], F32) for i in range(2)]
            NSA, NSB, NSC = 3, 2, 6
            slotA = [fw.tile(es, f"slA{i}", [128, 2048], BF16) for i in range(NSA)]
            slotB = [fw.tile(es, f"slB{i}", [128, 2816], BF16) for i in range(NSB)]
            slotC = [fw.tile(es, f"slC{i}", [128, 1024], BF16) for i in range(NSC)]
            cnt = {"A": 0, "B": 0, "C": 0, "tmp": 0, "stg": 0, "sa": 0, "stg32": 0}
            even_b = lb is not None and lb % 2 == 0
            odd_b = lb is not None and lb % 2 == 1
            if even_b:
                eb_ = lb // 2
                cbuf_t, cbuf = fw.tiles(es, "cbuf", 4, [128, 32 + TT], BF16)
                dg = fw.tile(es, "dg", [128, 124 * 128], BF16)
                ysb_t, ysb = fw.tiles(es, "ysb", 4, [128, TT], F32)
                ybf_t, ybf = fw.tiles(es, "ybf", 4, [128, TT], BF16)
                mean = fw.tile(es, "mean", [128, TT], F32)
                for i in range(124):
                    col = C_CONVW + eb_ * 124 + i
                    fw.op("pool" if i % 2 else "dve", [cb, cf], [dg],
                          lambda e, i=i, col=col: e.tensor_scalar(dg[:, i * 128:(i + 1) * 128], cb_id(),
                                                                   cfc(col), None, op0=ALU.mult))
            if odd_b:
                ob_t, ob = fw.tiles(es, "ob", 8, [128, TT], BF16)

            def nxt(key, lst):
                i = cnt[key]; cnt[key] += 1
                return lst[i % len(lst)]

            def wload(kind, unit):
                if kind == "A":
                    s = nxt("A", slotA); src = waA.ap()[unit * 128:(unit + 1) * 128, :]; Tsrc = T_waA
                elif kind == "B":
                    s = nxt("B", slotB); src = waB.ap()[unit * 128:(unit + 1) * 128, :]; Tsrc = T_waB
                else:
                    s = nxt("C", slotC); src = waC.ap()[unit * 128:(unit + 1) * 128, :]; Tsrc = T_waC
                fw.dma("sp", s[:], src, [Tsrc], [s])
                return s

            plan = []
            def plan_ffn(l, which):
                for fc in range(NFC): plan.append(("A", uA(l, which, fc)))
                for dc in range(8): plan.append(("B", uB(l, which, dc)))
            if even_b:
                for dc in range(8): plan.append(("C", UC_BASE[lb] + 20 + dc))
            if odd_b:
                for dc in range(8): plan.append(("C", UC_BASE[lb] + 24 + dc))
            if lb is not None: plan_ffn(lb, 1)
            if la is not None:
                plan_ffn(la, 0)
                if la % 2 == 0:
                    order = list(range(12)) + [12, 16, 13, 17, 14, 18, 15, 19]
                else:
                    order = list(range(24))
                for u in order: plan.append(("C", UC_BASE[la] + u))
            full = plan * NTT
            loaded = []
            ptr = {"issue": 0, "use": 0}
            NS = {"A": NSA, "B": NSB, "C": NSC}

            def wget(kind):
                i = ptr["use"]
                assert full[i][0] == kind, (full[i], kind, i)
                lo = max(i - 1, 0)
                while ptr["issue"] < len(full) and ptr["issue"] - i <= 12:
                    kd, un = full[ptr["issue"]]
                    infl = sum(1 for jj in range(lo, ptr["issue"]) if full[jj][0] == kd)
                    if infl >= NS[kd]:
                        assert ptr["issue"] > i
                        break
                    loaded.append(wload(kd, un))
                    ptr["issue"] += 1
                ptr["use"] += 1
                return loaded[i]

            def gcol(l, i):
                return C_NORMG + (l * 6 + i) * 8

            def rstd_from(psT, n, extra_bias=0.0):
                fw.op("act", [psT], [lnv], lambda e: e.activation(out=lnv[:], in_=psT[:], func=AF.Ln,
                                                                   scale=1.0 / n, bias=EPS))
                fw.op("act", [lnv], [rstd], lambda e: e.activation(out=rstd[:], in_=lnv[:], func=AF.Exp,
                                                                    scale=-0.5, bias=extra_bias))

            def sumsq_mm(ps, srcs):
                n = len(srcs)
                for i, s in enumerate(srcs):
                    fw.op("pe", [cb, s], [ps], lambda e, s=s, i=i: e.matmul(ps[:], cb_ones(), s[:], start=(i == 0),
                                                                           stop=(i == n - 1)), inc=(i == n - 1))

            def prenorm(l, gi):
                for kc in range(8):
                    fw.op("act", [h[kc]], [sq[kc]], lambda e, kc=kc: e.activation(out=sq[kc][:], in_=h[kc][:], func=AF.Square))
                sumsq_mm(P[6], sq)
                rstd_from(P[6], D)
                gc = gcol(l, gi)
                for kc in range(8):
                    fw.op("dve", [h[kc], cf, rstd], [xn[kc]],
                          lambda e, kc=kc: e.scalar_tensor_tensor(out=xn[kc][:], in0=h[kc][:], scalar=cfc(gc + kc),
                                                                  in1=rstd[:], op0=ALU.mult, op1=ALU.mult))

            def post_residual(l, gi, res_scale):
                sumsq_mm(P[6], sq)
                rstd_from(P[6], D, extra_bias=math.log(res_scale))
                gc = gcol(l, gi)
                for dc in range(8):
                    tp = nxt("tmp", tmp)
                    fw.op("dve", [f[dc], cf, rstd], [tp],
                          lambda e, dc=dc, tp=tp: e.scalar_tensor_tensor(out=tp[:], in0=f[dc][:], scalar=cfc(gc + dc),
                                                                         in1=rstd[:], op0=ALU.mult, op1=ALU.mult))
                    fw.op("pool", [tp, h[dc]], [h[dc]],
                          lambda e, dc=dc, tp=tp: e.tensor_tensor(h[dc][:], tp[:], h[dc][:], op=ALU.add))

            def evac_f(ps, dc):
                fw.op("dve", [ps], [f[dc]], lambda e: e.tensor_copy(f[dc][:], ps[:]))
                fw.op("act", [f[dc]], [sq[dc]], lambda e: e.activation(out=sq[dc][:], in_=f[dc][:], func=AF.Square))

            def ffn(l, which):
                prenorm(l, 0 if which == 0 else 4)
                for fc in range(NFC):
                    wu = wget("A")
                    pa, pb = (P[0], P[1]) if fc % 2 == 0 else (P[2], P[3])
                    for half, ps in ((0, pa), (1, pb)):
                        for kc in range(8):
                            off = kc * 256 + half * 128
                            fw.op("pe", [wu, xn[kc]], [ps],
                                  lambda e, ps=ps, wu=wu, kc=kc, off=off: e.matmul(ps[:], wu[:, off:off + 128], xn[kc][:],
                                                                                  start=(kc == 0), stop=(kc == 7)),
                                  inc=(kc == 7))
                    s_ = nxt("sa", sa)
                    fw.op("act", [pa], [s_], lambda e, s_=s_, pa=pa: e.activation(out=s_[:], in_=pa[:], func=AF.Silu))
                    fw.op("dve", [s_, pb], [hm[fc]], lambda e, s_=s_, pb=pb, fc=fc: e.tensor_tensor(hm[fc][:], s_[:], pb[:], op=ALU.mult))
                for dc in range(8):
                    wo = wget("B")
                    ps = P[4 + dc % 2]
                    for fc in range(NFC):
                        fw.op("pe", [wo, hm[fc]], [ps],
                              lambda e, ps=ps, wo=wo, fc=fc: e.matmul(ps[:], wo[:, fc * 128:(fc + 1) * 128], hm[fc][:],
                                                                      start=(fc == 0), stop=(fc == NFC - 1)),
                              inc=(fc == NFC - 1))
                    evac_f(ps, dc)
                post_residual(l, 1 if which == 0 else 5, 0.5)

            def proj_fm(wu, ps, src=None):
                src = src or xn
                for kc in range(8):
                    fw.op("pe", [wu, src[kc]], [ps],
                          lambda e, kc=kc: e.matmul(ps[:], wu[:, kc * 128:(kc + 1) * 128], src[kc][:],
                                                    start=(kc == 0), stop=(kc == 7)), inc=(kc == 7))

            def proj_tm(nun, pss):
                for i in range(nun):
                    wu = wget("C")
                    for tb in range(4):
                        for kc in range(8):
                            fw.op("pe", [wu, xn[kc]], [pss[tb]],
                                  lambda e, i=i, wu=wu, tb=tb, kc=kc: e.matmul(
                                      pss[tb][:, i * 128:(i + 1) * 128], xn[kc][:, tb * 128:(tb + 1) * 128],
                                      wu[:, kc * 128:(kc + 1) * 128], start=(kc == 0), stop=(kc == 7)),
                                  inc=(kc == 7 and tb == 3))

            def out_proj_and_residual(l):
                for dc in range(8):
                    wu = wget("C")
                    ps = P[4 + dc % 2]
                    proj_fm(wu, ps, src=cat)
                    evac_f(ps, dc)
                post_residual(l, 3, 1.0)

            for tt in range(NTT):
                t0 = tt * TT
                src = hT_d if k > 0 else xT_d
                fw.dma("pool", h_t[:], src.ap()[:, t0:t0 + TT].rearrange("(kc p) t -> p kc t", p=128),
                       [T_h[tt]] if k > 0 else [], h)
                if even_b:
                    l = lb
                    for kc in range(4):
                        fw.dma("pool", cat[kc][:], locR[l][kc // 2].ap()[(kc % 2) * 128:(kc % 2 + 1) * 128, t0:t0 + TT],
                               [T_locR[l][kc // 2]], [cat[kc]])
                    fw.dma("pool", cbuf_t[:, :, 32:32 + TT],
                           cdr[l].ap()[:, t0:t0 + TT].rearrange("(cc p) t -> p cc t", p=128), [T_cdr[l][tt]], cbuf)
                    if tt > 0:
                        fw.dma("pool", cbuf_t[:, :, 0:32],
                               cdr[l].ap()[:, t0 - 32:t0].rearrange("(cc p) t -> p cc t", p=128), [T_cdr[l][tt - 1]], cbuf)
                    else:
                        fw.dma("pool", cbuf_t[:, :, 0:32],
                               locF[l][0].ap()[768:772, :].rearrange("cc (p i) -> p cc i", i=32),
                               [T_locF[l][0]], cbuf)
                        fw.op("pool", cbuf + [cf], cbuf,
                              lambda e: e.tensor_scalar(cbuf_t[:, :, 0:32], cbuf_t[:, :, 0:32], cfc(C_FLAG), None, op0=ALU.mult))
                    pc = C_CONVP + eb_ * 12
                    for cc in range(4):
                        ps = P[cc]
                        for kk in range(31):
                            fw.op("pe", [dg, cbuf[cc]], [ps],
                                  lambda e, ps=ps, cc=cc, kk=kk: e.matmul(ps[:], dg[:, (cc * 31 + kk) * 128:(cc * 31 + kk + 1) * 128],
                                                                          cbuf[cc][:, 2 + kk:2 + kk + TT], start=(kk == 0), stop=(kk == 30)),
                                  inc=(kk == 30))
                        fw.op("act", [ps, cf], [ysb[cc]], lambda e, ps=ps, cc=cc: e.activation(out=ysb[cc][:], in_=ps[:], func=AF.Identity,
                                                                                             bias=cfc(pc + cc)))
                        fw.op("act", [ysb[cc]], [sq[cc]], lambda e, cc=cc: e.activation(out=sq[cc][:], in_=ysb[cc][:], func=AF.Square))
                        fw.op("pool", [ysb[cc]], [ybf[cc]], lambda e, cc=cc: e.tensor_copy(ybf[cc][:], ysb[cc][:]))
                    sumsq_mm(P[4], ybf)
                    sumsq_mm(P[5], sq[0:4])
                    m2 = nxt("tmp", tmp)
                    var = nxt("tmp", tmp)
                    fw.op("dve", [P[4]], [mean], lambda e: e.tensor_scalar(mean[:], P[4][:], 1.0 / 512, None, op0=ALU.mult))
                    fw.op("dve", [mean], [m2], lambda e: e.tensor_tensor(m2[:], mean[:], mean[:], op=ALU.mult))
                    fw.op("dve", [P[5], m2], [var], lambda e: e.scalar_tensor_tensor(out=var[:], in0=P[5][:], scalar=1.0 / 512, in1=m2[:],
                                                                                   op0=ALU.mult, op1=ALU.subtract))
                    rstd_from(var, 1.0)
                    for cc in range(4):
                        t1 = nxt("tmp", tmp)
                        fw.op("dve", [ysb[cc], mean], [t1], lambda e, cc=cc, t1=t1: e.tensor_tensor(t1[:], ysb[cc][:], mean[:], op=ALU.subtract))
                        fw.op("pool", [t1, rstd], [t1], lambda e, t1=t1: e.tensor_tensor(t1[:], t1[:], rstd[:], op=ALU.mult))
                        fw.op("act", [t1, cf], [cat[4 + cc]],
                              lambda e, cc=cc, t1=t1: e.activation(out=cat[4 + cc][:], in_=t1[:], func=AF.Silu,
                                                                   scale=cfc(pc + 4 + cc), bias=cfc(pc + 8 + cc)))
                    out_proj_and_residual(l)
                if odd_b:
                    l = lb
                    o_ = l // 2
                    for oc in range(8):
                        fw.dma("pool", ob[oc][:], locR[l][oc // 4].ap()[(oc % 4) * 128:(oc % 4 + 1) * 128, t0:t0 + TT],
                               [T_locR[l][oc // 4]], [ob[oc]])
                    for hd in range(4):
                        for cc in range(2):
                            fw.op("act", [ob[2 * hd + cc]], [sq[cc]],
                                  lambda e, hd=hd, cc=cc: e.activation(out=sq[cc][:], in_=ob[2 * hd + cc][:], func=AF.Square))
                        sumsq_mm(P[hd % 2], sq[0:2])
                        rstd_from(P[hd % 2], 256)
                        for cc in range(2):
                            oc = 2 * hd + cc
                            t1 = nxt("tmp", tmp)
                            s32 = nxt("stg32", stg32)
                            fw.dma("pool", s32[:], rdr[l].ap()[oc * 128:(oc + 1) * 128, t0:t0 + TT], [T_rdr[l][tt]], [s32])
                            fw.op("dve", [ob[oc], cf, rstd], [t1],
                                  lambda e, oc=oc, cc=cc, t1=t1: e.scalar_tensor_tensor(out=t1[:], in0=ob[oc][:],
                                                                                        scalar=cfc(C_GLANG + o_ * 2 + cc),
                                                                                        in1=rstd[:], op0=ALU.mult, op1=ALU.mult))
                            fw.op("pool", [t1, s32], [cat[oc]], lambda e, oc=oc, t1=t1, s32=s32: e.tensor_tensor(cat[oc][:], t1[:], s32[:], op=ALU.mult))
                    out_proj_and_residual(l)
                if lb is not None:
                    ffn(lb, 1)
                if la is not None:
                    l = la
                    ffn(l, 0)
                    prenorm(l, 2)
                    sF = sendF[l].ap()
                    if l % 2 == 0:
                        for fc in range(8):
                            wu = wget("C")
                            ps = P[fc % 4]
                            proj_fm(wu, ps)
                            st = nxt("stg", stg)
                            sc_ = 0.125 if fc < 4 else 1.0
                            fw.op("act", [ps], [st], lambda e, ps=ps, st=st, sc_=sc_: e.activation(out=st[:], in_=ps[:], func=AF.Copy, scale=sc_))
                            f4 = fc % 4
                            row0 = (f4 // 2) * BE + (256 if fc >= 4 else 0) + (f4 % 2) * 128
                            fw.dma("pool", sF[row0:row0 + 128, t0:t0 + TT], st[:], [st], [T_sendF[l]])
                        proj_tm(4, P[0:4])
                        for tb in range(4):
                            st = nxt("stg", stg)
                            fw.op("dve" if tb % 2 else "act", [P[tb]], [st],
                                  (lambda e, tb=tb, st=st: e.tensor_copy(st[:], P[tb][:])) if tb % 2 else
                                  (lambda e, tb=tb, st=st: e.activation(out=st[:], in_=P[tb][:], func=AF.Copy)))
                            for hf in range(2):
                                vsec = sF[hf * BE + 512:hf * BE + 768, :].rearrange("r (t16 c) -> (r t16) c", c=256)
                                fw.dma("pool", vsec[t0 + tb * 128:t0 + (tb + 1) * 128, :], st[:, hf * 256:(hf + 1) * 256], [st], [T_sendF[l]])
                        for fc in range(4):
                            pu, pg = (P[4], P[5]) if fc % 2 == 0 else (P[6], P[7])
                            wuu = wget("C")
                            wug = wget("C")
                            proj_fm(wuu, pu)
                            proj_fm(wug, pg)
                            s_ = nxt("sa", sa)
                            st = nxt("stg", stg)
                            fw.op("act", [pg], [s_], lambda e, pg=pg, s_=s_: e.activation(out=s_[:], in_=pg[:], func=AF.Sigmoid))
                            fw.op("dve", [s_, pu], [st], lambda e, pu=pu, s_=s_, st=st: e.tensor_tensor(st[:], s_[:], pu[:], op=ALU.mult))
                            fw.dma("pool", cdr[l].ap()[fc * 128:(fc + 1) * 128, t0:t0 + TT], st[:], [st], [T_cdr[l][tt]])
                            if tt == NTT - 1:
                                for hf in range(2):
                                    fw.dma("pool", sF[hf * BE + 768 + fc:hf * BE + 769 + fc, :].rearrange("o (p i) -> (o p) i", i=32),
                                           st[:, TT - 32:TT], [st], [T_sendF[l]])
                    else:
                        o_ = l // 2
                        for fc in range(8):
                            wu = wget("C")
                            ps = P[fc % 4]
                            proj_fm(wu, ps)
                            st = nxt("stg", stg)
                            fw.op("act", [ps], [st], lambda e, ps=ps, st=st: e.activation(out=st[:], in_=ps[:], func=AF.Copy))
                            f4 = fc % 4
                            row0 = (f4 // 2) * BG + (256 if fc >= 4 else 0) + (f4 % 2) * 128
                            fw.dma("pool", sF[row0:row0 + 128, t0:t0 + TT], st[:], [st], [T_sendF[l]])
                        for hf in range(2):
                            vsec = sF[hf * BG + 512:hf * BG + 1024, :].rearrange("r (t8 c) -> (r t8) c", c=512)
                            pss = P[0:4] if hf == 0 else P[4:8]
                            proj_tm(4, pss)
                            for tb in range(4):
                                st = nxt("stg", stg)
                                fw.op("dve" if tb % 2 else "act", [pss[tb]], [st],
                                      (lambda e, tb=tb, st=st, pss=pss: e.tensor_copy(st[:], pss[tb][:])) if tb % 2 else
                                      (lambda e, tb=tb, st=st, pss=pss: e.activation(out=st[:], in_=pss[tb][:], func=AF.Copy)))
                                fw.dma("pool", vsec[t0 + tb * 128:t0 + (tb + 1) * 128, :], st[:], [st], [T_sendF[l]])
                        for fc in range(8):
                            wu = wget("C")
                            ps = P[fc % 4]
                            proj_fm(wu, ps)
                            s32 = nxt("stg32", stg32)
                            fw.op("act", [ps], [s32], lambda e, ps=ps, s32=s32: e.activation(out=s32[:], in_=ps[:], func=AF.Silu))
                            fw.dma("pool", rdr[l].ap()[fc * 128:(fc + 1) * 128, t0:t0 + TT], s32[:], [s32], [T_rdr[l][tt]])
                        psg = P[4]
                        for kc in range(8):
                            c0 = C_WGLR + o_ * 128 + kc * 16
                            fw.op("pe", [cb, xn[kc]], [psg],
                                  lambda e, kc=kc, c0=c0: e.matmul(psg[0:16, :], cb[:, c0:c0 + 16], xn[kc][:], start=(kc == 0), stop=(kc == 7)),
                                  inc=(kc == 7))
                        fw.op("act", [psg], [glr_aug], lambda e: e.activation(out=glr_aug[0:16, :], in_=psg[0:16, :], func=AF.Copy))
                        for tb in range(4):
                            ps = P[tb]
                            fw.op("pe", [glr_aug, cb], [ps],
                                  lambda e, tb=tb, ps=ps: e.matmul(ps[:], glr_aug[0:17, tb * 128:(tb + 1) * 128],
                                                                   cb[0:17, C_WGATE + o_ * 512:C_WGATE + (o_ + 1) * 512], start=True, stop=True))
                            s_ = nxt("sa", sa)
                            st = nxt("stg", stg)
                            fw.op("act", [ps], [s_], lambda e, ps=ps, s_=s_: e.activation(out=s_[:], in_=ps[:], func=AF.Exp, scale=-1.0))
                            fw.op("act", [s_], [st], lambda e, s_=s_, st=st: e.activation(out=st[:], in_=s_[:], func=AF.Ln, bias=1.0))
                            for hf in range(2):
                                gsec = sF[hf * BG + 1024:hf * BG + 1280, :].rearrange("r (t16 c) -> (r t16) c", c=256)
                                fw.dma("pool", gsec[t0 + tb * 128:t0 + (tb + 1) * 128, :], st[:, hf * 256:(hf + 1) * 256], [st], [T_sendF[l]])
                last = (k == 4) or (stop_after is not None and k == stop_after)
                dstd = out_d if last else hT_d
                fw.dma("pool", dstd.ap()[:, t0:t0 + TT].rearrange("(kc p) t -> p kc t", p=128), h_t[:],
                       h, [T_out] if last else [T_h[tt]])
            assert ptr["use"] == len(full), (ptr, len(full))
            fw.barrier()

    def attention(l):
        with ExitStack() as es:
            P = [fw.ptile(es, f"PA{i}", [128, TT], F32) for i in range(8)]
            QT_t, QT = fw.tiles(es, "QT", 2, [128, SEQ], BF16)
            KT_t, KT = fw.tiles(es, "KT", 2, [128, SEQ], BF16)
            V = fw.tile(es, "V", [128, 64, 256], BF16)
            OT_t, OT = fw.tiles(es, "OT", 2, [128, SEQ], BF16)
            E = [fw.tile(es, f"E{i}", [128, TT], F32) for i in range(2)]
            L1 = [fw.tile(es, f"L1{i}", [128, TT], BF16) for i in range(3)]
            X = [fw.tile(es, f"X{i}", [128, TT], F32) for i in range(2)]
            W = [fw.tile(es, f"W{i}", [128, TT], BF16) for i in range(3)]
            Cc = fw.tile(es, "Cc", [128, TT], F32)
            for s in range(2):
                lf = locF[l][s].ap()
                for hp in range(2):
                    fw.dma("pool", QT[hp][:, s * TOK:(s + 1) * TOK], lf[hp * 128:(hp + 1) * 128, :], [T_locF[l][s]], [QT[hp]])
                    fw.dma("pool", KT[hp][:, s * TOK:(s + 1) * TOK], lf[256 + hp * 128:256 + (hp + 1) * 128, :], [T_locF[l][s]], [KT[hp]])
                fw.dma("pool", V[:, s * 32:(s + 1) * 32, :],
                       lf[512:768, :].rearrange("y (t16 c) -> (y t16) c", c=256).rearrange("(blk p) c -> p blk c", p=128),
                       [T_locF[l][s]], [V])
            cnt = {"E": 0, "L1": 0, "X": 0, "W": 0, "z": 0, "x": 0, "s": 0, "o": 0}
            if ATT_NH < 4 or ATT_NG < 16:
                for hp in range(2):
                    fw.op("pool", [], [OT[hp]], lambda e, hp=hp: e.memset(OT[hp][:], 0.0))

            def nxt(key, lst):
                i = cnt[key]; cnt[key] += 1
                return lst[i % len(lst)]

            for hl in range(ATT_NH):
                hp, hh = hl // 2, hl % 2
                pl, ph = hh * 64, hh * 64 + 64
                for g in range(ATT_NG):
                    q0 = g * TT
                    Ob = nxt("o", [P[6], P[7]])
                    jtop = 4 * g + 3
                    for jb in range(jtop, -1, -1):
                        dgn = jb - 4 * g
                        k0 = jb * 128
                        Zb = nxt("z", [P[0], P[1]])
                        Xb = nxt("x", [P[2], P[3]])
                        Sb = nxt("s", [P[4], P[5]])
                        e_ = nxt("E", E); l1 = nxt("L1", L1); w_ = nxt("W", W)
                        fw.op("pe", [KT[hp], QT[hp]], [Zb],
                              lambda e: e.matmul(Zb[:], KT[hp][pl:ph, k0:k0 + 128], QT[hp][pl:ph, q0:q0 + TT], start=True, stop=True))
                        fw.op("act", [Zb], [e_], lambda e: e.activation(out=e_[:], in_=Zb[:], func=AF.Exp))
                        fw.op("act", [e_], [l1], lambda e: e.activation(out=l1[:], in_=e_[:], func=AF.Ln, bias=1.0))
                        if dgn >= 0:
                            mk = C_AMASK + dgn * 512
                            fw.op("pool", [l1, cb], [l1], lambda e: e.tensor_tensor(l1[:], l1[:], cb[:, mk:mk + 512], op=ALU.mult))
                        fw.op("pe", [KT[hp], QT[hp]], [Xb],
                              lambda e: e.matmul(Xb[:], KT[hp][pl:ph, k0:k0 + 128], QT[hp][pl:ph, q0:q0 + TT], start=True, stop=False), inc=False)
                        fw.op("pe", [cb, l1], [Xb],
                              lambda e: e.matmul(Xb[:], cb[:, C_NEGU:C_NEGU + 128], l1[:], start=False, stop=True))
                        if jb > 0:
                            fw.op("pe", [cb, l1], [Sb], lambda e: e.matmul(Sb[:], cb_ones(), l1[:], start=True, stop=True))
                        if jb == jtop:
                            fw.op("act", [Xb], [w_], lambda e: e.activation(out=w_[:], in_=Xb[:], func=AF.Exp))
                            if jb > 0:
                                fw.op("dve", [Sb], [Cc], lambda e: e.tensor_copy(Cc[:], Sb[:]))
                        else:
                            x_ = nxt("X", X)
                            fw.op("dve", [Xb, Cc], [x_], lambda e: e.tensor_tensor(x_[:], Xb[:], Cc[:], op=ALU.subtract))
                            fw.op("act", [x_], [w_], lambda e: e.activation(out=w_[:], in_=x_[:], func=AF.Exp))
                            if jb > 0:
                                fw.op("dve", [Sb, Cc], [Cc], lambda e: e.tensor_tensor(Cc[:], Cc[:], Sb[:], op=ALU.add))
                        if dgn >= 0:
                            mk = C_AMASK + dgn * 512
                            fw.op("pool", [w_, cb], [w_], lambda e: e.tensor_tensor(w_[:], w_[:], cb[:, mk:mk + 512], op=ALU.mult))
                        fw.op("pe", [V, w_], [Ob],
                              lambda e: e.matmul(Ob[pl:ph, :], V[:, jb, hl * 64:(hl + 1) * 64], w_[:], start=(jb == jtop), stop=(jb == 0)),
                              inc=(jb == 0))
                    fw.op("dve", [Ob], [OT[hp]], lambda e: e.tensor_copy(OT[hp][pl:ph, q0:q0 + TT], Ob[pl:ph, :]))
            for hf in range(2):
                fw.dma("pool", sendR[l].ap()[hf * 256:(hf + 1) * 256, :].rearrange("(hp p) t -> p hp t", p=128),
                       OT_t[:, :, hf * TOK:(hf + 1) * TOK], OT, [T_sendR[l]])
            fw.barrier()

    def gla(l):
        with ExitStack() as es:
            Pb = [fw.ptile(es, f"PGb{i}", [128, TT], F32) for i in range(2)]
            Pa = [fw.ptile(es, f"PGa{i}", [128, TT], F32) for i in range(2)]
            Po = [fw.ptile(es, f"PGo{i}", [128, TT], F32) for i in range(2)]
            Ps = fw.ptile(es, "PGs", [128, TT], F32)
            Pt = fw.ptile(es, "PGt", [128, 1024], BF16)
            NSEG = 8
            SEGT = SEQ // NSEG
            QT = [fw.tile(es, f"gQ{i}", [128, 2, SEGT], BF16) for i in range(2)]
            KT = [fw.tile(es, f"gK{i}", [128, 2, SEGT], BF16) for i in range(2)]
            Vs = [fw.tile(es, f"gV{i}", [128, 8, 512], BF16) for i in range(2)]
            Gs = [fw.tile(es, f"gG{i}", [128, 8, 256], BF16) for i in range(2)]
            Os = [fw.tile(es, f"gO{i}", [128, 4, SEGT], BF16) for i in range(2)]
            S32 = [fw.tile(es, f"S32_{i}", [128, 256], F32) for i in range(2)]
            Sbf = [fw.tile(es, f"Sbf_{i}", [128, 256], BF16) for i in range(2)]
            tS = [fw.tile(es, f"tS_{i}", [128, 256], F32) for i in range(2)]
            ebt = [fw.tile(es, f"eb{i}", [128, 128], F32) for i in range(4)]
            enb = [fw.tile(es, f"enb{i}", [128, 128], F32) for i in range(4)]
            qt = [fw.tile(es, f"qt{i}", [128, 128], BF16) for i in range(4)]
            kt = [fw.tile(es, f"kt{i}", [128, 128], BF16) for i in range(4)]
            ktok = [fw.tile(es, f"ktok{i}", [128, 128], BF16) for i in range(4)]
            Am = [fw.tile(es, f"Am{i}", [128, 128], BF16) for i in range(4)]
            for i in range(2):
                fw.op("pool", [], [S32[i]], lambda e, i=i: e.memset(S32[i][:], 0.0))
                fw.op("pool", [], [Sbf[i]], lambda e, i=i: e.memset(Sbf[i][:], 0.0))
            sR = sendR[l].ap()
            it = [0]
            for sg in range(min(NSEG, GLA_NSEG)):
                hft = sg // 4
                loc = (sg % 4) * SEGT
                q_, k_, v_, g_, o_t = QT[sg % 2], KT[sg % 2], Vs[sg % 2], Gs[sg % 2], Os[sg % 2]
                lf = locF[l][hft].ap()
                Tl = T_locF[l][hft]
                fw.dma("pool", q_[:], lf[0:256, loc:loc + SEGT].rearrange("(h p) t -> p h t", p=128), [Tl], [q_])
                fw.dma("pool", k_[:], lf[256:512, loc:loc + SEGT].rearrange("(h p) t -> p h t", p=128), [Tl], [k_])
                fw.dma("pool", v_[:],
                       lf[512:1024, :].rearrange("y (t8 c) -> (y t8) c", c=512)[loc:loc + SEGT, :].rearrange("(ch p) c -> p ch c", p=128),
                       [Tl], [v_])
                fw.dma("pool", g_[:],
                       lf[1024:1280, :].rearrange("y (t16 c) -> (y t16) c", c=256)[loc:loc + SEGT, :].rearrange("(ch p) c -> p ch c", p=128),
                       [Tl], [g_])
                for ch in range(8):
                    c0 = ch * 128
                    for hh in range(2):
                        i4 = it[0] % 4; it[0] += 1
                        pb = Pb[i4 % 2]; pa = Pa[i4 % 2]; po = Po[i4 % 2]
                        eb_, en_, qt_, kt_, ktk, am = ebt[i4], enb[i4], qt[i4], kt[i4], ktok[i4], Am[i4]
                        fw.op("pe", [g_, cb], [pb], lambda e: e.matmul(pb[:, 0:128], g_[:, ch, hh * 128:(hh + 1) * 128],
                                                                       cb[:, C_TRIN:C_TRIN + 128], start=True, stop=True))
                        fw.op("act", [pb], [eb_], lambda e: e.activation(out=eb_[:], in_=pb[:, 0:128], func=AF.Exp))
                        fw.op("act", [pb], [en_], lambda e: e.activation(out=en_[:], in_=pb[:, 0:128], func=AF.Exp, scale=-1.0))
                        fw.op("dve", [q_, eb_], [qt_], lambda e: e.scalar_tensor_tensor(out=qt_[:], in0=q_[:, hh, c0:c0 + 128], scalar=128.0 ** -0.5,
                                                                                        in1=eb_[:], op0=ALU.mult, op1=ALU.mult))
                        fw.op("pool", [k_, en_], [kt_], lambda e: e.tensor_tensor(kt_[:], k_[:, hh, c0:c0 + 128], en_[:], op=ALU.mult))
                        ptv = Pt[:, i4 * 128:(i4 + 1) * 128]
                        fw.op("pe", [kt_, cb], [Pt], lambda e: e.transpose(ptv, kt_[:], cb_id()))
                        fw.op("act", [Pt], [ktk], lambda e: e.activation(out=ktk[:], in_=ptv, func=AF.Copy))
                        fw.op("pe", [kt_, qt_], [pa], lambda e: e.matmul(pa[:, 0:128], kt_[:], qt_[:], start=True, stop=True))
                        fw.op("dve", [pa, cb], [am], lambda e: e.tensor_tensor(am[:], pa[:, 0:128], cb[:, C_GMASK:C_GMASK + 128], op=ALU.mult))
                        for vc in range(2):
                            fw.op("pe", [v_, am], [po],
                                  lambda e, vc=vc: e.matmul(po[:, vc * 128:(vc + 1) * 128], v_[:, ch, hh * 256 + vc * 128:hh * 256 + (vc + 1) * 128],
                                                            am[:], start=True, stop=False), inc=False)
                            fw.op("pe", [Sbf[hh], qt_], [po],
                                  lambda e, vc=vc: e.matmul(po[:, vc * 128:(vc + 1) * 128], Sbf[hh][:, vc * 128:(vc + 1) * 128],
                                                            qt_[:], start=False, stop=True), inc=(vc == 1))
                        fw.op("act", [po], [o_t], lambda e: e.activation(out=o_t[:, hh * 2:hh * 2 + 2, c0:c0 + 128],
                                                                       in_=po[:, 0:256].rearrange("p (v t) -> p v t", v=2), func=AF.Copy))
                        fw.op("pe", [ktk, v_], [Ps], lambda e: e.matmul(Ps[:, hh * 256:(hh + 1) * 256], ktk[:], v_[:, ch, hh * 256:(hh + 1) * 256],
                                                                        start=True, stop=True))
                        fw.op("dve", [S32[hh], eb_], [tS[hh]], lambda e: e.tensor_scalar(tS[hh][:], S32[hh][:], eb_[:, 127:128], None, op0=ALU.mult))
                        fw.op("dve", [Ps, eb_, tS[hh]], [S32[hh]],
                              lambda e: e.scalar_tensor_tensor(out=S32[hh][:], in0=Ps[:, hh * 256:(hh + 1) * 256], scalar=eb_[:, 127:128],
                                                               in1=tS[hh][:], op0=ALU.mult, op1=ALU.add))
                        fw.op("pool", [S32[hh]], [Sbf[hh]], lambda e: e.tensor_copy(Sbf[hh][:], S32[hh][:]))
                fw.dma("pool", sR[hft * 512:(hft + 1) * 512, loc:loc + SEGT].rearrange("(c p) t -> p c t", p=128), o_t[:], [o_t], [T_sendR[l]])
            fw.barrier()

    if test in ("loop1", "loop2"):
        stop_after = int(test[-1])
        token_loop(stop_after)
        return nc, fw
    if test == "attn":
        attention(0)
        return nc, fw
    if test == "gla":
        gla(1)
        return nc, fw
    if stop_after is not None and stop_after < 0:
        with ExitStack() as es:
            ht = fw.tile(es, "pt_h", [128, 8, TT], F32)
            w16 = fw.tile(es, "pt_w", [128, 2048], BF16)
            fw.dma("sp", w16[:], waA.ap()[175 * 128:176 * 128, :], [T_waA], [w16])
            for tt in range(NTT):
                fw.dma("pool", ht[:], xT_d.ap()[:, tt * TT:(tt + 1) * TT].rearrange("(kc p) t -> p kc t", p=128), [], [ht])
                if tt == 0:
                    fw.op("dve", [w16, ht], [ht], lambda e: e.tensor_copy(ht[:, 0, :], w16[:, 0:TT]))
                fw.dma("pool", out_d.ap()[:, tt * TT:(tt + 1) * TT].rearrange("(kc p) t -> p kc t", p=128), ht[:], [ht], [T_out])
        fw.barrier()
        return nc, fw
    nloops = 5 if stop_after is None else stop_after + 1
    for k in range(nloops):
        token_loop(k)
        if k < 4 and not (stop_after is not None and k == stop_after):
            l = k
            allgather(sendF[l], gathF[l], T_sendF[l], T_gathF[l])
            fetch_blocks(gathF[l], T_gathF[l], locF[l], T_locF[l], BE if l % 2 == 0 else BG)
            fw.barrier()
            if l % 2 == 0:
                attention(l)
            else:
                gla(l)
            allgather(sendR[l], gathR[l], T_sendR[l], T_gathR[l])
            fetch_blocks(gathR[l], T_gathR[l], locR[l], T_locR[l], 256 if l % 2 == 0 else 512)
            fw.barrier()
    fw.barrier()
    return nc, fw


def _consts(norm_g, gla_w_in, gla_w_gate, gla_b_gate, gla_norm_g, conv_dw_w, conv_dw_b, conv_ln_g, conv_ln_b, j):
    c = np.zeros((128, NCONST), np.float32)
    ii = np.arange(128)
    c[:, C_ID:C_ID + 128] = np.eye(128, dtype=np.float32)
    c[:, C_ONES:C_ONES + 128] = 1.0
    c[:, C_NEGU:C_NEGU + 128] = -(ii[:, None] >= ii[None, :]).astype(np.float32)
    c[:, C_TRIN:C_TRIN + 128] = (ii[:, None] <= ii[None, :]).astype(np.float32) * (-1.0 / 16.0)
    c[:, C_GMASK:C_GMASK + 128] = (ii[:, None] <= ii[None, :]).astype(np.float32)
    qq = np.arange(512)
    for d in range(4):
        c[:, C_AMASK + d * 512:C_AMASK + (d + 1) * 512] = ((d * 128 + ii[:, None]) < qq[None, :]).astype(np.float32)
    for o in range(2):
        c[:, C_WGLR + o * 128:C_WGLR + (o + 1) * 128] = \
            gla_w_in[o][:, 3072:3088].reshape(8, 128, 16).transpose(1, 0, 2).reshape(128, 128)
        c[0:16, C_WGATE + o * 512:C_WGATE + (o + 1) * 512] = gla_w_gate[o]
        c[16, C_WGATE + o * 512:C_WGATE + (o + 1) * 512] = gla_b_gate[o]
        c[:, C_GLANG + o * 2:C_GLANG + o * 2 + 2] = gla_norm_g[o].reshape(2, 128).T
    c[:, C_NORMG:C_NORMG + 192] = norm_g.reshape(4, 6, 8, 128).transpose(3, 0, 1, 2).reshape(128, 192)
    for e in range(2):
        c[:, C_CONVW + e * 124:C_CONVW + (e + 1) * 124] = conv_dw_w[e].reshape(31, 4, 128).transpose(2, 1, 0).reshape(128, 124)
        for wi, arr in enumerate((conv_dw_b, conv_ln_g, conv_ln_b)):
            c[:, C_CONVP + e * 12 + wi * 4:C_CONVP + e * 12 + wi * 4 + 4] = arr[e].reshape(4, 128).T
    c[:, C_FLAG] = float(j)
    return c


def _weight_units(ffn1_w_in, ffn1_w_out, ffn2_w_in, ffn2_w_out, hyb_w_in, hyb_w_out, gla_w_in, gla_w_out):
    A = np.empty((176, 128, 2048), np.float32)
    B = np.empty((64, 128, 2816), np.float32)
    for l in range(4):
        for which, (wi, wo) in enumerate(((ffn1_w_in, ffn1_w_out), (ffn2_w_in, ffn2_w_out))):
            u = (l * 2 + which)
            A[u * 22:(u + 1) * 22] = wi[l].reshape(8, 128, 2, 22, 128).transpose(3, 1, 0, 2, 4).reshape(22, 128, 2048)
            B[u * 8:(u + 1) * 8] = wo[l].reshape(22, 128, 8, 128).transpose(2, 1, 0, 3).reshape(8, 128, 2816)
    C = np.empty((120, 128, 1024), np.float32)

    def units(w, nf):
        return w.reshape(8, 128, nf, 128).transpose(2, 1, 0, 3).reshape(nf, 128, 1024)
    for e in range(2):
        b0 = e * 28
        C[b0:b0 + 20] = units(hyb_w_in[e], 20)
        C[b0 + 20:b0 + 28] = units(hyb_w_out[e], 8)
    for o in range(2):
        b0 = 56 + o * 32
        C[b0:b0 + 24] = units(np.ascontiguousarray(gla_w_in[o][:, :3072]), 24)
        C[b0 + 24:b0 + 32] = units(gla_w_out[o], 8)
    return A, B, C


_CACHE = {}


def _run(inputs, stop_after=None):
    x = np.asarray(inputs["x"], np.float32)
    g = {k: np.asarray(v, np.float32) for k, v in inputs.items() if k != "x"}
    A, B, C = _weight_units(g["ffn1_w_in"], g["ffn1_w_out"], g["ffn2_w_in"], g["ffn2_w_out"],
                            g["hyb_w_in"], g["hyb_w_out"], g["gla_w_in"], g["gla_w_out"])
    in_maps = []
    for c in range(NCORES):
        b, j = c // 2, c % 2
        in_maps.append({
            "xT": np.ascontiguousarray(x[b, j * TOK:(j + 1) * TOK, :].T),
            "wA": np.ascontiguousarray(A[c * 22:(c + 1) * 22].reshape(22 * 128, 2048)),
            "wB": np.ascontiguousarray(B[c * 8:(c + 1) * 8].reshape(8 * 128, 2816)),
            "wC": np.ascontiguousarray(C[c * 15:(c + 1) * 15].reshape(15 * 128, 1024)),
            "consts": _consts(g["norm_g"], g["gla_w_in"], g["gla_w_gate"], g["gla_b_gate"], g["gla_norm_g"],
                              g["conv_dw_w"], g["conv_dw_b"], g["conv_ln_g"], g["conv_ln_b"], j),
        })
    key = stop_after
    if key not in _CACHE:
        _CACHE[key] = build_program(stop_after)[0]
    nc = _CACHE[key]
    res = run_bass_kernel_spmd(nc, in_maps, core_ids=list(range(NCORES)))
    out = np.empty((4, SEQ, D), np.float32)
    for c in range(NCORES):
        b, j = c // 2, c % 2
        out[b, j * TOK:(j + 1) * TOK, :] = res.results[c]["outT"].T
    return out


def kernel(**inputs):
    return _run(inputs)
```

```python
import os
import math
import numpy as np
from contextlib import ExitStack
import concourse.bass as bass
import concourse.mybir as mybir
from concourse.bass_utils import run_bass_kernel_spmd

F32 = mybir.dt.float32
BF16 = mybir.dt.bfloat16
AF = mybir.ActivationFunctionType
ALU = mybir.AluOpType

D = 1024
DFF = 2816
NFC = 22
TOK = 4096
TT = 512
NTT = TOK // TT
SEQ = 8192
EPS = 1e-6
NCORES = 8
ATT_NH, ATT_NG, GLA_NSEG = 4, 16, 8

C_ID, C_ONES, C_NEGU, C_TRIN, C_GMASK = 0, 128, 256, 384, 512
C_AMASK = 640
C_WGLR = C_AMASK + 2048
C_WGATE = C_WGLR + 256
NBF = C_WGATE + 1024
C_NORMG = NBF
C_GLANG = C_NORMG + 192
C_CONVW = C_GLANG + 4
C_CONVP = C_CONVW + 248
C_FLAG = C_CONVP + 24
NCONST = C_FLAG + 1
NF32 = NCONST - NBF

RE = 1544
BE = 772
RG = 2560
BG = 1280


class T:
    __slots__ = ("base", "w", "r", "name", "multi")

    def __init__(self, base, name, multi=False):
        self.base = base
        self.w = None
        self.r = {}
        self.name = name
        self.multi = multi

    def __getitem__(self, idx):
        return self.base[idx]


class Eng:
    def __init__(self, name, h, sem):
        self.name = name
        self.h = h
        self.sem = sem
        self.count = 0
        self.known = {}
        self.pend_r = []
        self.pend_w = []


class FW:
    def __init__(self, nc):
        self.nc = nc
        self.es = ExitStack()
        self.eng = {}
        for name, h in (("pe", nc.tensor), ("act", nc.scalar), ("dve", nc.vector),
                        ("pool", nc.gpsimd), ("sp", nc.sync)):
            sem = self.es.enter_context(nc.semaphore("s_" + name))
            self.eng[name] = Eng(name, h, sem)
        self.dq = {}
        for q in ("sp", "pool"):
            sems = [self.es.enter_context(nc.semaphore(f"dq_{q}_{i}")) for i in range(8)]
            self.dq[q] = {"sems": sems, "n": 0}
        self.nwaits = 0
        self.nins = 0
        self._uid = 0
        self.pending = set()

    def sb(self, es, name, shape, dtype=F32):
        self._uid += 1
        t = es.enter_context(self.nc.sbuf_tensor(f"{name}_{self._uid}", list(shape), dtype))
        return t

    def ps(self, es, name, shape, dtype=F32):
        self._uid += 1
        t = es.enter_context(self.nc.psum_tensor(f"{name}_{self._uid}", list(shape), dtype))
        return t

    def tile(self, es, name, shape, dtype=F32):
        return T(self.sb(es, name, shape, dtype), name)

    def tiles(self, es, name, n, shape, dtype=F32):
        t = self.sb(es, name, [shape[0], n] + list(shape[1:]), dtype)
        return t, [T(t[:, i], f"{name}{i}") for i in range(n)]

    def ptile(self, es, name, shape, dtype=F32):
        return T(self.ps(es, name, shape, dtype), name)

    def sem(self, name):
        return self.es.enter_context(self.nc.semaphore(name))

    def _wait(self, E, ev):
        sem, val = ev
        k = id(sem)
        if E.known.get(k, 0) >= val:
            return
        E.h.wait_ge(sem, val)
        E.known[k] = val
        self.nwaits += 1

    def _deps(self, E, reads, writes, skip_self=False):
        for b in list(reads) + list(writes):
            assert id(b) not in self.pending or b in E.pend_r or b in E.pend_w, \
                f"access to {b.name} with pending (un-incremented) accesses"
        for b in reads:
            if b.multi:
                for ev in b.r.values():
                    self._wait(E, ev)
                continue
            if b.w is not None and not (skip_self and b.w[0] is E.sem):
                self._wait(E, b.w)
        for b in writes:
            if b.multi:
                continue
            if b.w is not None and not (skip_self and b.w[0] is E.sem):
                self._wait(E, b.w)
            for ev in b.r.values():
                if not (skip_self and ev[0] is E.sem):
                    self._wait(E, ev)

    def _mark(self, ev, reads, writes):
        k = id(ev[0])
        for b in reads:
            if b.multi:
                continue
            b.r[k] = ev
        for b in writes:
            if b.multi:
                b.r[k] = ev
            else:
                b.w = ev
                b.r = {}

    def op(self, eng, reads, writes, fn, inc=True):
        E = self.eng[eng]
        self._deps(E, reads, writes, skip_self=(eng == "pe"))
        ins = fn(E.h)
        self.nins += 1
        if inc:
            E.count += 1
            ins.then_inc(E.sem, 1)
            ev = (E.sem, E.count)
            self._mark(ev, E.pend_r + list(reads), E.pend_w + list(writes))
            for b in E.pend_r + E.pend_w:
                self.pending.discard(id(b))
            E.pend_r = []
            E.pend_w = []
        else:
            E.pend_r += list(reads)
            E.pend_w += list(writes)
            for b in list(reads) + list(writes):
                self.pending.add(id(b))
        return ins

    def dma(self, q, out_ap, in_ap, reads, writes, **kw):
        E = self.eng[q]
        Dq = self.dq[q]
        i = Dq["n"]
        Dq["n"] += 1
        sem = Dq["sems"][i % 8]
        prev = 16 * (i // 8)
        if prev > 0:
            self._wait(E, (sem, prev))
        self._deps(E, reads, writes)
        ins = E.h.dma_start(out=out_ap, in_=in_ap, **kw)
        ins.then_inc(sem, 16)
        self.nins += 1
        ev = (sem, prev + 16)
        self._mark(ev, reads, writes)
        return ev

    def wait_all_dma(self, q):
        E = self.eng[q]
        Dq = self.dq[q]
        n = Dq["n"]
        for s in range(8):
            cnt = (n - s + 7) // 8
            if cnt > 0:
                self._wait(E, (Dq["sems"][s], 16 * cnt))

    def collective(self, sem, in_ap, out_ap, reads, writes):
        E = self.eng["pool"]
        self._deps(E, reads, writes)
        ins = E.h.collective_compute("AllGather", ALU.bypass,
                                     replica_groups=[list(range(NCORES))],
                                     ins=[in_ap], outs=[out_ap])
        ins.then_inc(sem)
        self.nins += 1
        self._mark((sem, 1), reads, writes)

    def barrier(self):
        for q in ("sp", "pool"):
            self.wait_all_dma(q)
        evs = []
        for name, E in self.eng.items():
            assert not E.pend_r and not E.pend_w
            if E.count > 0:
                self._wait(E, (E.sem, E.count))
            E.count += 1
            E.h.sem_inc(E.sem, 1)
            evs.append((E.sem, E.count))
        for name, E in self.eng.items():
            for ev in evs:
                self._wait(E, ev)


def build_program(stop_after=None, sim=False, test=None):
    nc = bass.Bass("TRN2", target_bir_lowering=False)
    fw = FW(nc)
    ges = fw.es

    if test in ("attn", "gla"):
        xT_d = nc.dram_tensor("xT", [D, TOK], F32)
        wA_d = nc.dram_tensor("wA", [22 * 128, 2048], F32)
        wB_d = nc.dram_tensor("wB", [8 * 128, 2816], F32)
        wC_d = nc.dram_tensor("wC", [15 * 128, 1024], F32)
    else:
        xT_d = nc.dram_tensor("xT", [D, TOK], F32, kind="ExternalInput")
        wA_d = nc.dram_tensor("wA", [22 * 128, 2048], F32, kind="ExternalInput")
        wB_d = nc.dram_tensor("wB", [8 * 128, 2816], F32, kind="ExternalInput")
        wC_d = nc.dram_tensor("wC", [15 * 128, 1024], F32, kind="ExternalInput")
    cst_d = nc.dram_tensor("consts", [128, NCONST], F32, kind="ExternalInput")
    out_d = nc.dram_tensor("outT", [D, TOK], F32, kind="ExternalOutput") if test not in ("attn", "gla") else nc.dram_tensor("outT", [D, TOK], F32)

    ext = {}
    if test == "attn":
        ext = {"locF0_0": "ExternalInput", "locF0_1": "ExternalInput", "sendR0": "ExternalOutput"}
    if test == "gla":
        ext = {"locF1_0": "ExternalInput", "locF1_1": "ExternalInput", "sendR1": "ExternalOutput"}
    if test == "loop1":
        ext = {"hT": "ExternalInput", "locR0_0": "ExternalInput", "locR0_1": "ExternalInput", "cdr0": "ExternalInput",
               "locF0_0": "ExternalInput", "sendF1": "ExternalOutput", "rdr1": "ExternalOutput"}
    if test == "loop2":
        ext = {"hT": "ExternalInput", "locR1_0": "ExternalInput", "locR1_1": "ExternalInput", "rdr1": "ExternalInput",
               "sendF0": "ExternalOutput", "cdr0": "ExternalOutput"}

    def idram(name, shape, dt):
        if name in ext:
            return nc.dram_tensor(name, list(shape), dt, kind=ext[name])
        return nc.dram_tensor(name, list(shape), dt)

    wsA = idram("wsA", [22 * 128, 2048], BF16); wsB = idram("wsB", [8 * 128, 2816], BF16); wsC = idram("wsC", [15 * 128, 1024], BF16)
    if sim and test not in ("attn", "gla"):
        waA = nc.dram_tensor("waA", [176 * 128, 2048], BF16, kind="ExternalInput")
        waB = nc.dram_tensor("waB", [64 * 128, 2816], BF16, kind="ExternalInput")
        waC = nc.dram_tensor("waC", [120 * 128, 1024], BF16, kind="ExternalInput")
    else:
        waA = idram("waA", [176 * 128, 2048], BF16)
        waB = idram("waB", [64 * 128, 2816], BF16)
        waC = idram("waC", [120 * 128, 1024], BF16)
    hT_d = idram("hT", [D, TOK], F32)
    sendF = {}; gathF = {}; sendR = {}; gathR = {}; cdr = {}; rdr = {}
    for l in range(2):
        if l % 2 == 0:
            sendF[l] = idram(f"sendF{l}", [RE, TOK], BF16); gathF[l] = idram(f"gathF{l}", [8 * RE, TOK], BF16)
            sendR[l] = idram(f"sendR{l}", [512, TOK], BF16); gathR[l] = idram(f"gathR{l}", [8 * 512, TOK], BF16)
            cdr[l] = idram(f"cdr{l}", [512, TOK], BF16)
        else:
            sendF[l] = idram(f"sendF{l}", [RG, TOK], BF16); gathF[l] = idram(f"gathF{l}", [8 * RG, TOK], BF16)
            sendR[l] = idram(f"sendR{l}", [1024, TOK], BF16); gathR[l] = idram(f"gathR{l}", [8 * 1024, TOK], BF16)
            rdr[l] = idram(f"rdr{l}", [D, TOK], F32)
    for l in (2, 3):
        sendF[l] = sendF[l - 2]; gathF[l] = gathF[l - 2]; sendR[l] = sendR[l - 2]; gathR[l] = gathR[l - 2]
        if l == 2: cdr[l] = cdr[0]
        else: rdr[l] = rdr[1]
    T_wsA = T(wsA, "wsA", multi=True); T_waA = T(waA, "waA")
    T_wsB = T(wsB, "wsB", multi=True); T_waB = T(waB, "waB")
    T_wsC = T(wsC, "wsC", multi=True); T_waC = T(waC, "waC")
    T_h = [T(hT_d, f"hT{i}") for i in range(NTT)]
    T_sendF = {l: T(sendF[l], f"sendF{l}", multi=True) for l in range(2)}
    T_gathF = {l: T(gathF[l], f"gathF{l}") for l in range(2)}
    T_sendR = {l: T(sendR[l], f"sendR{l}", multi=True) for l in range(2)}
    T_gathR = {l: T(gathR[l], f"gathR{l}") for l in range(2)}
    T_cdr = {0: [T(cdr[0], f"cdr_{i}") for i in range(NTT)]}
    T_rdr = {1: [T(rdr[1], f"rdr_{i}") for i in range(NTT)]}
    for l in (2, 3):
        T_sendF[l] = T_sendF[l - 2]; T_gathF[l] = T_gathF[l - 2]; T_sendR[l] = T_sendR[l - 2]; T_gathR[l] = T_gathR[l - 2]
    T_cdr[2] = T_cdr[0]; T_rdr[3] = T_rdr[1]
    T_out = T(out_d, "out", multi=True)
    cc_sems = [fw.sem(f"cc{i}") for i in range(11)]
    cc_i = [0]

    def allgather(send, gath, Ts, Tg):
        fw.collective(cc_sems[cc_i[0]], send.ap().opt(), gath.ap().opt(), [Ts], [Tg])
        cc_i[0] += 1

    pid = nc.partition_id([mybir.EngineType.Pool])
    B0 = nc.gpsimd.snap(pid + (pid // 2) * 2)
    locF = {}; locR = {}; T_locF = {}; T_locR = {}
    for l in range(2):
        rf, rr = (BE, 256) if l % 2 == 0 else (BG, 512)
        locF[l] = [idram(f"locF{l}_{s_}", [rf, TOK], BF16) for s_ in range(2)]
        locR[l] = [idram(f"locR{l}_{s_}", [rr, TOK], BF16) for s_ in range(2)]
        T_locF[l] = [T(locF[l][s_], f"locF{l}_{s_}") for s_ in range(2)]
        T_locR[l] = [T(locR[l][s_], f"locR{l}_{s_}") for s_ in range(2)]
    for l in (2, 3):
        locF[l] = locF[l - 2]; locR[l] = locR[l - 2]; T_locF[l] = T_locF[l - 2]; T_locR[l] = T_locR[l - 2]

    def fetch_blocks(gath, Tg, loc, Tloc, rows):
        v3 = gath.ap().rearrange("(b r) t -> b r t", r=rows)
        for s_ in range(2):
            fw.dma("pool", loc[s_].ap(), v3[2 * s_:, :, :][bass.ds(B0, 1), :, :].rearrange("o r t -> (o r) t"),
                   [Tg], [Tloc[s_]])

    cb_t = fw.sb(ges, "cb", [128, NBF], BF16); cb = T(cb_t, "cb")
    cf_t = fw.sb(ges, "cf", [128, NF32], F32); cf = T(cf_t, "cf")
    glr_aug = fw.tile(ges, "glr_aug", [32, TT], BF16)

    def cfc(col, n=1):
        return cf[:, col - NBF: col - NBF + n]

    with ExitStack() as es:
        ctmp = fw.tile(es, "ctmp", [128, NCONST], F32)
        fw.dma("sp", ctmp[:], cst_d.ap(), [], [ctmp])
        fw.op("dve", [ctmp], [cb], lambda e: e.tensor_copy(cb[:, 0:2048], ctmp[:, 0:2048]))
        fw.op("pool", [ctmp], [cb], lambda e: e.tensor_copy(cb[:, 2048:NBF], ctmp[:, 2048:NBF]))
        fw.op("act", [ctmp], [cf], lambda e: e.activation(out=cf[:], in_=ctmp[:, NBF:NCONST], func=AF.Copy))
        fw.op("pool", [], [glr_aug], lambda e: e.memset(glr_aug[:], 1.0))
        wi = 0
        for (src, dst, Tdst, n, L) in (() if sim else ((wA_d, wsA, T_wsA, 22, 2048), (wB_d, wsB, T_wsB, 8, 2816),
                                       (wC_d, wsC, T_wsC, 15, 1024))):
            st32 = [fw.tile(es, f"wst32_{L}_{i}", [128, L], F32) for i in range(2)]
            st16 = [fw.tile(es, f"wst16_{L}_{i}", [128, L], BF16) for i in range(2)]
            for u in range(n):
                a, b_ = st32[u % 2], st16[u % 2]
                fw.dma("sp", a[:], src.ap()[u * 128:(u + 1) * 128, :], [], [a])
                eng = ("dve", "pool", "act")[wi % 3]; wi += 1
                if eng == "act":
                    fw.op("act", [a], [b_], lambda e, a=a, b_=b_: e.activation(out=b_[:], in_=a[:], func=AF.Copy))
                else:
                    fw.op(eng, [a], [b_], lambda e, a=a, b_=b_: e.tensor_copy(b_[:], a[:]))
                fw.dma("pool", dst.ap()[u * 128:(u + 1) * 128, :], b_[:], [b_], [Tdst])
        if not sim:
            allgather(wsA, waA, T_wsA, T_waA)
            allgather(wsB, waB, T_wsB, T_waB)
            allgather(wsC, waC, T_wsC, T_waC)
        fw.barrier()

    cb_id = lambda: cb[:, C_ID:C_ID + 128]
    cb_ones = lambda: cb[:, C_ONES:C_ONES + 128]

    def uA(l, which, fc): return (l * 2 + which) * 22 + fc
    def uB(l, which, dc): return (l * 2 + which) * 8 + dc
    UC_BASE = {0: 0, 2: 28, 1: 56, 3: 88}

    def token_loop(k):
        lb = k - 1 if k > 0 else None
        la = k if k < 4 else None
        with ExitStack() as es:
            P = [fw.ptile(es, f"P{i}", [128, TT], F32) for i in range(8)]
            h_t, h = fw.tiles(es, "h", 8, [128, TT], F32)
            xn_t, xn = fw.tiles(es, "xn", 8, [128, TT], BF16)
            sq_t, sq = fw.tiles(es, "sq", 8, [128, TT], BF16)
            f_t, f = fw.tiles(es, "f", 8, [128, TT], F32)
            hm_t, hm = fw.tiles(es, "hm", NFC, [128, TT], BF16)
            cat_t, cat = fw.tiles(es, "cat", 8, [128, TT], BF16)
            lnv = fw.tile(es, "lnv", [128, TT], F32)
            rstd = fw.tile(es, "rstd", [128, TT], F32)
            sa = [fw.tile(es, f"sa{i}", [128, TT], F32) for i in range(2)]
            tmp = [fw.tile(es, f"tmp{i}", [128, TT], F32) for i in range(3)]
            stg = [fw.tile(es, f"stg{i}", [128, TT], BF16) for i in range(3)]
            stg32 = [fw.tile(es, f"stg32_{i}", [128, TT], F32) for i in range(2)]
            NSA, NSB, NSC = 3, 2, 6
            slotA = [fw.tile(es, f"slA{i}", [128, 2048], BF16) for i in range(NSA)]
            slotB = [fw.tile(es, f"slB{i}", [128, 2816], BF16) for i in range(NSB)]
            slotC = [fw.tile(es, f"slC{i}", [128, 1024], BF16) for i in range(NSC)]
            cnt = {"A": 0, "B": 0, "C": 0, "tmp": 0, "stg": 0, "sa": 0, "stg32": 0}
            even_b = lb is not None and lb % 2 == 0
            odd_b = lb is not None and lb % 2 == 1
            if even_b:
                eb_ = lb // 2
                cbuf_t, cbuf = fw.tiles(es, "cbuf", 4, [128, 32 + TT], BF16)
                dg = fw.tile(es, "dg", [128, 124 * 128], BF16)
                ysb_t, ysb = fw.tiles(es, "ysb", 4, [128, TT], F32)
                ybf_t, ybf = fw.tiles(es, "ybf", 4, [128, TT], BF16)
                mean = fw.tile(es, "mean", [128, TT], F32)
                for i in range(124):
                    col = C_CONVW + eb_ * 124 + i
                    fw.op("pool" if i % 2 else "dve", [cb, cf], [dg],
                          lambda e, i=i, col=col: e.tensor_scalar(dg[:, i * 128:(i + 1) * 128], cb_id(),
                                                                   cfc(col), None, op0=ALU.mult))
            if odd_b:
                ob_t, ob = fw.tiles(es, "ob", 8, [128, TT], BF16)

            def nxt(key, lst):
                i = cnt[key]; cnt[key] += 1
                return lst[i % len(lst)]

            def wload(kind, unit):
                if kind == "A":
                    s = nxt("A", slotA); src = waA.ap()[unit * 128:(unit + 1) * 128, :]; Tsrc = T_waA
                elif kind == "B":
                    s = nxt("B", slotB); src = waB.ap()[unit * 128:(unit + 1) * 128, :]; Tsrc = T_waB
                else:
                    s = nxt("C", slotC); src = waC.ap()[unit * 128:(unit + 1) * 128, :]; Tsrc = T_waC
                fw.dma("sp", s[:], src, [Tsrc], [s])
                return s

            plan = []
            def plan_ffn(l, which):
                for fc in range(NFC): plan.append(("A", uA(l, which, fc)))
                for dc in range(8): plan.append(("B", uB(l, which, dc)))
            if even_b:
                for dc in range(8): plan.append(("C", UC_BASE[lb] + 20 + dc))
            if odd_b:
                for dc in range(8): plan.append(("C", UC_BASE[lb] + 24 + dc))
            if lb is not None: plan_ffn(lb, 1)
            if la is not None:
                plan_ffn(la, 0)
                if la % 2 == 0:
                    order = list(range(12)) + [12, 16, 13, 17, 14, 18, 15, 19]
                else:
                    order = list(range(24))
                for u in order: plan.append(("C", UC_BASE[la] + u))
            full = plan * NTT
            loaded = []
            ptr = {"issue": 0, "use": 0}
            NS = {"A": NSA, "B": NSB, "C": NSC}

            def wget(kind):
                i = ptr["use"]
                assert full[i][0] == kind, (full[i], kind, i)
                lo = max(i - 1, 0)
                while ptr["issue"] < len(full) and ptr["issue"] - i <= 12:
                    kd, un = full[ptr["issue"]]
                    infl = sum(1 for jj in range(lo, ptr["issue"]) if full[jj][0] == kd)
                    if infl >= NS[kd]:
                        assert ptr["issue"] > i
                        break
                    loaded.append(wload(kd, un))
                    ptr["issue"] += 1
                ptr["use"] += 1
                return loaded[i]

            def gcol(l, i):
                return C_NORMG + (l * 6 + i) * 8

            def rstd_from(psT, n, extra_bias=0.0):
                fw.op("act", [psT], [lnv], lambda e: e.activation(out=lnv[:], in_=psT[:], func=AF.Ln,
                                                                   scale=1.0 / n, bias=EPS))
                fw.op("act", [lnv], [rstd], lambda e: e.activation(out=rstd[:], in_=lnv[:], func=AF.Exp,
                                                                    scale=-0.5, bias=extra_bias))

            def sumsq_mm(ps, srcs):
                n = len(srcs)
                for i, s in enumerate(srcs):
                    fw.op("pe", [cb, s], [ps], lambda e, s=s, i=i: e.matmul(ps[:], cb_ones(), s[:], start=(i == 0),
                                                                           stop=(i == n - 1)), inc=(i == n - 1))

            def prenorm(l, gi):
                for kc in range(8):
                    fw.op("act", [h[kc]], [sq[kc]], lambda e, kc=kc: e.activation(out=sq[kc][:], in_=h[kc][:], func=AF.Square))
                sumsq_mm(P[6], sq)
                rstd_from(P[6], D)
                gc = gcol(l, gi)
                for kc in range(8):
                    fw.op("dve", [h[kc], cf, rstd], [xn[kc]],
                          lambda e, kc=kc: e.scalar_tensor_tensor(out=xn[kc][:], in0=h[kc][:], scalar=cfc(gc + kc),
                                                                  in1=rstd[:], op0=ALU.mult, op1=ALU.mult))

            def post_residual(l, gi, res_scale):
                sumsq_mm(P[6], sq)
                rstd_from(P[6], D, extra_bias=math.log(res_scale))
                gc = gcol(l, gi)
                for dc in range(8):
                    tp = nxt("tmp", tmp)
                    fw.op("dve", [f[dc], cf, rstd], [tp],
                          lambda e, dc=dc, tp=tp: e.scalar_tensor_tensor(out=tp[:], in0=f[dc][:], scalar=cfc(gc + dc),
                                                                         in1=rstd[:], op0=ALU.mult, op1=ALU.mult))
                    fw.op("pool", [tp, h[dc]], [h[dc]],
                          lambda e, dc=dc, tp=tp: e.tensor_tensor(h[dc][:], tp[:], h[dc][:], op=ALU.add))

            def evac_f(ps, dc):
                fw.op("dve", [ps], [f[dc]], lambda e: e.tensor_copy(f[dc][:], ps[:]))
                fw.op("act", [f[dc]], [sq[dc]], lambda e: e.activation(out=sq[dc][:], in_=f[dc][:], func=AF.Square))

            def ffn(l, which):
                prenorm(l, 0 if which == 0 else 4)
                for fc in range(NFC):
                    wu = wget("A")
                    pa, pb = (P[0], P[1]) if fc % 2 == 0 else (P[2], P[3])
                    for half, ps in ((0, pa), (1, pb)):
                        for kc in range(8):
                            off = kc * 256 + half * 128
                            fw.op("pe", [wu, xn[kc]], [ps],
                                  lambda e, ps=ps, wu=wu, kc=kc, off=off: e.matmul(ps[:], wu[:, off:off + 128], xn[kc][:],
                                                                                  start=(kc == 0), stop=(kc == 7)),
                                  inc=(kc == 7))
                    s_ = nxt("sa", sa)
                    fw.op("act", [pa], [s_], lambda e, s_=s_, pa=pa: e.activation(out=s_[:], in_=pa[:], func=AF.Silu))
                    fw.op("dve", [s_, pb], [hm[fc]], lambda e, s_=s_, pb=pb, fc=fc: e.tensor_tensor(hm[fc][:], s_[:], pb[:], op=ALU.mult))
                for dc in range(8):
                    wo = wget("B")
                    ps = P[4 + dc % 2]
                    for fc in range(NFC):
                        fw.op("pe", [wo, hm[fc]], [ps],
                              lambda e, ps=ps, wo=wo, fc=fc: e.matmul(ps[:], wo[:, fc * 128:(fc + 1) * 128], hm[fc][:],
                                                                      start=(fc == 0), stop=(fc == NFC - 1)),
                              inc=(fc == NFC - 1))
                    evac_f(ps, dc)
                post_residual(l, 1 if which == 0 else 5, 0.5)

            def proj_fm(wu, ps, src=None):
                src = src or xn
                for kc in range(8):
                    fw.op("pe", [wu, src[kc]], [ps],
                          lambda e, kc=kc: e.matmul(ps[:], wu[:, kc * 128:(kc + 1) * 128], src[kc][:],
                                                    start=(kc == 0), stop=(kc == 7)), inc=(kc == 7))

            def proj_tm(nun, pss):
                for i in range(nun):
                    wu = wget("C")
                    for tb in range(4):
                        for kc in range(8):
                            fw.op("pe", [wu, xn[kc]], [pss[tb]],
                                  lambda e, i=i, wu=wu, tb=tb, kc=kc: e.matmul(
                                      pss[tb][:, i * 128:(i + 1) * 128], xn[kc][:, tb * 128:(tb + 1) * 128],
                                      wu[:, kc * 128:(kc + 1) * 128], start=(kc == 0), stop=(kc == 7)),
                                  inc=(kc == 7 and tb == 3))

            def out_proj_and_residual(l):
                for dc in range(8):
                    wu = wget("C")
                    ps = P[4 + dc % 2]
                    proj_fm(wu, ps, src=cat)
                    evac_f(ps, dc)
                post_residual(l, 3, 1.0)

            for tt in range(NTT):
                t0 = tt * TT
                src = hT_d if k > 0 else xT_d
                fw.dma("pool", h_t[:], src.ap()[:, t0:t0 + TT].rearrange("(kc p) t -> p kc t", p=128),
                       [T_h[tt]] if k > 0 else [], h)
                if even_b:
                    l = lb
                    for kc in range(4):
                        fw.dma("pool", cat[kc][:], locR[l][kc // 2].ap()[(kc % 2) * 128:(kc % 2 + 1) * 128, t0:t0 + TT],
                               [T_locR[l][kc // 2]], [cat[kc]])
                    fw.dma("pool", cbuf_t[:, :, 32:32 + TT],
                           cdr[l].ap()[:, t0:t0 + TT].rearrange("(cc p) t -> p cc t", p=128), [T_cdr[l][tt]], cbuf)
                    if tt > 0:
                        fw.dma("pool", cbuf_t[:, :, 0:32],
                               cdr[l].ap()[:, t0 - 32:t0].rearrange("(cc p) t -> p cc t", p=128), [T_cdr[l][tt - 1]], cbuf)
                    else:
                        fw.dma("pool", cbuf_t[:, :, 0:32],
                               locF[l][0].ap()[768:772, :].rearrange("cc (p i) -> p cc i", i=32),
                               [T_locF[l][0]], cbuf)
                        fw.op("pool", cbuf + [cf], cbuf,
                              lambda e: e.tensor_scalar(cbuf_t[:, :, 0:32], cbuf_t[:, :, 0:32], cfc(C_FLAG), None, op0=ALU.mult))
                    pc = C_CONVP + eb_ * 12
                    for cc in range(4):
                        ps = P[cc]
                        for kk in range(31):
                            fw.op("pe", [dg, cbuf[cc]], [ps],
                                  lambda e, ps=ps, cc=cc, kk=kk: e.matmul(ps[:], dg[:, (cc * 31 + kk) * 128:(cc * 31 + kk + 1) * 128],
                                                                          cbuf[cc][:, 2 + kk:2 + kk + TT], start=(kk == 0), stop=(kk == 30)),
                                  inc=(kk == 30))
                        fw.op("act", [ps, cf], [ysb[cc]], lambda e, ps=ps, cc=cc: e.activation(out=ysb[cc][:], in_=ps[:], func=AF.Identity,
                                                                                             bias=cfc(pc + cc)))
                        fw.op("act", [ysb[cc]], [sq[cc]], lambda e, cc=cc: e.activation(out=sq[cc][:], in_=ysb[cc][:], func=AF.Square))
                        fw.op("pool", [ysb[cc]], [ybf[cc]], lambda e, cc=cc: e.tensor_copy(ybf[cc][:], ysb[cc][:]))
                    sumsq_mm(P[4], ybf)
                    sumsq_mm(P[5], sq[0:4])
                    m2 = nxt("tmp", tmp)
                    var = nxt("tmp", tmp)
                    fw.op("dve", [P[4]], [mean], lambda e: e.tensor_scalar(mean[:], P[4][:], 1.0 / 512, None, op0=ALU.mult))
                    fw.op("dve", [mean], [m2], lambda e: e.tensor_tensor(m2[:], mean[:], mean[:], op=ALU.mult))
                    fw.op("dve", [P[5], m2], [var], lambda e: e.scalar_tensor_tensor(out=var[:], in0=P[5][:], scalar=1.0 / 512, in1=m2[:],
                                                                                   op0=ALU.mult, op1=ALU.subtract))
                    rstd_from(var, 1.0)
                    for cc in range(4):
                        t1 = nxt("tmp", tmp)
                        fw.op("dve", [ysb[cc], mean], [t1], lambda e, cc=cc, t1=t1: e.tensor_tensor(t1[:], ysb[cc][:], mean[:], op=ALU.subtract))
                        fw.op("pool", [t1, rstd], [t1], lambda e, t1=t1: e.tensor_tensor(t1[:], t1[:], rstd[:], op=ALU.mult))
                        fw.op("act", [t1, cf], [cat[4 + cc]],
                              lambda e, cc=cc, t1=t1: e.activation(out=cat[4 + cc][:], in_=t1[:], func=AF.Silu,
                                                                   scale=cfc(pc + 4 + cc), bias=cfc(pc + 8 + cc)))
                    out_proj_and_residual(l)
                if odd_b:
                    l = lb
                    o_ = l // 2
                    for oc in range(8):
                        fw.dma("pool", ob[oc][:], locR[l][oc // 4].ap()[(oc % 4) * 128:(oc % 4 + 1) * 128, t0:t0 + TT],
                               [T_locR[l][oc // 4]], [ob[oc]])
                    for hd in range(4):
                        for cc in range(2):
                            fw.op("act", [ob[2 * hd + cc]], [sq[cc]],
                                  lambda e, hd=hd, cc=cc: e.activation(out=sq[cc][:], in_=ob[2 * hd + cc][:], func=AF.Square))
                        sumsq_mm(P[hd % 2], sq[0:2])
                        rstd_from(P[hd % 2], 256)
                        for cc in range(2):
                            oc = 2 * hd + cc
                            t1 = nxt("tmp", tmp)
                            s32 = nxt("stg32", stg32)
                            fw.dma("pool", s32[:], rdr[l].ap()[oc * 128:(oc + 1) * 128, t0:t0 + TT], [T_rdr[l][tt]], [s32])
                            fw.op("dve", [ob[oc], cf, rstd], [t1],
                                  lambda e, oc=oc, cc=cc, t1=t1: e.scalar_tensor_tensor(out=t1[:], in0=ob[oc][:],
                                                                                        scalar=cfc(C_GLANG + o_ * 2 + cc),
                                                                                        in1=rstd[:], op0=ALU.mult, op1=ALU.mult))
                            fw.op("pool", [t1, s32], [cat[oc]], lambda e, oc=oc, t1=t1, s32=s32: e.tensor_tensor(cat[oc][:], t1[:], s32[:], op=ALU.mult))
                    out_proj_and_residual(l)
                if lb is not None:
                    ffn(lb, 1)
                if la is not None:
                    l = la
                    ffn(l, 0)
                    prenorm(l, 2)
                    sF = sendF[l].ap()
                    if l % 2 == 0:
                        for fc in range(8):
                            wu = wget("C")
                            ps = P[fc % 4]
                            proj_fm(wu, ps)
                            st = nxt("stg", stg)
                            sc_ = 0.125 if fc < 4 else 1.0
                            fw.op("act", [ps], [st], lambda e, ps=ps, st=st, sc_=sc_: e.activation(out=st[:], in_=ps[:], func=AF.Copy, scale=sc_))
                            f4 = fc % 4
                            row0 = (f4 // 2) * BE + (256 if fc >= 4 else 0) + (f4 % 2) * 128
                            fw.dma("pool", sF[row0:row0 + 128, t0:t0 + TT], st[:], [st], [T_sendF[l]])
                        proj_tm(4, P[0:4])
                        for tb in range(4):
                            st = nxt("stg", stg)
                            fw.op("dve" if tb % 2 else "act", [P[tb]], [st],
                                  (lambda e, tb=tb, st=st: e.tensor_copy(st[:], P[tb][:])) if tb % 2 else
                                  (lambda e, tb=tb, st=st: e.activation(out=st[:], in_=P[tb][:], func=AF.Copy)))
                            for hf in range(2):
                                vsec = sF[hf * BE + 512:hf * BE + 768, :].rearrange("r (t16 c) -> (r t16) c", c=256)
                                fw.dma("pool", vsec[t0 + tb * 128:t0 + (tb + 1) * 128, :], st[:, hf * 256:(hf + 1) * 256], [st], [T_sendF[l]])
                        for fc in range(4):
                            pu, pg = (P[4], P[5]) if fc % 2 == 0 else (P[6], P[7])
                            wuu = wget("C")
                            wug = wget("C")
                            proj_fm(wuu, pu)
                            proj_fm(wug, pg)
                            s_ = nxt("sa", sa)
                            st = nxt("stg", stg)
                            fw.op("act", [pg], [s_], lambda e, pg=pg, s_=s_: e.activation(out=s_[:], in_=pg[:], func=AF.Sigmoid))
                            fw.op("dve", [s_, pu], [st], lambda e, pu=pu, s_=s_, st=st: e.tensor_tensor(st[:], s_[:], pu[:], op=ALU.mult))
                            fw.dma("pool", cdr[l].ap()[fc * 128:(fc + 1) * 128, t0:t0 + TT], st[:], [st], [T_cdr[l][tt]])
                            if tt == NTT - 1:
                                for hf in range(2):
                                    fw.dma("pool", sF[hf * BE + 768 + fc:hf * BE + 769 + fc, :].rearrange("o (p i) -> (o p) i", i=32),
                                           st[:, TT - 32:TT], [st], [T_sendF[l]])
                    else:
                        o_ = l // 2
                        for fc in range(8):
                            wu = wget("C")
                            ps = P[fc % 4]
                            proj_fm(wu, ps)
                            st = nxt("stg", stg)
                            fw.op("act", [ps], [st], lambda e, ps=ps, st=st: e.activation(out=st[:], in_=ps[:], func=AF.Copy))
                            f4 = fc % 4
                            row0 = (f4 // 2) * BG + (256 if fc >= 4 else 0) + (f4 % 2) * 128
                            fw.dma("pool", sF[row0:row0 + 128, t0:t0 + TT], st[:], [st], [T_sendF[l]])
                        for hf in range(2):
                            vsec = sF[hf * BG + 512:hf * BG + 1024, :].rearrange("r (t8 c) -> (r t8) c", c=512)
                            pss = P[0:4] if hf == 0 else P[4:8]
                            proj_tm(4, pss)
                            for tb in range(4):
                                st = nxt("stg", stg)
                                fw.op("dve" if tb % 2 else "act", [pss[tb]], [st],
                                      (lambda e, tb=tb, st=st, pss=pss: e.tensor_copy(st[:], pss[tb][:])) if tb % 2 else
                                      (lambda e, tb=tb, st=st, pss=pss: e.activation(out=st[:], in_=pss[tb][:], func=AF.Copy)))
                                fw.dma("pool", vsec[t0 + tb * 128:t0 + (tb + 1) * 128, :], st[:], [st], [T_sendF[l]])
                        for fc in range(8):
                            wu = wget("C")
                            ps = P[fc % 4]
                            proj_fm(wu, ps)
                            s32 = nxt("stg32", stg32)
                            fw.op("act", [ps], [s32], lambda e, ps=ps, s32=s32: e.activation(out=s32[:], in_=ps[:], func=AF.Silu))
                            fw.dma("pool", rdr[l].ap()[fc * 128:(fc + 1) * 128, t0:t0 + TT], s32[:], [s32], [T_rdr[l][tt]])
                        psg = P[4]
                        for kc in range(8):
                            c0 = C_WGLR + o_ * 128 + kc * 16
                            fw.op("pe", [cb, xn[kc]], [psg],
                                  lambda e, kc=kc, c0=c0: e.matmul(psg[0:16, :], cb[:, c0:c0 + 16], xn[kc][:], start=(kc == 0), stop=(kc == 7)),
                                  inc=(kc == 7))
                        fw.op("act", [psg], [glr_aug], lambda e: e.activation(out=glr_aug[0:16, :], in_=psg[0:16, :], func=AF.Copy))
                        for tb in range(4):
                            ps = P[tb]
                            fw.op("pe", [glr_aug, cb], [ps],
                                  lambda e, tb=tb, ps=ps: e.matmul(ps[:], glr_aug[0:17, tb * 128:(tb + 1) * 128],
                                                                   cb[0:17, C_WGATE + o_ * 512:C_WGATE + (o_ + 1) * 512], start=True, stop=True))
                            s_ = nxt("sa", sa)
                            st = nxt("stg", stg)
                            fw.op("act", [ps], [s_], lambda e, ps=ps, s_=s_: e.activation(out=s_[:], in_=ps[:], func=AF.Exp, scale=-1.0))
                            fw.op("act", [s_], [st], lambda e, s_=s_, st=st: e.activation(out=st[:], in_=s_[:], func=AF.Ln, bias=1.0))
                            for hf in range(2):
                                gsec = sF[hf * BG + 1024:hf * BG + 1280, :].rearrange("r (t16 c) -> (r t16) c", c=256)
                                fw.dma("pool", gsec[t0 + tb * 128:t0 + (tb + 1) * 128, :], st[:, hf * 256:(hf + 1) * 256], [st], [T_sendF[l]])
                last = (k == 4) or (stop_after is not None and k == stop_after)
                dstd = out_d if last else hT_d
                fw.dma("pool", dstd.ap()[:, t0:t0 + TT].rearrange("(kc p) t -> p kc t", p=128), h_t[:],
                       h, [T_out] if last else [T_h[tt]])
            assert ptr["use"] == len(full), (ptr, len(full))
            fw.barrier()

    def attention(l):
        with ExitStack() as es:
            P = [fw.ptile(es, f"PA{i}", [128, TT], F32) for i in range(8)]
            QT_t, QT = fw.tiles(es, "QT", 2, [128, SEQ], BF16)
            KT_t, KT = fw.tiles(es, "KT", 2, [128, SEQ], BF16)
            V = fw.tile(es, "V", [128, 64, 256], BF16)
            OT_t, OT = fw.tiles(es, "OT", 2, [128, SEQ], BF16)
            E = [fw.tile(es, f"E{i}", [128, TT], F32) for i in range(2)]
            L1 = [fw.tile(es, f"L1{i}", [128, TT], BF16) for i in range(3)]
            X = [fw.tile(es, f"X{i}", [128, TT], F32) for i in range(2)]
            W = [fw.tile(es, f"W{i}", [128, TT], BF16) for i in range(3)]
            Cc = fw.tile(es, "Cc", [128, TT], F32)
            for s in range(2):
                lf = locF[l][s].ap()
                for hp in range(2):
                    fw.dma("pool", QT[hp][:, s * TOK:(s + 1) * TOK], lf[hp * 128:(hp + 1) * 128, :], [T_locF[l][s]], [QT[hp]])
                    fw.dma("pool", KT[hp][:, s * TOK:(s + 1) * TOK], lf[256 + hp * 128:256 + (hp + 1) * 128, :], [T_locF[l][s]], [KT[hp]])
                fw.dma("pool", V[:, s * 32:(s + 1) * 32, :],
                       lf[512:768, :].rearrange("y (t16 c) -> (y t16) c", c=256).rearrange("(blk p) c -> p blk c", p=128),
                       [T_locF[l][s]], [V])
            cnt = {"E": 0, "L1": 0, "X": 0, "W": 0, "z": 0, "x": 0, "s": 0, "o": 0}
            if ATT_NH < 4 or ATT_NG < 16:
                for hp in range(2):
                    fw.op("pool", [], [OT[hp]], lambda e, hp=hp: e.memset(OT[hp][:], 0.0))

            def nxt(key, lst):
                i = cnt[key]; cnt[key] += 1
                return lst[i % len(lst)]

            pairs = []
            for hl in range(ATT_NH):
                for g in range(ATT_NG):
                    jtop = 4 * g + 3
                    for jb in range(jtop, -1, -1):
                        pairs.append({"hl": hl, "g": g, "jb": jb, "jtop": jtop})
            Zs, Xs, Ss, Os = [P[0], P[1]], [P[2], P[3]], [P[4], P[5]], [P[6], P[7]]
            ob_i = [-1]

            def stA(i):
                p = pairs[i]
                hp, hh = p["hl"] // 2, p["hl"] % 2
                pl, ph = hh * 64, hh * 64 + 64
                k0, q0 = p["jb"] * 128, p["g"] * TT
                Zb = Zs[i % 2]; p["Zb"] = Zb
                fw.op("pe", [KT[hp], QT[hp]], [Zb],
                      lambda e: e.matmul(Zb[:], KT[hp][pl:ph, k0:k0 + 128], QT[hp][pl:ph, q0:q0 + TT], start=True, stop=True))

            def stBC(i):
                p = pairs[i]
                hp, hh = p["hl"] // 2, p["hl"] % 2
                pl, ph = hh * 64, hh * 64 + 64
                k0, q0 = p["jb"] * 128, p["g"] * TT
                dgn = p["jb"] - 4 * p["g"]
                Zb = p["Zb"]; Xb = Xs[i % 2]; p["Xb"] = Xb
                e_ = E[i % 2]; l1 = L1[i % 3]
                fw.op("act", [Zb], [e_], lambda e: e.activation(out=e_[:], in_=Zb[:], func=AF.Exp))
                fw.op("act", [e_], [l1], lambda e: e.activation(out=l1[:], in_=e_[:], func=AF.Ln, bias=1.0))
                if dgn >= 0:
                    mk = C_AMASK + dgn * 512
                    fw.op("pool", [l1, cb], [l1], lambda e: e.tensor_tensor(l1[:], l1[:], cb[:, mk:mk + 512], op=ALU.mult))
                fw.op("pe", [KT[hp], QT[hp]], [Xb],
                      lambda e: e.matmul(Xb[:], KT[hp][pl:ph, k0:k0 + 128], QT[hp][pl:ph, q0:q0 + TT], start=True, stop=False), inc=False)
                fw.op("pe", [cb, l1], [Xb],
                      lambda e: e.matmul(Xb[:], cb[:, C_NEGU:C_NEGU + 128], l1[:], start=False, stop=True))
                if p["jb"] > 0:
                    Sb = Ss[i % 2]; p["Sb"] = Sb
                    fw.op("pe", [cb, l1], [Sb], lambda e: e.matmul(Sb[:], cb_ones(), l1[:], start=True, stop=True))

            def stDE(i):
                p = pairs[i]
                hl = p["hl"]
                hp, hh = hl // 2, hl % 2
                pl, ph = hh * 64, hh * 64 + 64
                jb, jtop, q0 = p["jb"], p["jtop"], p["g"] * TT
                dgn = jb - 4 * p["g"]
                Xb = p["Xb"]
                w_ = W[i % 3]
                if jb == jtop:
                    ob_i[0] += 1
                    fw.op("act", [Xb], [w_], lambda e: e.activation(out=w_[:], in_=Xb[:], func=AF.Exp))
                    if jb > 0:
                        fw.op("dve", [p["Sb"]], [Cc], lambda e: e.tensor_copy(Cc[:], p["Sb"][:]))
                else:
                    x_ = X[i % 2]
                    fw.op("dve", [Xb, Cc], [x_], lambda e: e.tensor_tensor(x_[:], Xb[:], Cc[:], op=ALU.subtract))
                    if jb > 0:
                        fw.op("dve", [p["Sb"], Cc], [Cc], lambda e: e.tensor_tensor(Cc[:], Cc[:], p["Sb"][:], op=ALU.add))
                    fw.op("act", [x_], [w_], lambda e: e.activation(out=w_[:], in_=x_[:], func=AF.Exp))
                if dgn >= 0:
                    mk = C_AMASK + dgn * 512
                    fw.op("pool", [w_, cb], [w_], lambda e: e.tensor_tensor(w_[:], w_[:], cb[:, mk:mk + 512], op=ALU.mult))
                Ob = Os[ob_i[0] % 2]
                fw.op("pe", [V, w_], [Ob],
                      lambda e: e.matmul(Ob[pl:ph, :], V[:, jb, hl * 64:(hl + 1) * 64], w_[:], start=(jb == jtop), stop=(jb == 0)),
                      inc=(jb == 0))
                if jb == 0:
                    fw.op("dve", [Ob], [OT[hp]], lambda e: e.tensor_copy(OT[hp][pl:ph, q0:q0 + TT], Ob[pl:ph, :]))

            npairs = len(pairs)
            for i in range(-1, npairs):
                if 0 <= i + 1 < npairs:
                    stA(i + 1)
                    stBC(i + 1)
                if i >= 0:
                    stDE(i)
            for hf in range(2):
                fw.dma("pool", sendR[l].ap()[hf * 256:(hf + 1) * 256, :].rearrange("(hp p) t -> p hp t", p=128),
                       OT_t[:, :, hf * TOK:(hf + 1) * TOK], OT, [T_sendR[l]])
            fw.barrier()

    def gla(l):
        with ExitStack() as es:
            Pb = [fw.ptile(es, f"PGb{i}", [128, TT], F32) for i in range(2)]
            Pa = [fw.ptile(es, f"PGa{i}", [128, TT], F32) for i in range(2)]
            Po = [fw.ptile(es, f"PGo{i}", [128, TT], F32) for i in range(2)]
            Ps = fw.ptile(es, "PGs", [128, TT], F32)
            Pt = fw.ptile(es, "PGt", [128, 1024], BF16)
            NSEG = 8
            SEGT = SEQ // NSEG
            QT = [fw.tile(es, f"gQ{i}", [128, 2, SEGT], BF16) for i in range(2)]
            KT = [fw.tile(es, f"gK{i}", [128, 2, SEGT], BF16) for i in range(2)]
            Vs = [fw.tile(es, f"gV{i}", [128, 8, 512], BF16) for i in range(2)]
            Gs = [fw.tile(es, f"gG{i}", [128, 8, 256], BF16) for i in range(2)]
            Os = [fw.tile(es, f"gO{i}", [128, 4, SEGT], BF16) for i in range(2)]
            S32 = [fw.tile(es, f"S32_{i}", [128, 256], F32) for i in range(2)]
            Sbf = [fw.tile(es, f"Sbf_{i}", [128, 256], BF16) for i in range(2)]
            tS = [fw.tile(es, f"tS_{i}", [128, 256], F32) for i in range(2)]
            ebt = [fw.tile(es, f"eb{i}", [128, 128], F32) for i in range(4)]
            enb = [fw.tile(es, f"enb{i}", [128, 128], F32) for i in range(4)]
            qt = [fw.tile(es, f"qt{i}", [128, 128], BF16) for i in range(4)]
            kt = [fw.tile(es, f"kt{i}", [128, 128], BF16) for i in range(4)]
            ktok = [fw.tile(es, f"ktok{i}", [128, 128], BF16) for i in range(4)]
            Am = [fw.tile(es, f"Am{i}", [128, 128], BF16) for i in range(4)]
            for i in range(2):
                fw.op("pool", [], [S32[i]], lambda e, i=i: e.memset(S32[i][:], 0.0))
                fw.op("pool", [], [Sbf[i]], lambda e, i=i: e.memset(Sbf[i][:], 0.0))
            sR = sendR[l].ap()
            it = [0]
            for sg in range(min(NSEG, GLA_NSEG)):
                hft = sg // 4
                loc = (sg % 4) * SEGT
                q_, k_, v_, g_, o_t = QT[sg % 2], KT[sg % 2], Vs[sg % 2], Gs[sg % 2], Os[sg % 2]
                lf = locF[l][hft].ap()
                Tl = T_locF[l][hft]
                fw.dma("pool", q_[:], lf[0:256, loc:loc + SEGT].rearrange("(h p) t -> p h t", p=128), [Tl], [q_])
                fw.dma("pool", k_[:], lf[256:512, loc:loc + SEGT].rearrange("(h p) t -> p h t", p=128), [Tl], [k_])
                fw.dma("pool", v_[:],
                       lf[512:1024, :].rearrange("y (t8 c) -> (y t8) c", c=512)[loc:loc + SEGT, :].rearrange("(ch p) c -> p ch c", p=128),
                       [Tl], [v_])
                fw.dma("pool", g_[:],
                       lf[1024:1280, :].rearrange("y (t16 c) -> (y t16) c", c=256)[loc:loc + SEGT, :].rearrange("(ch p) c -> p ch c", p=128),
                       [Tl], [g_])
                for ch in range(8):
                    c0 = ch * 128
                    for hh in range(2):
                        i4 = it[0] % 4; it[0] += 1
                        pb = Pb[i4 % 2]; pa = Pa[i4 % 2]; po = Po[i4 % 2]
                        eb_, en_, qt_, kt_, ktk, am = ebt[i4], enb[i4], qt[i4], kt[i4], ktok[i4], Am[i4]
                        fw.op("pe", [g_, cb], [pb], lambda e: e.matmul(pb[:, 0:128], g_[:, ch, hh * 128:(hh + 1) * 128],
                                                                       cb[:, C_TRIN:C_TRIN + 128], start=True, stop=True))
                        fw.op("act", [pb], [eb_], lambda e: e.activation(out=eb_[:], in_=pb[:, 0:128], func=AF.Exp))
                        fw.op("act", [pb], [en_], lambda e: e.activation(out=en_[:], in_=pb[:, 0:128], func=AF.Exp, scale=-1.0))
                        fw.op("dve", [q_, eb_], [qt_], lambda e: e.scalar_tensor_tensor(out=qt_[:], in0=q_[:, hh, c0:c0 + 128], scalar=128.0 ** -0.5,
                                                                                        in1=eb_[:], op0=ALU.mult, op1=ALU.mult))
                        fw.op("pool", [k_, en_], [kt_], lambda e: e.tensor_tensor(kt_[:], k_[:, hh, c0:c0 + 128], en_[:], op=ALU.mult))
                        ptv = Pt[:, i4 * 128:(i4 + 1) * 128]
                        fw.op("pe", [kt_, cb], [Pt], lambda e: e.transpose(ptv, kt_[:], cb_id()))
                        fw.op("act", [Pt], [ktk], lambda e: e.activation(out=ktk[:], in_=ptv, func=AF.Copy))
                        fw.op("pe", [kt_, qt_], [pa], lambda e: e.matmul(pa[:, 0:128], kt_[:], qt_[:], start=True, stop=True))
                        fw.op("dve", [pa, cb], [am], lambda e: e.tensor_tensor(am[:], pa[:, 0:128], cb[:, C_GMASK:C_GMASK + 128], op=ALU.mult))
                        for vc in range(2):
                            fw.op("pe", [v_, am], [po],
                                  lambda e, vc=vc: e.matmul(po[:, vc * 128:(vc + 1) * 128], v_[:, ch, hh * 256 + vc * 128:hh * 256 + (vc + 1) * 128],
                                                            am[:], start=True, stop=False), inc=False)
                            fw.op("pe", [Sbf[hh], qt_], [po],
                                  lambda e, vc=vc: e.matmul(po[:, vc * 128:(vc + 1) * 128], Sbf[hh][:, vc * 128:(vc + 1) * 128],
                                                            qt_[:], start=False, stop=True), inc=(vc == 1))
                        fw.op("act", [po], [o_t], lambda e: e.activation(out=o_t[:, hh * 2:hh * 2 + 2, c0:c0 + 128],
                                                                       in_=po[:, 0:256].rearrange("p (v t) -> p v t", v=2), func=AF.Copy))
                        fw.op("pe", [ktk, v_], [Ps], lambda e: e.matmul(Ps[:, hh * 256:(hh + 1) * 256], ktk[:], v_[:, ch, hh * 256:(hh + 1) * 256],
                                                                        start=True, stop=True))
                        fw.op("dve", [S32[hh], eb_], [tS[hh]], lambda e: e.tensor_scalar(tS[hh][:], S32[hh][:], eb_[:, 127:128], None, op0=ALU.mult))
                        fw.op("dve", [Ps, eb_, tS[hh]], [S32[hh]],
                              lambda e: e.scalar_tensor_tensor(out=S32[hh][:], in0=Ps[:, hh * 256:(hh + 1) * 256], scalar=eb_[:, 127:128],
                                                               in1=tS[hh][:], op0=ALU.mult, op1=ALU.add))
                        fw.op("pool", [S32[hh]], [Sbf[hh]], lambda e: e.tensor_copy(Sbf[hh][:], S32[hh][:]))
                fw.dma("pool", sR[hft * 512:(hft + 1) * 512, loc:loc + SEGT].rearrange("(c p) t -> p c t", p=128), o_t[:], [o_t], [T_sendR[l]])
            fw.barrier()

    if test in ("loop1", "loop2"):
        stop_after = int(test[-1])
        token_loop(stop_after)
        return nc, fw
    if test == "attn":
        attention(0)
        return nc, fw
    if test == "gla":
        gla(1)
        return nc, fw
    if stop_after is not None and stop_after < 0:
        with ExitStack() as es:
            ht = fw.tile(es, "pt_h", [128, 8, TT], F32)
            w16 = fw.tile(es, "pt_w", [128, 2048], BF16)
            fw.dma("sp", w16[:], waA.ap()[175 * 128:176 * 128, :], [T_waA], [w16])
            for tt in range(NTT):
                fw.dma("pool", ht[:], xT_d.ap()[:, tt * TT:(tt + 1) * TT].rearrange("(kc p) t -> p kc t", p=128), [], [ht])
                if tt == 0:
                    fw.op("dve", [w16, ht], [ht], lambda e: e.tensor_copy(ht[:, 0, :], w16[:, 0:TT]))
                fw.dma("pool", out_d.ap()[:, tt * TT:(tt + 1) * TT].rearrange("(kc p) t -> p kc t", p=128), ht[:], [ht], [T_out])
        fw.barrier()
        return nc, fw
    nloops = 5 if stop_after is None else stop_after + 1
    for k in range(nloops):
        token_loop(k)
        if k < 4 and not (stop_after is not None and k == stop_after):
            l = k
            allgather(sendF[l], gathF[l], T_sendF[l], T_gathF[l])
            fetch_blocks(gathF[l], T_gathF[l], locF[l], T_locF[l], BE if l % 2 == 0 else BG)
            fw.barrier()
            if l % 2 == 0:
                attention(l)
            else:
                gla(l)
            allgather(sendR[l], gathR[l], T_sendR[l], T_gathR[l])
            fetch_blocks(gathR[l], T_gathR[l], locR[l], T_locR[l], 256 if l % 2 == 0 else 512)
            fw.barrier()
    fw.barrier()
    return nc, fw


def _consts(norm_g, gla_w_in, gla_w_gate, gla_b_gate, gla_norm_g, conv_dw_w, conv_dw_b, conv_ln_g, conv_ln_b, j):
    c = np.zeros((128, NCONST), np.float32)
    ii = np.arange(128)
    c[:, C_ID:C_ID + 128] = np.eye(128, dtype=np.float32)
    c[:, C_ONES:C_ONES + 128] = 1.0
    c[:, C_NEGU:C_NEGU + 128] = -(ii[:, None] >= ii[None, :]).astype(np.float32)
    c[:, C_TRIN:C_TRIN + 128] = (ii[:, None] <= ii[None, :]).astype(np.float32) * (-1.0 / 16.0)
    c[:, C_GMASK:C_GMASK + 128] = (ii[:, None] <= ii[None, :]).astype(np.float32)
    qq = np.arange(512)
    for d in range(4):
        c[:, C_AMASK + d * 512:C_AMASK + (d + 1) * 512] = ((d * 128 + ii[:, None]) < qq[None, :]).astype(np.float32)
    for o in range(2):
        c[:, C_WGLR + o * 128:C_WGLR + (o + 1) * 128] = \
            gla_w_in[o][:, 3072:3088].reshape(8, 128, 16).transpose(1, 0, 2).reshape(128, 128)
        c[0:16, C_WGATE + o * 512:C_WGATE + (o + 1) * 512] = gla_w_gate[o]
        c[16, C_WGATE + o * 512:C_WGATE + (o + 1) * 512] = gla_b_gate[o]
        c[:, C_GLANG + o * 2:C_GLANG + o * 2 + 2] = gla_norm_g[o].reshape(2, 128).T
    c[:, C_NORMG:C_NORMG + 192] = norm_g.reshape(4, 6, 8, 128).transpose(3, 0, 1, 2).reshape(128, 192)
    for e in range(2):
        c[:, C_CONVW + e * 124:C_CONVW + (e + 1) * 124] = conv_dw_w[e].reshape(31, 4, 128).transpose(2, 1, 0).reshape(128, 124)
        for wi, arr in enumerate((conv_dw_b, conv_ln_g, conv_ln_b)):
            c[:, C_CONVP + e * 12 + wi * 4:C_CONVP + e * 12 + wi * 4 + 4] = arr[e].reshape(4, 128).T
    c[:, C_FLAG] = float(j)
    return c


def _weight_units(ffn1_w_in, ffn1_w_out, ffn2_w_in, ffn2_w_out, hyb_w_in, hyb_w_out, gla_w_in, gla_w_out):
    A = np.empty((176, 128, 2048), np.float32)
    B = np.empty((64, 128, 2816), np.float32)
    for l in range(4):
        for which, (wi, wo) in enumerate(((ffn1_w_in, ffn1_w_out), (ffn2_w_in, ffn2_w_out))):
            u = (l * 2 + which)
            A[u * 22:(u + 1) * 22] = wi[l].reshape(8, 128, 2, 22, 128).transpose(3, 1, 0, 2, 4).reshape(22, 128, 2048)
            B[u * 8:(u + 1) * 8] = wo[l].reshape(22, 128, 8, 128).transpose(2, 1, 0, 3).reshape(8, 128, 2816)
    C = np.empty((120, 128, 1024), np.float32)

    def units(w, nf):
        return w.reshape(8, 128, nf, 128).transpose(2, 1, 0, 3).reshape(nf, 128, 1024)
    for e in range(2):
        b0 = e * 28
        C[b0:b0 + 20] = units(hyb_w_in[e], 20)
        C[b0 + 20:b0 + 28] = units(hyb_w_out[e], 8)
    for o in range(2):
        b0 = 56 + o * 32
        C[b0:b0 + 24] = units(np.ascontiguousarray(gla_w_in[o][:, :3072]), 24)
        C[b0 + 24:b0 + 32] = units(gla_w_out[o], 8)
    return A, B, C


_CACHE = {}


def _run(inputs, stop_after=None):
    x = np.asarray(inputs["x"], np.float32)
    g = {k: np.asarray(v, np.float32) for k, v in inputs.items() if k != "x"}
    A, B, C = _weight_units(g["ffn1_w_in"], g["ffn1_w_out"], g["ffn2_w_in"], g["ffn2_w_out"],
                            g["hyb_w_in"], g["hyb_w_out"], g["gla_w_in"], g["gla_w_out"])
    in_maps = []
    for c in range(NCORES):
        b, j = c // 2, c % 2
        in_maps.append({
            "xT": np.ascontiguousarray(x[b, j * TOK:(j + 1) * TOK, :].T),
            "wA": np.ascontiguousarray(A[c * 22:(c + 1) * 22].reshape(22 * 128, 2048)),
            "wB": np.ascontiguousarray(B[c * 8:(c + 1) * 8].reshape(8 * 128, 2816)),
            "wC": np.ascontiguousarray(C[c * 15:(c + 1) * 15].reshape(15 * 128, 1024)),
            "consts": _consts(g["norm_g"], g["gla_w_in"], g["gla_w_gate"], g["gla_b_gate"], g["gla_norm_g"],
                              g["conv_dw_w"], g["conv_dw_b"], g["conv_ln_g"], g["conv_ln_b"], j),
        })
    key = stop_after
    if key not in _CACHE:
        _CACHE[key] = build_program(stop_after)[0]
    nc = _CACHE[key]
    res = run_bass_kernel_spmd(nc, in_maps, core_ids=list(range(NCORES)))
    out = np.empty((4, SEQ, D), np.float32)
    for c in range(NCORES):
        b, j = c // 2, c % 2
        out[b, j * TOK:(j + 1) * TOK, :] = res.results[c]["outT"].T
    return out


def kernel(**inputs):
    return _run(inputs)
```

```python
import os
import math
import numpy as np
from contextlib import ExitStack
import concourse.bass as bass
import concourse.mybir as mybir
from concourse.bass_utils import run_bass_kernel_spmd

F32 = mybir.dt.float32
BF16 = mybir.dt.bfloat16
AF = mybir.ActivationFunctionType
ALU = mybir.AluOpType

D = 1024
DFF = 2816
NFC = 22
TOK = 4096
TT = 512
NTT = TOK // TT
SEQ = 8192
EPS = 1e-6
NCORES = 8
ATT_NH, ATT_NG, GLA_NSEG = 4, 16, 8

C_ID, C_ONES, C_NEGU, C_TRIN, C_GMASK = 0, 128, 256, 384, 512
C_AMASK = 640
C_WGLR = C_AMASK + 2048
C_WGATE = C_WGLR + 256
NBF = C_WGATE + 1024
C_NORMG = NBF
C_GLANG = C_NORMG + 192
C_CONVW = C_GLANG + 4
C_CONVP = C_CONVW + 248
C_FLAG = C_CONVP + 24
NCONST = C_FLAG + 1
NF32 = NCONST - NBF

RE = 1544
BE = 772
RG = 2560
BG = 1280


class T:
    __slots__ = ("base", "w", "r", "name", "multi")

    def __init__(self, base, name, multi=False):
        self.base = base
        self.w = None
        self.r = {}
        self.name = name
        self.multi = multi

    def __getitem__(self, idx):
        return self.base[idx]


class Eng:
    def __init__(self, name, h, sem):
        self.name = name
        self.h = h
        self.sem = sem
        self.count = 0
        self.known = {}
        self.pend_r = []
        self.pend_w = []


class FW:
    def __init__(self, nc):
        self.nc = nc
        self.es = ExitStack()
        self.eng = {}
        for name, h in (("pe", nc.tensor), ("act", nc.scalar), ("dve", nc.vector),
                        ("pool", nc.gpsimd), ("sp", nc.sync)):
            sem = self.es.enter_context(nc.semaphore("s_" + name))
            self.eng[name] = Eng(name, h, sem)
        self.dq = {}
        for q in ("sp", "pool"):
            sems = [self.es.enter_context(nc.semaphore(f"dq_{q}_{i}")) for i in range(8)]
            self.dq[q] = {"sems": sems, "n": 0}
        self.nwaits = 0
        self.nins = 0
        self._uid = 0
        self.pending = set()

    def sb(self, es, name, shape, dtype=F32):
        self._uid += 1
        t = es.enter_context(self.nc.sbuf_tensor(f"{name}_{self._uid}", list(shape), dtype))
        return t

    def ps(self, es, name, shape, dtype=F32):
        self._uid += 1
        t = es.enter_context(self.nc.psum_tensor(f"{name}_{self._uid}", list(shape), dtype))
        return t

    def tile(self, es, name, shape, dtype=F32):
        return T(self.sb(es, name, shape, dtype), name)

    def tiles(self, es, name, n, shape, dtype=F32):
        t = self.sb(es, name, [shape[0], n] + list(shape[1:]), dtype)
        return t, [T(t[:, i], f"{name}{i}") for i in range(n)]

    def ptile(self, es, name, shape, dtype=F32):
        return T(self.ps(es, name, shape, dtype), name)

    def sem(self, name):
        return self.es.enter_context(self.nc.semaphore(name))

    def _wait(self, E, ev):
        sem, val = ev
        k = id(sem)
        if E.known.get(k, 0) >= val:
            return
        E.h.wait_ge(sem, val)
        E.known[k] = val
        self.nwaits += 1

    def _deps(self, E, reads, writes, skip_self=False):
        for b in list(reads) + list(writes):
            assert id(b) not in self.pending or b in E.pend_r or b in E.pend_w, \
                f"access to {b.name} with pending (un-incremented) accesses"
        for b in reads:
            if b.multi:
                for ev in b.r.values():
                    self._wait(E, ev)
                continue
            if b.w is not None and not (skip_self and b.w[0] is E.sem):
                self._wait(E, b.w)
        for b in writes:
            if b.multi:
                continue
            if b.w is not None and not (skip_self and b.w[0] is E.sem):
                self._wait(E, b.w)
            for ev in b.r.values():
                if not (skip_self and ev[0] is E.sem):
                    self._wait(E, ev)

    def _mark(self, ev, reads, writes):
        k = id(ev[0])
        for b in reads:
            if b.multi:
                continue
            b.r[k] = ev
        for b in writes:
            if b.multi:
                b.r[k] = ev
            else:
                b.w = ev
                b.r = {}

    def op(self, eng, reads, writes, fn, inc=True):
        E = self.eng[eng]
        self._deps(E, reads, writes, skip_self=(eng == "pe"))
        ins = fn(E.h)
        self.nins += 1
        if inc:
            E.count += 1
            ins.then_inc(E.sem, 1)
            ev = (E.sem, E.count)
            self._mark(ev, E.pend_r + list(reads), E.pend_w + list(writes))
            for b in E.pend_r + E.pend_w:
                self.pending.discard(id(b))
            E.pend_r = []
            E.pend_w = []
        else:
            E.pend_r += list(reads)
            E.pend_w += list(writes)
            for b in list(reads) + list(writes):
                self.pending.add(id(b))
        return ins

    def dma(self, q, out_ap, in_ap, reads, writes, **kw):
        E = self.eng[q]
        Dq = self.dq[q]
        i = Dq["n"]
        Dq["n"] += 1
        sem = Dq["sems"][i % 8]
        prev = 16 * (i // 8)
        if prev > 0:
            self._wait(E, (sem, prev))
        self._deps(E, reads, writes)
        ins = E.h.dma_start(out=out_ap, in_=in_ap, **kw)
        ins.then_inc(sem, 16)
        self.nins += 1
        ev = (sem, prev + 16)
        self._mark(ev, reads, writes)
        return ev

    def wait_all_dma(self, q):
        E = self.eng[q]
        Dq = self.dq[q]
        n = Dq["n"]
        for s in range(8):
            cnt = (n - s + 7) // 8
            if cnt > 0:
                self._wait(E, (Dq["sems"][s], 16 * cnt))

    def collective(self, sem, in_ap, out_ap, reads, writes):
        E = self.eng["pool"]
        self._deps(E, reads, writes)
        ins = E.h.collective_compute("AllGather", ALU.bypass,
                                     replica_groups=[list(range(NCORES))],
                                     ins=[in_ap], outs=[out_ap])
        ins.then_inc(sem)
        self.nins += 1
        self._mark((sem, 1), reads, writes)

    def barrier(self):
        for q in ("sp", "pool"):
            self.wait_all_dma(q)
        evs = []
        for name, E in self.eng.items():
            assert not E.pend_r and not E.pend_w
            if E.count > 0:
                self._wait(E, (E.sem, E.count))
            E.count += 1
            E.h.sem_inc(E.sem, 1)
            evs.append((E.sem, E.count))
        for name, E in self.eng.items():
            for ev in evs:
                self._wait(E, ev)


def build_program(stop_after=None, sim=False, test=None):
    nc = bass.Bass("TRN2", target_bir_lowering=False)
    fw = FW(nc)
    ges = fw.es

    if test in ("attn", "gla"):
        xT_d = nc.dram_tensor("xT", [D, TOK], F32)
        wA_d = nc.dram_tensor("wA", [22 * 128, 2048], F32)
        wB_d = nc.dram_tensor("wB", [8 * 128, 2816], F32)
        wC_d = nc.dram_tensor("wC", [15 * 128, 1024], F32)
    else:
        xT_d = nc.dram_tensor("xT", [D, TOK], F32, kind="ExternalInput")
        wA_d = nc.dram_tensor("wA", [22 * 128, 2048], F32, kind="ExternalInput")
        wB_d = nc.dram_tensor("wB", [8 * 128, 2816], F32, kind="ExternalInput")
        wC_d = nc.dram_tensor("wC", [15 * 128, 1024], F32, kind="ExternalInput")
    cst_d = nc.dram_tensor("consts", [128, NCONST], F32, kind="ExternalInput")
    out_d = nc.dram_tensor("outT", [D, TOK], F32, kind="ExternalOutput") if test not in ("attn", "gla") else nc.dram_tensor("outT", [D, TOK], F32)

    ext = {}
    if test == "attn":
        ext = {"locF0_0": "ExternalInput", "locF0_1": "ExternalInput", "sendR0": "ExternalOutput"}
    if test == "gla":
        ext = {"locF1_0": "ExternalInput", "locF1_1": "ExternalInput", "sendR1": "ExternalOutput"}
    if test == "loop1":
        ext = {"hT": "ExternalInput", "locR0_0": "ExternalInput", "locR0_1": "ExternalInput", "cdr0": "ExternalInput",
               "locF0_0": "ExternalInput", "sendF1": "ExternalOutput", "rdr1": "ExternalOutput"}
    if test == "loop2":
        ext = {"hT": "ExternalInput", "locR1_0": "ExternalInput", "locR1_1": "ExternalInput", "rdr1": "ExternalInput",
               "sendF0": "ExternalOutput", "cdr0": "ExternalOutput"}

    def idram(name, shape, dt):
        if name in ext:
            return nc.dram_tensor(name, list(shape), dt, kind=ext[name])
        return nc.dram_tensor(name, list(shape), dt)

    wsA = idram("wsA", [22 * 128, 2048], BF16); wsB = idram("wsB", [8 * 128, 2816], BF16); wsC = idram("wsC", [15 * 128, 1024], BF16)
    if sim and test not in ("attn", "gla"):
        waA = nc.dram_tensor("waA", [176 * 128, 2048], BF16, kind="ExternalInput")
        waB = nc.dram_tensor("waB", [64 * 128, 2816], BF16, kind="ExternalInput")
        waC = nc.dram_tensor("waC", [120 * 128, 1024], BF16, kind="ExternalInput")
    else:
        waA = idram("waA", [176 * 128, 2048], BF16)
        waB = idram("waB", [64 * 128, 2816], BF16)
        waC = idram("waC", [120 * 128, 1024], BF16)
    hT_d = idram("hT", [D, TOK], F32)
    sendF = {}; gathF = {}; sendR = {}; gathR = {}; cdr = {}; rdr = {}
    for l in range(2):
        if l % 2 == 0:
            sendF[l] = idram(f"sendF{l}", [RE, TOK], BF16); gathF[l] = idram(f"gathF{l}", [8 * RE, TOK], BF16)
            sendR[l] = idram(f"sendR{l}", [512, TOK], BF16); gathR[l] = idram(f"gathR{l}", [8 * 512, TOK], BF16)
            cdr[l] = idram(f"cdr{l}", [512, TOK], BF16)
        else:
            sendF[l] = idram(f"sendF{l}", [RG, TOK], BF16); gathF[l] = idram(f"gathF{l}", [8 * RG, TOK], BF16)
            sendR[l] = idram(f"sendR{l}", [1024, TOK], BF16); gathR[l] = idram(f"gathR{l}", [8 * 1024, TOK], BF16)
            rdr[l] = idram(f"rdr{l}", [D, TOK], F32)
    for l in (2, 3):
        sendF[l] = sendF[l - 2]; gathF[l] = gathF[l - 2]; sendR[l] = sendR[l - 2]; gathR[l] = gathR[l - 2]
        if l == 2: cdr[l] = cdr[0]
        else: rdr[l] = rdr[1]
    T_wsA = T(wsA, "wsA", multi=True); T_waA = T(waA, "waA")
    T_wsB = T(wsB, "wsB", multi=True); T_waB = T(waB, "waB")
    T_wsC = T(wsC, "wsC", multi=True); T_waC = T(waC, "waC")
    T_h = [T(hT_d, f"hT{i}") for i in range(NTT)]
    T_sendF = {l: T(sendF[l], f"sendF{l}", multi=True) for l in range(2)}
    T_gathF = {l: T(gathF[l], f"gathF{l}") for l in range(2)}
    T_sendR = {l: T(sendR[l], f"sendR{l}", multi=True) for l in range(2)}
    T_gathR = {l: T(gathR[l], f"gathR{l}") for l in range(2)}
    T_cdr = {0: [T(cdr[0], f"cdr_{i}") for i in range(NTT)]}
    T_rdr = {1: [T(rdr[1], f"rdr_{i}") for i in range(NTT)]}
    for l in (2, 3):
        T_sendF[l] = T_sendF[l - 2]; T_gathF[l] = T_gathF[l - 2]; T_sendR[l] = T_sendR[l - 2]; T_gathR[l] = T_gathR[l - 2]
    T_cdr[2] = T_cdr[0]; T_rdr[3] = T_rdr[1]
    T_out = T(out_d, "out", multi=True)
    cc_sems = [fw.sem(f"cc{i}") for i in range(11)]
    cc_i = [0]

    def allgather(send, gath, Ts, Tg):
        fw.collective(cc_sems[cc_i[0]], send.ap().opt(), gath.ap().opt(), [Ts], [Tg])
        cc_i[0] += 1

    pid = nc.partition_id([mybir.EngineType.Pool])
    B0 = nc.gpsimd.snap(pid + (pid // 2) * 2)
    locF = {}; locR = {}; T_locF = {}; T_locR = {}
    for l in range(2):
        rf, rr = (BE, 256) if l % 2 == 0 else (BG, 512)
        locF[l] = [idram(f"locF{l}_{s_}", [rf, TOK], BF16) for s_ in range(2)]
        locR[l] = [idram(f"locR{l}_{s_}", [rr, TOK], BF16) for s_ in range(2)]
        T_locF[l] = [T(locF[l][s_], f"locF{l}_{s_}") for s_ in range(2)]
        T_locR[l] = [T(locR[l][s_], f"locR{l}_{s_}") for s_ in range(2)]
    for l in (2, 3):
        locF[l] = locF[l - 2]; locR[l] = locR[l - 2]; T_locF[l] = T_locF[l - 2]; T_locR[l] = T_locR[l - 2]

    def fetch_blocks(gath, Tg, loc, Tloc, rows):
        v3 = gath.ap().rearrange("(b r) t -> b r t", r=rows)
        for s_ in range(2):
            fw.dma("pool", loc[s_].ap(), v3[2 * s_:, :, :][bass.ds(B0, 1), :, :].rearrange("o r t -> (o r) t"),
                   [Tg], [Tloc[s_]])

    cb_t = fw.sb(ges, "cb", [128, NBF], BF16); cb = T(cb_t, "cb")
    cf_t = fw.sb(ges, "cf", [128, NF32], F32); cf = T(cf_t, "cf")
    glr_aug = fw.tile(ges, "glr_aug", [32, TT], BF16)

    def cfc(col, n=1):
        return cf[:, col - NBF: col - NBF + n]

    with ExitStack() as es:
        ctmp = fw.tile(es, "ctmp", [128, NCONST], F32)
        fw.dma("sp", ctmp[:], cst_d.ap(), [], [ctmp])
        fw.op("dve", [ctmp], [cb], lambda e: e.tensor_copy(cb[:, 0:2048], ctmp[:, 0:2048]))
        fw.op("pool", [ctmp], [cb], lambda e: e.tensor_copy(cb[:, 2048:NBF], ctmp[:, 2048:NBF]))
        fw.op("act", [ctmp], [cf], lambda e: e.activation(out=cf[:], in_=ctmp[:, NBF:NCONST], func=AF.Copy))
        fw.op("pool", [], [glr_aug], lambda e: e.memset(glr_aug[:], 1.0))
        wi = 0
        for (src, dst, Tdst, n, L) in (() if sim else ((wA_d, wsA, T_wsA, 22, 2048), (wB_d, wsB, T_wsB, 8, 2816),
                                       (wC_d, wsC, T_wsC, 15, 1024))):
            st32 = [fw.tile(es, f"wst32_{L}_{i}", [128, L], F32) for i in range(2)]
            st16 = [fw.tile(es, f"wst16_{L}_{i}", [128, L], BF16) for i in range(2)]
            for u in range(n):
                a, b_ = st32[u % 2], st16[u % 2]
                fw.dma("sp", a[:], src.ap()[u * 128:(u + 1) * 128, :], [], [a])
                eng = ("dve", "pool", "act")[wi % 3]; wi += 1
                if eng == "act":
                    fw.op("act", [a], [b_], lambda e, a=a, b_=b_: e.activation(out=b_[:], in_=a[:], func=AF.Copy))
                else:
                    fw.op(eng, [a], [b_], lambda e, a=a, b_=b_: e.tensor_copy(b_[:], a[:]))
                fw.dma("pool", dst.ap()[u * 128:(u + 1) * 128, :], b_[:], [b_], [Tdst])
        if not sim:
            allgather(wsA, waA, T_wsA, T_waA)
            allgather(wsB, waB, T_wsB, T_waB)
            allgather(wsC, waC, T_wsC, T_waC)
        fw.barrier()

    cb_id = lambda: cb[:, C_ID:C_ID + 128]
    cb_ones = lambda: cb[:, C_ONES:C_ONES + 128]

    def uA(l, which, fc): return (l * 2 + which) * 22 + fc
    def uB(l, which, dc): return (l * 2 + which) * 8 + dc
    UC_BASE = {0: 0, 2: 28, 1: 56, 3: 88}

    def token_loop(k):
        lb = k - 1 if k > 0 else None
        la = k if k < 4 else None
        with ExitStack() as es:
            P = [fw.ptile(es, f"P{i}", [128, TT], F32) for i in range(8)]
            h_t, h = fw.tiles(es, "h", 8, [128, TT], F32)
            xn_t, xn = fw.tiles(es, "xn", 8, [128, TT], BF16)
            sq_t, sq = fw.tiles(es, "sq", 8, [128, TT], BF16)
            f_t, f = fw.tiles(es, "f", 8, [128, TT], F32)
            hm_t, hm = fw.tiles(es, "hm", NFC, [128, TT], BF16)
            cat_t, cat = fw.tiles(es, "cat", 8, [128, TT], BF16)
            lnv = fw.tile(es, "lnv", [128, TT], F32)
            rstd = fw.tile(es, "rstd", [128, TT], F32)
            sa = [fw.tile(es, f"sa{i}", [128, TT], F32) for i in range(2)]
            tmp = [fw.tile(es, f"tmp{i}", [128, TT], F32) for i in range(3)]
            stg = [fw.tile(es, f"stg{i}", [128, TT], BF16) for i in range(3)]
            stg32 = [fw.tile(es, f"stg32_{i}", [128, TT], F32) for i in range(2)]
            NSA, NSB, NSC = 3, 2, 6
            slotA = [fw.tile(es, f"slA{i}", [128, 2048], BF16) for i in range(NSA)]
            slotB = [fw.tile(es, f"slB{i}", [128, 2816], BF16) for i in range(NSB)]
            slotC = [fw.tile(es, f"slC{i}", [128, 1024], BF16) for i in range(NSC)]
            cnt = {"A": 0, "B": 0, "C": 0, "tmp": 0, "stg": 0, "sa": 0, "stg32": 0}
            even_b = lb is not None and lb % 2 == 0
            odd_b = lb is not None and lb % 2 == 1
            if even_b:
                eb_ = lb // 2
                cbuf_t, cbuf = fw.tiles(es, "cbuf", 4, [128, 32 + TT], BF16)
                dg = fw.tile(es, "dg", [128, 124 * 128], BF16)
                ysb_t, ysb = fw.tiles(es, "ysb", 4, [128, TT], F32)
                ybf_t, ybf = fw.tiles(es, "ybf", 4, [128, TT], BF16)
                mean = fw.tile(es, "mean", [128, TT], F32)
                for i in range(124):
                    col = C_CONVW + eb_ * 124 + i
                    fw.op("pool" if i % 2 else "dve", [cb, cf], [dg],
                          lambda e, i=i, col=col: e.tensor_scalar(dg[:, i * 128:(i + 1) * 128], cb_id(),
                                                                   cfc(col), None, op0=ALU.mult))
            if odd_b:
                ob_t, ob = fw.tiles(es, "ob", 8, [128, TT], BF16)

            def nxt(key, lst):
                i = cnt[key]; cnt[key] += 1
                return lst[i % len(lst)]

            def wload(kind, unit):
                if kind == "A":
                    s = nxt("A", slotA); src = waA.ap()[unit * 128:(unit + 1) * 128, :]; Tsrc = T_waA
                elif kind == "B":
                    s = nxt("B", slotB); src = waB.ap()[unit * 128:(unit + 1) * 128, :]; Tsrc = T_waB
                else:
                    s = nxt("C", slotC); src = waC.ap()[unit * 128:(unit + 1) * 128, :]; Tsrc = T_waC
                fw.dma("sp", s[:], src, [Tsrc], [s])
                return s

            plan = []
            def plan_ffn(l, which):
                for fc in range(NFC): plan.append(("A", uA(l, which, fc)))
                for dc in range(8): plan.append(("B", uB(l, which, dc)))
            if even_b:
                for dc in range(8): plan.append(("C", UC_BASE[lb] + 20 + dc))
            if odd_b:
                for dc in range(8): plan.append(("C", UC_BASE[lb] + 24 + dc))
            if lb is not None: plan_ffn(lb, 1)
            if la is not None:
                plan_ffn(la, 0)
                if la % 2 == 0:
                    order = list(range(12)) + [12, 16, 13, 17, 14, 18, 15, 19]
                else:
                    order = list(range(24))
                for u in order: plan.append(("C", UC_BASE[la] + u))
            full = plan * NTT
            loaded = []
            ptr = {"issue": 0, "use": 0}
            NS = {"A": NSA, "B": NSB, "C": NSC}

            def wget(kind):
                i = ptr["use"]
                assert full[i][0] == kind, (full[i], kind, i)
                lo = max(i - 1, 0)
                while ptr["issue"] < len(full) and ptr["issue"] - i <= 12:
                    kd, un = full[ptr["issue"]]
                    infl = sum(1 for jj in range(lo, ptr["issue"]) if full[jj][0] == kd)
                    if infl >= NS[kd]:
                        assert ptr["issue"] > i
                        break
                    loaded.append(wload(kd, un))
                    ptr["issue"] += 1
                ptr["use"] += 1
                return loaded[i]

            def gcol(l, i):
                return C_NORMG + (l * 6 + i) * 8

            def rstd_from(psT, n, extra_bias=0.0):
                fw.op("act", [psT], [lnv], lambda e: e.activation(out=lnv[:], in_=psT[:], func=AF.Ln,
                                                                   scale=1.0 / n, bias=EPS))
                fw.op("act", [lnv], [rstd], lambda e: e.activation(out=rstd[:], in_=lnv[:], func=AF.Exp,
                                                                    scale=-0.5, bias=extra_bias))

            def sumsq_mm(ps, srcs):
                n = len(srcs)
                for i, s in enumerate(srcs):
                    fw.op("pe", [cb, s], [ps], lambda e, s=s, i=i: e.matmul(ps[:], cb_ones(), s[:], start=(i == 0),
                                                                           stop=(i == n - 1)), inc=(i == n - 1))

            def prenorm(l, gi):
                for kc in range(8):
                    fw.op("act", [h[kc]], [sq[kc]], lambda e, kc=kc: e.activation(out=sq[kc][:], in_=h[kc][:], func=AF.Square))
                sumsq_mm(P[6], sq)
                rstd_from(P[6], D)
                gc = gcol(l, gi)
                for kc in range(8):
                    fw.op("dve", [h[kc], cf, rstd], [xn[kc]],
                          lambda e, kc=kc: e.scalar_tensor_tensor(out=xn[kc][:], in0=h[kc][:], scalar=cfc(gc + kc),
                                                                  in1=rstd[:], op0=ALU.mult, op1=ALU.mult))

            def post_residual(l, gi, res_scale):
                sumsq_mm(P[6], sq)
                rstd_from(P[6], D, extra_bias=math.log(res_scale))
                gc = gcol(l, gi)
                for dc in range(8):
                    tp = nxt("tmp", tmp)
                    fw.op("dve", [f[dc], cf, rstd], [tp],
                          lambda e, dc=dc, tp=tp: e.scalar_tensor_tensor(out=tp[:], in0=f[dc][:], scalar=cfc(gc + dc),
                                                                         in1=rstd[:], op0=ALU.mult, op1=ALU.mult))
                    fw.op("pool", [tp, h[dc]], [h[dc]],
                          lambda e, dc=dc, tp=tp: e.tensor_tensor(h[dc][:], tp[:], h[dc][:], op=ALU.add))

            def evac_f(ps, dc):
                fw.op("dve", [ps], [f[dc]], lambda e: e.tensor_copy(f[dc][:], ps[:]))
                fw.op("act", [f[dc]], [sq[dc]], lambda e: e.activation(out=sq[dc][:], in_=f[dc][:], func=AF.Square))

            def ffn(l, which):
                prenorm(l, 0 if which == 0 else 4)
                for fc in range(NFC):
                    wu = wget("A")
                    pa, pb = (P[0], P[1]) if fc % 2 == 0 else (P[2], P[3])
                    for half, ps in ((0, pa), (1, pb)):
                        for kc in range(8):
                            off = kc * 256 + half * 128
                            fw.op("pe", [wu, xn[kc]], [ps],
                                  lambda e, ps=ps, wu=wu, kc=kc, off=off: e.matmul(ps[:], wu[:, off:off + 128], xn[kc][:],
                                                                                  start=(kc == 0), stop=(kc == 7)),
                                  inc=(kc == 7))
                    s_ = nxt("sa", sa)
                    fw.op("act", [pa], [s_], lambda e, s_=s_, pa=pa: e.activation(out=s_[:], in_=pa[:], func=AF.Silu))
                    fw.op("dve", [s_, pb], [hm[fc]], lambda e, s_=s_, pb=pb, fc=fc: e.tensor_tensor(hm[fc][:], s_[:], pb[:], op=ALU.mult))
                for dc in range(8):
                    wo = wget("B")
                    ps = P[4 + dc % 2]
                    for fc in range(NFC):
                        fw.op("pe", [wo, hm[fc]], [ps],
                              lambda e, ps=ps, wo=wo, fc=fc: e.matmul(ps[:], wo[:, fc * 128:(fc + 1) * 128], hm[fc][:],
                                                                      start=(fc == 0), stop=(fc == NFC - 1)),
                              inc=(fc == NFC - 1))
                    evac_f(ps, dc)
                post_residual(l, 1 if which == 0 else 5, 0.5)

            def proj_fm(wu, ps, src=None):
                src = src or xn
                for kc in range(8):
                    fw.op("pe", [wu, src[kc]], [ps],
                          lambda e, kc=kc: e.matmul(ps[:], wu[:, kc * 128:(kc + 1) * 128], src[kc][:],
                                                    start=(kc == 0), stop=(kc == 7)), inc=(kc == 7))

            def proj_tm(nun, pss):
                for i in range(nun):
                    wu = wget("C")
                    for tb in range(4):
                        for kc in range(8):
                            fw.op("pe", [wu, xn[kc]], [pss[tb]],
                                  lambda e, i=i, wu=wu, tb=tb, kc=kc: e.matmul(
                                      pss[tb][:, i * 128:(i + 1) * 128], xn[kc][:, tb * 128:(tb + 1) * 128],
                                      wu[:, kc * 128:(kc + 1) * 128], start=(kc == 0), stop=(kc == 7)),
                                  inc=(kc == 7 and tb == 3))

            def out_proj_and_residual(l):
                for dc in range(8):
                    wu = wget("C")
                    ps = P[4 + dc % 2]
                    proj_fm(wu, ps, src=cat)
                    evac_f(ps, dc)
                post_residual(l, 3, 1.0)

            for tt in range(NTT):
                t0 = tt * TT
                src = hT_d if k > 0 else xT_d
                fw.dma("pool", h_t[:], src.ap()[:, t0:t0 + TT].rearrange("(kc p) t -> p kc t", p=128),
                       [T_h[tt]] if k > 0 else [], h)
                if even_b:
                    l = lb
                    for kc in range(4):
                        fw.dma("pool", cat[kc][:], locR[l][kc // 2].ap()[(kc % 2) * 128:(kc % 2 + 1) * 128, t0:t0 + TT],
                               [T_locR[l][kc // 2]], [cat[kc]])
                    fw.dma("pool", cbuf_t[:, :, 32:32 + TT],
                           cdr[l].ap()[:, t0:t0 + TT].rearrange("(cc p) t -> p cc t", p=128), [T_cdr[l][tt]], cbuf)
                    if tt > 0:
                        fw.dma("pool", cbuf_t[:, :, 0:32],
                               cdr[l].ap()[:, t0 - 32:t0].rearrange("(cc p) t -> p cc t", p=128), [T_cdr[l][tt - 1]], cbuf)
                    else:
                        fw.dma("pool", cbuf_t[:, :, 0:32],
                               locF[l][0].ap()[768:772, :].rearrange("cc (p i) -> p cc i", i=32),
                               [T_locF[l][0]], cbuf)
                        fw.op("pool", cbuf + [cf], cbuf,
                              lambda e: e.tensor_scalar(cbuf_t[:, :, 0:32], cbuf_t[:, :, 0:32], cfc(C_FLAG), None, op0=ALU.mult))
                    pc = C_CONVP + eb_ * 12
                    for cc in range(4):
                        ps = P[cc]
                        for kk in range(31):
                            fw.op("pe", [dg, cbuf[cc]], [ps],
                                  lambda e, ps=ps, cc=cc, kk=kk: e.matmul(ps[:], dg[:, (cc * 31 + kk) * 128:(cc * 31 + kk + 1) * 128],
                                                                          cbuf[cc][:, 2 + kk:2 + kk + TT], start=(kk == 0), stop=(kk == 30)),
                                  inc=(kk == 30))
                        fw.op("act", [ps, cf], [ysb[cc]], lambda e, ps=ps, cc=cc: e.activation(out=ysb[cc][:], in_=ps[:], func=AF.Identity,
                                                                                             bias=cfc(pc + cc)))
                        fw.op("act", [ysb[cc]], [sq[cc]], lambda e, cc=cc: e.activation(out=sq[cc][:], in_=ysb[cc][:], func=AF.Square))
                        fw.op("pool", [ysb[cc]], [ybf[cc]], lambda e, cc=cc: e.tensor_copy(ybf[cc][:], ysb[cc][:]))
                    sumsq_mm(P[4], ybf)
                    sumsq_mm(P[5], sq[0:4])
                    m2 = nxt("tmp", tmp)
                    var = nxt("tmp", tmp)
                    fw.op("dve", [P[4]], [mean], lambda e: e.tensor_scalar(mean[:], P[4][:], 1.0 / 512, None, op0=ALU.mult))
                    fw.op("dve", [mean], [m2], lambda e: e.tensor_tensor(m2[:], mean[:], mean[:], op=ALU.mult))
                    fw.op("dve", [P[5], m2], [var], lambda e: e.scalar_tensor_tensor(out=var[:], in0=P[5][:], scalar=1.0 / 512, in1=m2[:],
                                                                                   op0=ALU.mult, op1=ALU.subtract))
                    rstd_from(var, 1.0)
                    for cc in range(4):
                        t1 = nxt("tmp", tmp)
                        fw.op("dve", [ysb[cc], mean], [t1], lambda e, cc=cc, t1=t1: e.tensor_tensor(t1[:], ysb[cc][:], mean[:], op=ALU.subtract))
                        fw.op("pool", [t1, rstd], [t1], lambda e, t1=t1: e.tensor_tensor(t1[:], t1[:], rstd[:], op=ALU.mult))
                        fw.op("act", [t1, cf], [cat[4 + cc]],
                              lambda e, cc=cc, t1=t1: e.activation(out=cat[4 + cc][:], in_=t1[:], func=AF.Silu,
                                                                   scale=cfc(pc + 4 + cc), bias=cfc(pc + 8 + cc)))
                    out_proj_and_residual(l)
                if odd_b:
                    l = lb
                    o_ = l // 2
                    for oc in range(8):
                        fw.dma("pool", ob[oc][:], locR[l][oc // 4].ap()[(oc % 4) * 128:(oc % 4 + 1) * 128, t0:t0 + TT],
                               [T_locR[l][oc // 4]], [ob[oc]])
                    for hd in range(4):
                        for cc in range(2):
                            fw.op("act", [ob[2 * hd + cc]], [sq[cc]],
                                  lambda e, hd=hd, cc=cc: e.activation(out=sq[cc][:], in_=ob[2 * hd + cc][:], func=AF.Square))
                        sumsq_mm(P[hd % 2], sq[0:2])
                        rstd_from(P[hd % 2], 256)
                        for cc in range(2):
                            oc = 2 * hd + cc
                            t1 = nxt("tmp", tmp)
                            s32 = nxt("stg32", stg32)
                            fw.dma("pool", s32[:], rdr[l].ap()[oc * 128:(oc + 1) * 128, t0:t0 + TT], [T_rdr[l][tt]], [s32])
                            fw.op("dve", [ob[oc], cf, rstd], [t1],
                                  lambda e, oc=oc, cc=cc, t1=t1: e.scalar_tensor_tensor(out=t1[:], in0=ob[oc][:],
                                                                                        scalar=cfc(C_GLANG + o_ * 2 + cc),
                                                                                        in1=rstd[:], op0=ALU.mult, op1=ALU.mult))
                            fw.op("pool", [t1, s32], [cat[oc]], lambda e, oc=oc, t1=t1, s32=s32: e.tensor_tensor(cat[oc][:], t1[:], s32[:], op=ALU.mult))
                    out_proj_and_residual(l)
                if lb is not None:
                    ffn(lb, 1)
                if la is not None:
                    l = la
                    ffn(l, 0)
                    prenorm(l, 2)
                    sF = sendF[l].ap()
                    if l % 2 == 0:
                        for fc in range(8):
                            wu = wget("C")
                            ps = P[fc % 4]
                            proj_fm(wu, ps)
                            st = nxt("stg", stg)
                            sc_ = 0.125 if fc < 4 else 1.0
                            fw.op("act", [ps], [st], lambda e, ps=ps, st=st, sc_=sc_: e.activation(out=st[:], in_=ps[:], func=AF.Copy, scale=sc_))
                            f4 = fc % 4
                            row0 = (f4 // 2) * BE + (256 if fc >= 4 else 0) + (f4 % 2) * 128
                            fw.dma("pool", sF[row0:row0 + 128, t0:t0 + TT], st[:], [st], [T_sendF[l]])
                        proj_tm(4, P[0:4])
                        for tb in range(4):
                            st = nxt("stg", stg)
                            fw.op("dve" if tb % 2 else "act", [P[tb]], [st],
                                  (lambda e, tb=tb, st=st: e.tensor_copy(st[:], P[tb][:])) if tb % 2 else
                                  (lambda e, tb=tb, st=st: e.activation(out=st[:], in_=P[tb][:], func=AF.Copy)))
                            for hf in range(2):
                                vsec = sF[hf * BE + 512:hf * BE + 768, :].rearrange("r (t16 c) -> (r t16) c", c=256)
                                fw.dma("pool", vsec[t0 + tb * 128:t0 + (tb + 1) * 128, :], st[:, hf * 256:(hf + 1) * 256], [st], [T_sendF[l]])
                        for fc in range(4):
                            pu, pg = (P[4], P[5]) if fc % 2 == 0 else (P[6], P[7])
                            wuu = wget("C")
                            wug = wget("C")
                            proj_fm(wuu, pu)
                            proj_fm(wug, pg)
                            s_ = nxt("sa", sa)
                            st = nxt("stg", stg)
                            fw.op("act", [pg], [s_], lambda e, pg=pg, s_=s_: e.activation(out=s_[:], in_=pg[:], func=AF.Sigmoid))
                            fw.op("dve", [s_, pu], [st], lambda e, pu=pu, s_=s_, st=st: e.tensor_tensor(st[:], s_[:], pu[:], op=ALU.mult))
                            fw.dma("pool", cdr[l].ap()[fc * 128:(fc + 1) * 128, t0:t0 + TT], st[:], [st], [T_cdr[l][tt]])
                            if tt == NTT - 1:
                                for hf in range(2):
                                    fw.dma("pool", sF[hf * BE + 768 + fc:hf * BE + 769 + fc, :].rearrange("o (p i) -> (o p) i", i=32),
                                           st[:, TT - 32:TT], [st], [T_sendF[l]])
                    else:
                        o_ = l // 2
                        for fc in range(8):
                            wu = wget("C")
                            ps = P[fc % 4]
                            proj_fm(wu, ps)
                            st = nxt("stg", stg)
                            fw.op("act", [ps], [st], lambda e, ps=ps, st=st: e.activation(out=st[:], in_=ps[:], func=AF.Copy))
                            f4 = fc % 4
                            row0 = (f4 // 2) * BG + (256 if fc >= 4 else 0) + (f4 % 2) * 128
                            fw.dma("pool", sF[row0:row0 + 128, t0:t0 + TT], st[:], [st], [T_sendF[l]])
                        for hf in range(2):
                            vsec = sF[hf * BG + 512:hf * BG + 1024, :].rearrange("r (t8 c) -> (r t8) c", c=512)
                            pss = P[0:4] if hf == 0 else P[4:8]
                            proj_tm(4, pss)
                            for tb in range(4):
                                st = nxt("stg", stg)
                                fw.op("dve" if tb % 2 else "act", [pss[tb]], [st],
                                      (lambda e, tb=tb, st=st, pss=pss: e.tensor_copy(st[:], pss[tb][:])) if tb % 2 else
                                      (lambda e, tb=tb, st=st, pss=pss: e.activation(out=st[:], in_=pss[tb][:], func=AF.Copy)))
                                fw.dma("pool", vsec[t0 + tb * 128:t0 + (tb + 1) * 128, :], st[:], [st], [T_sendF[l]])
                        for fc in range(8):
                            wu = wget("C")
                            ps = P[fc % 4]
                            proj_fm(wu, ps)
                            s32 = nxt("stg32", stg32)
                            fw.op("act", [ps], [s32], lambda e, ps=ps, s32=s32: e.activation(out=s32[:], in_=ps[:], func=AF.Silu))
                            fw.dma("pool", rdr[l].ap()[fc * 128:(fc + 1) * 128, t0:t0 + TT], s32[:], [s32], [T_rdr[l][tt]])
                        psg = P[4]
                        for kc in range(8):
                            c0 = C_WGLR + o_ * 128 + kc * 16
                            fw.op("pe", [cb, xn[kc]], [psg],
                                  lambda e, kc=kc, c0=c0: e.matmul(psg[0:16, :], cb[:, c0:c0 + 16], xn[kc][:], start=(kc == 0), stop=(kc == 7)),
                                  inc=(kc == 7))
                        fw.op("act", [psg], [glr_aug], lambda e: e.activation(out=glr_aug[0:16, :], in_=psg[0:16, :], func=AF.Copy))
                        for tb in range(4):
                            ps = P[tb]
                            fw.op("pe", [glr_aug, cb], [ps],
                                  lambda e, tb=tb, ps=ps: e.matmul(ps[:], glr_aug[0:17, tb * 128:(tb + 1) * 128],
                                                                   cb[0:17, C_WGATE + o_ * 512:C_WGATE + (o_ + 1) * 512], start=True, stop=True))
                            s_ = nxt("sa", sa)
                            st = nxt("stg", stg)
                            fw.op("act", [ps], [s_], lambda e, ps=ps, s_=s_: e.activation(out=s_[:], in_=ps[:], func=AF.Exp, scale=-1.0))
                            fw.op("act", [s_], [st], lambda e, s_=s_, st=st: e.activation(out=st[:], in_=s_[:], func=AF.Ln, bias=1.0))
                            for hf in range(2):
                                gsec = sF[hf * BG + 1024:hf * BG + 1280, :].rearrange("r (t16 c) -> (r t16) c", c=256)
                                fw.dma("pool", gsec[t0 + tb * 128:t0 + (tb + 1) * 128, :], st[:, hf * 256:(hf + 1) * 256], [st], [T_sendF[l]])
                last = (k == 4) or (stop_after is not None and k == stop_after)
                dstd = out_d if last else hT_d
                fw.dma("pool", dstd.ap()[:, t0:t0 + TT].rearrange("(kc p) t -> p kc t", p=128), h_t[:],
                       h, [T_out] if last else [T_h[tt]])
            assert ptr["use"] == len(full), (ptr, len(full))
            fw.barrier()

    def attention(l):
        with ExitStack() as es:
            P = [fw.ptile(es, f"PA{i}", [128, TT], F32) for i in range(8)]
            QT_t, QT = fw.tiles(es, "QT", 2, [128, SEQ], BF16)
            KT_t, KT = fw.tiles(es, "KT", 2, [128, SEQ], BF16)
            V = fw.tile(es, "V", [128, 64, 256], BF16)
            OT_t, OT = fw.tiles(es, "OT", 2, [128, SEQ], BF16)
            E = [fw.tile(es, f"E{i}", [128, TT], F32) for i in range(2)]
            L1 = [fw.tile(es, f"L1{i}", [128, TT], BF16) for i in range(3)]
            X = [fw.tile(es, f"X{i}", [128, TT], F32) for i in range(2)]
            W = [fw.tile(es, f"W{i}", [128, TT], BF16) for i in range(3)]
            Cc = fw.tile(es, "Cc", [128, TT], F32)
            for s in range(2):
                lf = locF[l][s].ap()
                for hp in range(2):
                    fw.dma("pool", QT[hp][:, s * TOK:(s + 1) * TOK], lf[hp * 128:(hp + 1) * 128, :], [T_locF[l][s]], [QT[hp]])
                    fw.dma("pool", KT[hp][:, s * TOK:(s + 1) * TOK], lf[256 + hp * 128:256 + (hp + 1) * 128, :], [T_locF[l][s]], [KT[hp]])
                fw.dma("pool", V[:, s * 32:(s + 1) * 32, :],
                       lf[512:768, :].rearrange("y (t16 c) -> (y t16) c", c=256).rearrange("(blk p) c -> p blk c", p=128),
                       [T_locF[l][s]], [V])
            cnt = {"E": 0, "L1": 0, "X": 0, "W": 0, "z": 0, "x": 0, "s": 0, "o": 0}
            if ATT_NH < 4 or ATT_NG < 16:
                for hp in range(2):
                    fw.op("pool", [], [OT[hp]], lambda e, hp=hp: e.memset(OT[hp][:], 0.0))

            def nxt(key, lst):
                i = cnt[key]; cnt[key] += 1
                return lst[i % len(lst)]

            pairs = []
            for hl in range(ATT_NH):
                for g in range(ATT_NG):
                    jtop = 4 * g + 3
                    for jb in range(jtop, -1, -1):
                        pairs.append({"hl": hl, "g": g, "jb": jb, "jtop": jtop})
            Zs, Xs, Ss, Os = [P[0], P[1]], [P[2], P[3]], [P[4], P[5]], [P[6], P[7]]
            ob_i = [-1]

            def stA(i):
                p = pairs[i]
                hp, hh = p["hl"] // 2, p["hl"] % 2
                pl, ph = hh * 64, hh * 64 + 64
                k0, q0 = p["jb"] * 128, p["g"] * TT
                Zb = Zs[i % 2]; p["Zb"] = Zb
                fw.op("pe", [KT[hp], QT[hp]], [Zb],
                      lambda e: e.matmul(Zb[:], KT[hp][pl:ph, k0:k0 + 128], QT[hp][pl:ph, q0:q0 + TT], start=True, stop=True))

            def stBC(i):
                p = pairs[i]
                hp, hh = p["hl"] // 2, p["hl"] % 2
                pl, ph = hh * 64, hh * 64 + 64
                k0, q0 = p["jb"] * 128, p["g"] * TT
                dgn = p["jb"] - 4 * p["g"]
                Zb = p["Zb"]; Xb = Xs[i % 2]; p["Xb"] = Xb
                e_ = E[i % 2]; l1 = L1[i % 3]
                fw.op("act", [Zb], [e_], lambda e: e.activation(out=e_[:], in_=Zb[:], func=AF.Exp))
                fw.op("act", [e_], [l1], lambda e: e.activation(out=l1[:], in_=e_[:], func=AF.Ln, bias=1.0))
                if dgn >= 0:
                    mk = C_AMASK + dgn * 512
                    fw.op("pool", [l1, cb], [l1], lambda e: e.tensor_tensor(l1[:], l1[:], cb[:, mk:mk + 512], op=ALU.mult))
                fw.op("pe", [KT[hp], QT[hp]], [Xb],
                      lambda e: e.matmul(Xb[:], KT[hp][pl:ph, k0:k0 + 128], QT[hp][pl:ph, q0:q0 + TT], start=True, stop=False), inc=False)
                fw.op("pe", [cb, l1], [Xb],
                      lambda e: e.matmul(Xb[:], cb[:, C_NEGU:C_NEGU + 128], l1[:], start=False, stop=True))
                if p["jb"] > 0:
                    Sb = Ss[i % 2]; p["Sb"] = Sb
                    fw.op("pe", [cb, l1], [Sb], lambda e: e.matmul(Sb[:], cb_ones(), l1[:], start=True, stop=True))

            def stDE(i):
                p = pairs[i]
                hl = p["hl"]
                hp, hh = hl // 2, hl % 2
                pl, ph = hh * 64, hh * 64 + 64
                jb, jtop, q0 = p["jb"], p["jtop"], p["g"] * TT
                dgn = jb - 4 * p["g"]
                Xb = p["Xb"]
                w_ = W[i % 3]
                if jb == jtop:
                    ob_i[0] += 1
                    fw.op("act", [Xb], [w_], lambda e: e.activation(out=w_[:], in_=Xb[:], func=AF.Exp))
                    if jb > 0:
                        fw.op("dve", [p["Sb"]], [Cc], lambda e: e.tensor_copy(Cc[:], p["Sb"][:]))
                else:
                    x_ = X[i % 2]
                    fw.op("dve", [Xb, Cc], [x_], lambda e: e.tensor_tensor(x_[:], Xb[:], Cc[:], op=ALU.subtract))
                    if jb > 0:
                        fw.op("dve", [p["Sb"], Cc], [Cc], lambda e: e.tensor_tensor(Cc[:], Cc[:], p["Sb"][:], op=ALU.add))
                    fw.op("act", [x_], [w_], lambda e: e.activation(out=w_[:], in_=x_[:], func=AF.Exp))
                if dgn >= 0:
                    mk = C_AMASK + dgn * 512
                    fw.op("pool", [w_, cb], [w_], lambda e: e.tensor_tensor(w_[:], w_[:], cb[:, mk:mk + 512], op=ALU.mult))
                Ob = Os[ob_i[0] % 2]
                fw.op("pe", [V, w_], [Ob],
                      lambda e: e.matmul(Ob[pl:ph, :], V[:, jb, hl * 64:(hl + 1) * 64], w_[:], start=(jb == jtop), stop=(jb == 0)),
                      inc=(jb == 0))
                if jb == 0:
                    fw.op("dve", [Ob], [OT[hp]], lambda e: e.tensor_copy(OT[hp][pl:ph, q0:q0 + TT], Ob[pl:ph, :]))

            npairs = len(pairs)
            stA(0)
            for i in range(-1, npairs):
                if 0 <= i + 1 < npairs:
                    stBC(i + 1)
                if 0 <= i + 2 < npairs:
                    stA(i + 2)
                if i >= 0:
                    stDE(i)
            for hf in range(2):
                fw.dma("pool", sendR[l].ap()[hf * 256:(hf + 1) * 256, :].rearrange("(hp p) t -> p hp t", p=128),
                       OT_t[:, :, hf * TOK:(hf + 1) * TOK], OT, [T_sendR[l]])
            fw.barrier()

    def gla(l):
        with ExitStack() as es:
            Pb = [fw.ptile(es, f"PGb{i}", [128, TT], F32) for i in range(2)]
            Pa = [fw.ptile(es, f"PGa{i}", [128, TT], F32) for i in range(2)]
            Po = [fw.ptile(es, f"PGo{i}", [128, TT], F32) for i in range(2)]
            Ps = fw.ptile(es, "PGs", [128, TT], F32)
            Pt = fw.ptile(es, "PGt", [128, 1024], BF16)
            NSEG = 8
            SEGT = SEQ // NSEG
            QT = [fw.tile(es, f"gQ{i}", [128, 2, SEGT], BF16) for i in range(2)]
            KT = [fw.tile(es, f"gK{i}", [128, 2, SEGT], BF16) for i in range(2)]
            Vs = [fw.tile(es, f"gV{i}", [128, 8, 512], BF16) for i in range(2)]
            Gs = [fw.tile(es, f"gG{i}", [128, 8, 256], BF16) for i in range(2)]
            Os = [fw.tile(es, f"gO{i}", [128, 4, SEGT], BF16) for i in range(2)]
            S32 = [fw.tile(es, f"S32_{i}", [128, 256], F32) for i in range(2)]
            Sbf = [fw.tile(es, f"Sbf_{i}", [128, 256], BF16) for i in range(2)]
            tS = [fw.tile(es, f"tS_{i}", [128, 256], F32) for i in range(2)]
            ebt = [fw.tile(es, f"eb{i}", [128, 128], F32) for i in range(4)]
            enb = [fw.tile(es, f"enb{i}", [128, 128], F32) for i in range(4)]
            qt = [fw.tile(es, f"qt{i}", [128, 128], BF16) for i in range(4)]
            kt = [fw.tile(es, f"kt{i}", [128, 128], BF16) for i in range(4)]
            ktok = [fw.tile(es, f"ktok{i}", [128, 128], BF16) for i in range(4)]
            Am = [fw.tile(es, f"Am{i}", [128, 128], BF16) for i in range(4)]
            for i in range(2):
                fw.op("pool", [], [S32[i]], lambda e, i=i: e.memset(S32[i][:], 0.0))
                fw.op("pool", [], [Sbf[i]], lambda e, i=i: e.memset(Sbf[i][:], 0.0))
            sR = sendR[l].ap()
            it = [0]
            for sg in range(min(NSEG, GLA_NSEG)):
                hft = sg // 4
                loc = (sg % 4) * SEGT
                q_, k_, v_, g_, o_t = QT[sg % 2], KT[sg % 2], Vs[sg % 2], Gs[sg % 2], Os[sg % 2]
                lf = locF[l][hft].ap()
                Tl = T_locF[l][hft]
                fw.dma("pool", q_[:], lf[0:256, loc:loc + SEGT].rearrange("(h p) t -> p h t", p=128), [Tl], [q_])
                fw.dma("pool", k_[:], lf[256:512, loc:loc + SEGT].rearrange("(h p) t -> p h t", p=128), [Tl], [k_])
                fw.dma("pool", v_[:],
                       lf[512:1024, :].rearrange("y (t8 c) -> (y t8) c", c=512)[loc:loc + SEGT, :].rearrange("(ch p) c -> p ch c", p=128),
                       [Tl], [v_])
                fw.dma("pool", g_[:],
                       lf[1024:1280, :].rearrange("y (t16 c) -> (y t16) c", c=256)[loc:loc + SEGT, :].rearrange("(ch p) c -> p ch c", p=128),
                       [Tl], [g_])
                for ch in range(8):
                    c0 = ch * 128
                    for hh in range(2):
                        i4 = it[0] % 4; it[0] += 1
                        pb = Pb[i4 % 2]; pa = Pa[i4 % 2]; po = Po[i4 % 2]
                        eb_, en_, qt_, kt_, ktk, am = ebt[i4], enb[i4], qt[i4], kt[i4], ktok[i4], Am[i4]
                        fw.op("pe", [g_, cb], [pb], lambda e: e.matmul(pb[:, 0:128], g_[:, ch, hh * 128:(hh + 1) * 128],
                                                                       cb[:, C_TRIN:C_TRIN + 128], start=True, stop=True))
                        fw.op("act", [pb], [eb_], lambda e: e.activation(out=eb_[:], in_=pb[:, 0:128], func=AF.Exp))
                        fw.op("act", [pb], [en_], lambda e: e.activation(out=en_[:], in_=pb[:, 0:128], func=AF.Exp, scale=-1.0))
                        fw.op("dve", [q_, eb_], [qt_], lambda e: e.scalar_tensor_tensor(out=qt_[:], in0=q_[:, hh, c0:c0 + 128], scalar=128.0 ** -0.5,
                                                                                        in1=eb_[:], op0=ALU.mult, op1=ALU.mult))
                        fw.op("pool", [k_, en_], [kt_], lambda e: e.tensor_tensor(kt_[:], k_[:, hh, c0:c0 + 128], en_[:], op=ALU.mult))
                        ptv = Pt[:, i4 * 128:(i4 + 1) * 128]
                        fw.op("pe", [kt_, cb], [Pt], lambda e: e.transpose(ptv, kt_[:], cb_id()))
                        fw.op("act", [Pt], [ktk], lambda e: e.activation(out=ktk[:], in_=ptv, func=AF.Copy))
                        fw.op("pe", [kt_, qt_], [pa], lambda e: e.matmul(pa[:, 0:128], kt_[:], qt_[:], start=True, stop=True))
                        fw.op("dve", [pa, cb], [am], lambda e: e.tensor_tensor(am[:], pa[:, 0:128], cb[:, C_GMASK:C_GMASK + 128], op=ALU.mult))
                        for vc in range(2):
                            fw.op("pe", [v_, am], [po],
                                  lambda e, vc=vc: e.matmul(po[:, vc * 128:(vc + 1) * 128], v_[:, ch, hh * 256 + vc * 128:hh * 256 + (vc + 1) * 128],
                                                            am[:], start=True, stop=False), inc=False)
                            fw.op("pe", [Sbf[hh], qt_], [po],
                                  lambda e, vc=vc: e.matmul(po[:, vc * 128:(vc + 1) * 128], Sbf[hh][:, vc * 128:(vc + 1) * 128],
                                                            qt_[:], start=False, stop=True), inc=(vc == 1))
                        fw.op("act", [po], [o_t], lambda e: e.activation(out=o_t[:, hh * 2:hh * 2 + 2, c0:c0 + 128],
                                                                       in_=po[:, 0:256].rearrange("p (v t) -> p v t", v=2), func=AF.Copy))
                        fw.op("pe", [ktk, v_], [Ps], lambda e: e.matmul(Ps[:, hh * 256:(hh + 1) * 256], ktk[:], v_[:, ch, hh * 256:(hh + 1) * 256],
                                                                        start=True, stop=True))
                        fw.op("dve", [S32[hh], eb_], [tS[hh]], lambda e: e.tensor_scalar(tS[hh][:], S32[hh][:], eb_[:, 127:128], None, op0=ALU.mult))
                        fw.op("dve", [Ps, eb_, tS[hh]], [S32[hh]],
                              lambda e: e.scalar_tensor_tensor(out=S32[hh][:], in0=Ps[:, hh * 256:(hh + 1) * 256], scalar=eb_[:, 127:128],
                                                               in1=tS[hh][:], op0=ALU.mult, op1=ALU.add))
                        fw.op("pool", [S32[hh]], [Sbf[hh]], lambda e: e.tensor_copy(Sbf[hh][:], S32[hh][:]))
                fw.dma("pool", sR[hft * 512:(hft + 1) * 512, loc:loc + SEGT].rearrange("(c p) t -> p c t", p=128), o_t[:], [o_t], [T_sendR[l]])
            fw.barrier()

    if test in ("loop1", "loop2"):
        stop_after = int(test[-1])
        token_loop(stop_after)
        return nc, fw
    if test == "attn":
        attention(0)
        return nc, fw
    if test == "gla":
        gla(1)
        return nc, fw
    if stop_after is not None and stop_after < 0:
        with ExitStack() as es:
            ht = fw.tile(es, "pt_h", [128, 8, TT], F32)
            w16 = fw.tile(es, "pt_w", [128, 2048], BF16)
            fw.dma("sp", w16[:], waA.ap()[175 * 128:176 * 128, :], [T_waA], [w16])
            for tt in range(NTT):
                fw.dma("pool", ht[:], xT_d.ap()[:, tt * TT:(tt + 1) * TT].rearrange("(kc p) t -> p kc t", p=128), [], [ht])
                if tt == 0:
                    fw.op("dve", [w16, ht], [ht], lambda e: e.tensor_copy(ht[:, 0, :], w16[:, 0:TT]))
                fw.dma("pool", out_d.ap()[:, tt * TT:(tt + 1) * TT].rearrange("(kc p) t -> p kc t", p=128), ht[:], [ht], [T_out])
        fw.barrier()
        return nc, fw
    nloops = 5 if stop_after is None else stop_after + 1
    for k in range(nloops):
        token_loop(k)
        if k < 4 and not (stop_after is not None and k == stop_after):
            l = k
            allgather(sendF[l], gathF[l], T_sendF[l], T_gathF[l])
            fetch_blocks(gathF[l], T_gathF[l], locF[l], T_locF[l], BE if l % 2 == 0 else BG)
            fw.barrier()
            if l % 2 == 0:
                attention(l)
            else:
                gla(l)
            allgather(sendR[l], gathR[l], T_sendR[l], T_gathR[l])
            fetch_blocks(gathR[l], T_gathR[l], locR[l], T_locR[l], 256 if l % 2 == 0 else 512)
            fw.barrier()
    fw.barrier()
    return nc, fw


def _consts(norm_g, gla_w_in, gla_w_gate, gla_b_gate, gla_norm_g, conv_dw_w, conv_dw_b, conv_ln_g, conv_ln_b, j):
    c = np.zeros((128, NCONST), np.float32)
    ii = np.arange(128)
    c[:, C_ID:C_ID + 128] = np.eye(128, dtype=np.float32)
    c[:, C_ONES:C_ONES + 128] = 1.0
    c[:, C_NEGU:C_NEGU + 128] = -(ii[:, None] >= ii[None, :]).astype(np.float32)
    c[:, C_TRIN:C_TRIN + 128] = (ii[:, None] <= ii[None, :]).astype(np.float32) * (-1.0 / 16.0)
    c[:, C_GMASK:C_GMASK + 128] = (ii[:, None] <= ii[None, :]).astype(np.float32)
    qq = np.arange(512)
    for d in range(4):
        c[:, C_AMASK + d * 512:C_AMASK + (d + 1) * 512] = ((d * 128 + ii[:, None]) < qq[None, :]).astype(np.float32)
    for o in range(2):
        c[:, C_WGLR + o * 128:C_WGLR + (o + 1) * 128] = \
            gla_w_in[o][:, 3072:3088].reshape(8, 128, 16).transpose(1, 0, 2).reshape(128, 128)
        c[0:16, C_WGATE + o * 512:C_WGATE + (o + 1) * 512] = gla_w_gate[o]
        c[16, C_WGATE + o * 512:C_WGATE + (o + 1) * 512] = gla_b_gate[o]
        c[:, C_GLANG + o * 2:C_GLANG + o * 2 + 2] = gla_norm_g[o].reshape(2, 128).T
    c[:, C_NORMG:C_NORMG + 192] = norm_g.reshape(4, 6, 8, 128).transpose(3, 0, 1, 2).reshape(128, 192)
    for e in range(2):
        c[:, C_CONVW + e * 124:C_CONVW + (e + 1) * 124] = conv_dw_w[e].reshape(31, 4, 128).transpose(2, 1, 0).reshape(128, 124)
        for wi, arr in enumerate((conv_dw_b, conv_ln_g, conv_ln_b)):
            c[:, C_CONVP + e * 12 + wi * 4:C_CONVP + e * 12 + wi * 4 + 4] = arr[e].reshape(4, 128).T
    c[:, C_FLAG] = float(j)
    return c


def _weight_units(ffn1_w_in, ffn1_w_out, ffn2_w_in, ffn2_w_out, hyb_w_in, hyb_w_out, gla_w_in, gla_w_out):
    A = np.empty((176, 128, 2048), np.float32)
    B = np.empty((64, 128, 2816), np.float32)
    for l in range(4):
        for which, (wi, wo) in enumerate(((ffn1_w_in, ffn1_w_out), (ffn2_w_in, ffn2_w_out))):
            u = (l * 2 + which)
            A[u * 22:(u + 1) * 22] = wi[l].reshape(8, 128, 2, 22, 128).transpose(3, 1, 0, 2, 4).reshape(22, 128, 2048)
            B[u * 8:(u + 1) * 8] = wo[l].reshape(22, 128, 8, 128).transpose(2, 1, 0, 3).reshape(8, 128, 2816)
    C = np.empty((120, 128, 1024), np.float32)

    def units(w, nf):
        return w.reshape(8, 128, nf, 128).transpose(2, 1, 0, 3).reshape(nf, 128, 1024)
    for e in range(2):
        b0 = e * 28
        C[b0:b0 + 20] = units(hyb_w_in[e], 20)
        C[b0 + 20:b0 + 28] = units(hyb_w_out[e], 8)
    for o in range(2):
        b0 = 56 + o * 32
        C[b0:b0 + 24] = units(np.ascontiguousarray(gla_w_in[o][:, :3072]), 24)
        C[b0 + 24:b0 + 32] = units(gla_w_out[o], 8)
    return A, B, C


_CACHE = {}


def _run(inputs, stop_after=None):
    x = np.asarray(inputs["x"], np.float32)
    g = {k: np.asarray(v, np.float32) for k, v in inputs.items() if k != "x"}
    A, B, C = _weight_units(g["ffn1_w_in"], g["ffn1_w_out"], g["ffn2_w_in"], g["ffn2_w_out"],
                            g["hyb_w_in"], g["hyb_w_out"], g["gla_w_in"], g["gla_w_out"])
    in_maps = []
    for c in range(NCORES):
        b, j = c // 2, c % 2
        in_maps.append({
            "xT": np.ascontiguousarray(x[b, j * TOK:(j + 1) * TOK, :].T),
            "wA": np.ascontiguousarray(A[c * 22:(c + 1) * 22].reshape(22 * 128, 2048)),
            "wB": np.ascontiguousarray(B[c * 8:(c + 1) * 8].reshape(8 * 128, 2816)),
            "wC": np.ascontiguousarray(C[c * 15:(c + 1) * 15].reshape(15 * 128, 1024)),
            "consts": _consts(g["norm_g"], g["gla_w_in"], g["gla_w_gate"], g["gla_b_gate"], g["gla_norm_g"],
                              g["conv_dw_w"], g["conv_dw_b"], g["conv_ln_g"], g["conv_ln_b"], j),
        })
    key = stop_after
    if key not in _CACHE:
        _CACHE[key] = build_program(stop_after)[0]
    nc = _CACHE[key]
    res = run_bass_kernel_spmd(nc, in_maps, core_ids=list(range(NCORES)))
    out = np.empty((4, SEQ, D), np.float32)
    for c in range(NCORES):
        b, j = c // 2, c % 2
        out[b, j * TOK:(j + 1) * TOK, :] = res.results[c]["outT"].T
    return out


def kernel(**inputs):
    return _run(inputs)
```

```python
import os
import math
import numpy as np
from contextlib import ExitStack
import concourse.bass as bass
import concourse.mybir as mybir
from concourse.bass_utils import run_bass_kernel_spmd

F32 = mybir.dt.float32
BF16 = mybir.dt.bfloat16
AF = mybir.ActivationFunctionType
ALU = mybir.AluOpType

D = 1024
DFF = 2816
NFC = 22
TOK = 4096
TT = 512
NTT = TOK // TT
SEQ = 8192
EPS = 1e-6
NCORES = 8
ATT_NH, ATT_NG, GLA_NSEG = 4, 16, 8

C_ID, C_ONES, C_NEGU, C_TRIN, C_GMASK = 0, 128, 256, 384, 512
C_AMASK = 640
C_WGLR = C_AMASK + 2048
C_WGATE = C_WGLR + 256
NBF = C_WGATE + 1024
C_NORMG = NBF
C_GLANG = C_NORMG + 192
C_CONVW = C_GLANG + 4
C_CONVP = C_CONVW + 248
C_FLAG = C_CONVP + 24
NCONST = C_FLAG + 1
NF32 = NCONST - NBF

RE = 1544
BE = 772
RG = 2560
BG = 1280


class T:
    __slots__ = ("base", "w", "r", "name", "multi")

    def __init__(self, base, name, multi=False):
        self.base = base
        self.w = None
        self.r = {}
        self.name = name
        self.multi = multi

    def __getitem__(self, idx):
        return self.base[idx]


class Eng:
    def __init__(self, name, h, sem):
        self.name = name
        self.h = h
        self.sem = sem
        self.count = 0
        self.known = {}
        self.pend_r = []
        self.pend_w = []


class FW:
    def __init__(self, nc):
        self.nc = nc
        self.es = ExitStack()
        self.eng = {}
        for name, h in (("pe", nc.tensor), ("act", nc.scalar), ("dve", nc.vector),
                        ("pool", nc.gpsimd), ("sp", nc.sync)):
            sem = self.es.enter_context(nc.semaphore("s_" + name))
            self.eng[name] = Eng(name, h, sem)
        self.dq = {}
        for q in ("sp", "pool"):
            sems = [self.es.enter_context(nc.semaphore(f"dq_{q}_{i}")) for i in range(8)]
            self.dq[q] = {"sems": sems, "n": 0}
        self.nwaits = 0
        self.nins = 0
        self._uid = 0
        self.pending = set()

    def sb(self, es, name, shape, dtype=F32):
        self._uid += 1
        t = es.enter_context(self.nc.sbuf_tensor(f"{name}_{self._uid}", list(shape), dtype))
        return t

    def ps(self, es, name, shape, dtype=F32):
        self._uid += 1
        t = es.enter_context(self.nc.psum_tensor(f"{name}_{self._uid}", list(shape), dtype))
        return t

    def tile(self, es, name, shape, dtype=F32):
        return T(self.sb(es, name, shape, dtype), name)

    def tiles(self, es, name, n, shape, dtype=F32):
        t = self.sb(es, name, [shape[0], n] + list(shape[1:]), dtype)
        return t, [T(t[:, i], f"{name}{i}") for i in range(n)]

    def ptile(self, es, name, shape, dtype=F32):
        return T(self.ps(es, name, shape, dtype), name)

    def sem(self, name):
        return self.es.enter_context(self.nc.semaphore(name))

    def _wait(self, E, ev):
        sem, val = ev
        k = id(sem)
        if E.known.get(k, 0) >= val:
            return
        E.h.wait_ge(sem, val)
        E.known[k] = val
        self.nwaits += 1

    def _deps(self, E, reads, writes, skip_self=False):
        for b in list(reads) + list(writes):
            assert id(b) not in self.pending or b in E.pend_r or b in E.pend_w, \
                f"access to {b.name} with pending (un-incremented) accesses"
        for b in reads:
            if b.multi:
                for ev in b.r.values():
                    self._wait(E, ev)
                continue
            if b.w is not None and not (skip_self and b.w[0] is E.sem):
                self._wait(E, b.w)
        for b in writes:
            if b.multi:
                continue
            if b.w is not None and not (skip_self and b.w[0] is E.sem):
                self._wait(E, b.w)
            for ev in b.r.values():
                if not (skip_self and ev[0] is E.sem):
                    self._wait(E, ev)

    def _mark(self, ev, reads, writes):
        k = id(ev[0])
        for b in reads:
            if b.multi:
                continue
            b.r[k] = ev
        for b in writes:
            if b.multi:
                b.r[k] = ev
            else:
                b.w = ev
                b.r = {}

    def op(self, eng, reads, writes, fn, inc=True):
        E = self.eng[eng]
        self._deps(E, reads, writes, skip_self=(eng == "pe"))
        ins = fn(E.h)
        self.nins += 1
        if inc:
            E.count += 1
            ins.then_inc(E.sem, 1)
            ev = (E.sem, E.count)
            self._mark(ev, E.pend_r + list(reads), E.pend_w + list(writes))
            for b in E.pend_r + E.pend_w:
                self.pending.discard(id(b))
            E.pend_r = []
            E.pend_w = []
        else:
            E.pend_r += list(reads)
            E.pend_w += list(writes)
            for b in list(reads) + list(writes):
                self.pending.add(id(b))
        return ins

    def dma(self, q, out_ap, in_ap, reads, writes, **kw):
        E = self.eng[q]
        Dq = self.dq[q]
        i = Dq["n"]
        Dq["n"] += 1
        sem = Dq["sems"][i % 8]
        prev = 16 * (i // 8)
        if prev > 0:
            self._wait(E, (sem, prev))
        self._deps(E, reads, writes)
        ins = E.h.dma_start(out=out_ap, in_=in_ap, **kw)
        ins.then_inc(sem, 16)
        self.nins += 1
        ev = (sem, prev + 16)
        self._mark(ev, reads, writes)
        return ev

    def wait_all_dma(self, q):
        E = self.eng[q]
        Dq = self.dq[q]
        n = Dq["n"]
        for s in range(8):
            cnt = (n - s + 7) // 8
            if cnt > 0:
                self._wait(E, (Dq["sems"][s], 16 * cnt))

    def collective(self, sem, in_ap, out_ap, reads, writes):
        E = self.eng["pool"]
        self._deps(E, reads, writes)
        ins = E.h.collective_compute("AllGather", ALU.bypass,
                                     replica_groups=[list(range(NCORES))],
                                     ins=[in_ap], outs=[out_ap])
        ins.then_inc(sem)
        self.nins += 1
        self._mark((sem, 1), reads, writes)

    def barrier(self):
        for q in ("sp", "pool"):
            self.wait_all_dma(q)
        evs = []
        for name, E in self.eng.items():
            assert not E.pend_r and not E.pend_w
            if E.count > 0:
                self._wait(E, (E.sem, E.count))
            E.count += 1
            E.h.sem_inc(E.sem, 1)
            evs.append((E.sem, E.count))
        for name, E in self.eng.items():
            for ev in evs:
                self._wait(E, ev)


def build_program(stop_after=None, sim=False, test=None):
    nc = bass.Bass("TRN2", target_bir_lowering=False)
    fw = FW(nc)
    ges = fw.es

    if test in ("attn", "gla"):
        xT_d = nc.dram_tensor("xT", [D, TOK], F32)
        wA_d = nc.dram_tensor("wA", [22 * 128, 2048], F32)
        wB_d = nc.dram_tensor("wB", [8 * 128, 2816], F32)
        wC_d = nc.dram_tensor("wC", [15 * 128, 1024], F32)
    else:
        xT_d = nc.dram_tensor("xT", [D, TOK], F32, kind="ExternalInput")
        wA_d = nc.dram_tensor("wA", [22 * 128, 2048], F32, kind="ExternalInput")
        wB_d = nc.dram_tensor("wB", [8 * 128, 2816], F32, kind="ExternalInput")
        wC_d = nc.dram_tensor("wC", [15 * 128, 1024], F32, kind="ExternalInput")
    cst_d = nc.dram_tensor("consts", [128, NCONST], F32, kind="ExternalInput")
    out_d = nc.dram_tensor("outT", [D, TOK], F32, kind="ExternalOutput") if test not in ("attn", "gla") else nc.dram_tensor("outT", [D, TOK], F32)

    ext = {}
    if test == "attn":
        ext = {"locF0_0": "ExternalInput", "locF0_1": "ExternalInput", "sendR0": "ExternalOutput"}
    if test == "gla":
        ext = {"locF1_0": "ExternalInput", "locF1_1": "ExternalInput", "sendR1": "ExternalOutput"}
    if test == "loop1":
        ext = {"hT": "ExternalInput", "locR0_0": "ExternalInput", "locR0_1": "ExternalInput", "cdr0": "ExternalInput",
               "locF0_0": "ExternalInput", "sendF1": "ExternalOutput", "rdr1": "ExternalOutput"}
    if test == "loop2":
        ext = {"hT": "ExternalInput", "locR1_0": "ExternalInput", "locR1_1": "ExternalInput", "rdr1": "ExternalInput",
               "sendF0": "ExternalOutput", "cdr0": "ExternalOutput"}

    def idram(name, shape, dt):
        if name in ext:
            return nc.dram_tensor(name, list(shape), dt, kind=ext[name])
        return nc.dram_tensor(name, list(shape), dt)

    wsA = idram("wsA", [22 * 128, 2048], BF16); wsB = idram("wsB", [8 * 128, 2816], BF16); wsC = idram("wsC", [15 * 128, 1024], BF16)
    if sim and test not in ("attn", "gla"):
        waA = nc.dram_tensor("waA", [176 * 128, 2048], BF16, kind="ExternalInput")
        waB = nc.dram_tensor("waB", [64 * 128, 2816], BF16, kind="ExternalInput")
        waC = nc.dram_tensor("waC", [120 * 128, 1024], BF16, kind="ExternalInput")
    else:
        waA = idram("waA", [176 * 128, 2048], BF16)
        waB = idram("waB", [64 * 128, 2816], BF16)
        waC = idram("waC", [120 * 128, 1024], BF16)
    hT_d = idram("hT", [D, TOK], F32)
    sendF = {}; gathF = {}; sendR = {}; gathR = {}; cdr = {}; rdr = {}
    for l in range(2):
        if l % 2 == 0:
            sendF[l] = idram(f"sendF{l}", [RE, TOK], BF16); gathF[l] = idram(f"gathF{l}", [8 * RE, TOK], BF16)
            sendR[l] = idram(f"sendR{l}", [512, TOK], BF16); gathR[l] = idram(f"gathR{l}", [8 * 512, TOK], BF16)
            cdr[l] = idram(f"cdr{l}", [512, TOK], BF16)
        else:
            sendF[l] = idram(f"sendF{l}", [RG, TOK], BF16); gathF[l] = idram(f"gathF{l}", [8 * RG, TOK], BF16)
            sendR[l] = idram(f"sendR{l}", [1024, TOK], BF16); gathR[l] = idram(f"gathR{l}", [8 * 1024, TOK], BF16)
            rdr[l] = idram(f"rdr{l}", [D, TOK], F32)
    for l in (2, 3):
        sendF[l] = sendF[l - 2]; gathF[l] = gathF[l - 2]; sendR[l] = sendR[l - 2]; gathR[l] = gathR[l - 2]
        if l == 2: cdr[l] = cdr[0]
        else: rdr[l] = rdr[1]
    T_wsA = T(wsA, "wsA", multi=True); T_waA = T(waA, "waA")
    T_wsB = T(wsB, "wsB", multi=True); T_waB = T(waB, "waB")
    T_wsC = T(wsC, "wsC", multi=True); T_waC = T(waC, "waC")
    T_h = [T(hT_d, f"hT{i}") for i in range(NTT)]
    T_sendF = {l: T(sendF[l], f"sendF{l}", multi=True) for l in range(2)}
    T_gathF = {l: T(gathF[l], f"gathF{l}") for l in range(2)}
    T_sendR = {l: T(sendR[l], f"sendR{l}", multi=True) for l in range(2)}
    T_gathR = {l: T(gathR[l], f"gathR{l}") for l in range(2)}
    T_cdr = {0: [T(cdr[0], f"cdr_{i}") for i in range(NTT)]}
    T_rdr = {1: [T(rdr[1], f"rdr_{i}") for i in range(NTT)]}
    for l in (2, 3):
        T_sendF[l] = T_sendF[l - 2]; T_gathF[l] = T_gathF[l - 2]; T_sendR[l] = T_sendR[l - 2]; T_gathR[l] = T_gathR[l - 2]
    T_cdr[2] = T_cdr[0]; T_rdr[3] = T_rdr[1]
    T_out = T(out_d, "out", multi=True)
    cc_sems = [fw.sem(f"cc{i}") for i in range(11)]
    cc_i = [0]

    def allgather(send, gath, Ts, Tg):
        fw.collective(cc_sems[cc_i[0]], send.ap().opt(), gath.ap().opt(), [Ts], [Tg])
        cc_i[0] += 1

    pid = nc.partition_id([mybir.EngineType.Pool])
    B0 = nc.gpsimd.snap(pid + (pid // 2) * 2)
    locF = {}; locR = {}; T_locF = {}; T_locR = {}
    for l in range(2):
        rf, rr = (BE, 256) if l % 2 == 0 else (BG, 512)
        locF[l] = [idram(f"locF{l}_{s_}", [rf, TOK], BF16) for s_ in range(2)]
        locR[l] = [idram(f"locR{l}_{s_}", [rr, TOK], BF16) for s_ in range(2)]
        T_locF[l] = [T(locF[l][s_], f"locF{l}_{s_}") for s_ in range(2)]
        T_locR[l] = [T(locR[l][s_], f"locR{l}_{s_}") for s_ in range(2)]
    for l in (2, 3):
        locF[l] = locF[l - 2]; locR[l] = locR[l - 2]; T_locF[l] = T_locF[l - 2]; T_locR[l] = T_locR[l - 2]

    def fetch_blocks(gath, Tg, loc, Tloc, rows):
        v3 = gath.ap().rearrange("(b r) t -> b r t", r=rows)
        for s_ in range(2):
            fw.dma("pool", loc[s_].ap(), v3[2 * s_:, :, :][bass.ds(B0, 1), :, :].rearrange("o r t -> (o r) t"),
                   [Tg], [Tloc[s_]])

    cb_t = fw.sb(ges, "cb", [128, NBF], BF16); cb = T(cb_t, "cb")
    cf_t = fw.sb(ges, "cf", [128, NF32], F32); cf = T(cf_t, "cf")
    glr_aug = fw.tile(ges, "glr_aug", [32, TT], BF16)

    def cfc(col, n=1):
        return cf[:, col - NBF: col - NBF + n]

    with ExitStack() as es:
        ctmp = fw.tile(es, "ctmp", [128, NCONST], F32)
        fw.dma("sp", ctmp[:], cst_d.ap(), [], [ctmp])
        fw.op("dve", [ctmp], [cb], lambda e: e.tensor_copy(cb[:, 0:2048], ctmp[:, 0:2048]))
        fw.op("pool", [ctmp], [cb], lambda e: e.tensor_copy(cb[:, 2048:NBF], ctmp[:, 2048:NBF]))
        fw.op("act", [ctmp], [cf], lambda e: e.activation(out=cf[:], in_=ctmp[:, NBF:NCONST], func=AF.Copy))
        fw.op("pool", [], [glr_aug], lambda e: e.memset(glr_aug[:], 1.0))
        wi = 0
        for (src, dst, Tdst, n, L) in (() if sim else ((wA_d, wsA, T_wsA, 22, 2048), (wB_d, wsB, T_wsB, 8, 2816),
                                       (wC_d, wsC, T_wsC, 15, 1024))):
            st32 = [fw.tile(es, f"wst32_{L}_{i}", [128, L], F32) for i in range(2)]
            st16 = [fw.tile(es, f"wst16_{L}_{i}", [128, L], BF16) for i in range(2)]
            for u in range(n):
                a, b_ = st32[u % 2], st16[u % 2]
                fw.dma("sp", a[:], src.ap()[u * 128:(u + 1) * 128, :], [], [a])
                eng = ("dve", "pool", "act")[wi % 3]; wi += 1
                if eng == "act":
                    fw.op("act", [a], [b_], lambda e, a=a, b_=b_: e.activation(out=b_[:], in_=a[:], func=AF.Copy))
                else:
                    fw.op(eng, [a], [b_], lambda e, a=a, b_=b_: e.tensor_copy(b_[:], a[:]))
                fw.dma("pool", dst.ap()[u * 128:(u + 1) * 128, :], b_[:], [b_], [Tdst])
        if not sim:
            allgather(wsA, waA, T_wsA, T_waA)
            allgather(wsB, waB, T_wsB, T_waB)
            allgather(wsC, waC, T_wsC, T_waC)
        fw.barrier()

    cb_id = lambda: cb[:, C_ID:C_ID + 128]
    cb_ones = lambda: cb[:, C_ONES:C_ONES + 128]

    def uA(l, which, fc): return (l * 2 + which) * 22 + fc
    def uB(l, which, dc): return (l * 2 + which) * 8 + dc
    UC_BASE = {0: 0, 2: 28, 1: 56, 3: 88}

    def token_loop(k):
        lb = k - 1 if k > 0 else None
        la = k if k < 4 else None
        with ExitStack() as es:
            P = [fw.ptile(es, f"P{i}", [128, TT], F32) for i in range(8)]
            h_t, h = fw.tiles(es, "h", 8, [128, TT], F32)
            xn_t, xn = fw.tiles(es, "xn", 8, [128, TT], BF16)
            sq_t, sq = fw.tiles(es, "sq", 8, [128, TT], BF16)
            f_t, f = fw.tiles(es, "f", 8, [128, TT], F32)
            hm_t, hm = fw.tiles(es, "hm", NFC, [128, TT], BF16)
            cat_t, cat = fw.tiles(es, "cat", 8, [128, TT], BF16)
            lnv = fw.tile(es, "lnv", [128, TT], F32)
            rstd = fw.tile(es, "rstd", [128, TT], F32)
            sa = [fw.tile(es, f"sa{i}", [128, TT], F32) for i in range(2)]
            tmp = [fw.tile(es, f"tmp{i}", [128, TT], F32) for i in range(3)]
            stg = [fw.tile(es, f"stg{i}", [128, TT], BF16) for i in range(3)]
            stg32 = [fw.tile(es, f"stg32_{i}", [128, TT], F32) for i in range(2)]
            NSA, NSB, NSC = 3, 2, 6
            slotA = [fw.tile(es, f"slA{i}", [128, 2048], BF16) for i in range(NSA)]
            slotB = [fw.tile(es, f"slB{i}", [128, 2816], BF16) for i in range(NSB)]
            slotC = [fw.tile(es, f"slC{i}", [128, 1024], BF16) for i in range(NSC)]
            cnt = {"A": 0, "B": 0, "C": 0, "tmp": 0, "stg": 0, "sa": 0, "stg32": 0}
            even_b = lb is not None and lb % 2 == 0
            odd_b = lb is not None and lb % 2 == 1
            if even_b:
                eb_ = lb // 2
                cbuf_t, cbuf = fw.tiles(es, "cbuf", 4, [128, 32 + TT], BF16)
                dg = fw.tile(es, "dg", [128, 124 * 128], BF16)
                ysb_t, ysb = fw.tiles(es, "ysb", 4, [128, TT], F32)
                ybf_t, ybf = fw.tiles(es, "ybf", 4, [128, TT], BF16)
                mean = fw.tile(es, "mean", [128, TT], F32)
                for i in range(124):
                    col = C_CONVW + eb_ * 124 + i
                    fw.op("pool" if i % 2 else "dve", [cb, cf], [dg],
                          lambda e, i=i, col=col: e.tensor_scalar(dg[:, i * 128:(i + 1) * 128], cb_id(),
                                                                   cfc(col), None, op0=ALU.mult))
            if odd_b:
                ob_t, ob = fw.tiles(es, "ob", 8, [128, TT], BF16)

            def nxt(key, lst):
                i = cnt[key]; cnt[key] += 1
                return lst[i % len(lst)]

            def wload(kind, unit):
                if kind == "A":
                    s = nxt("A", slotA); src = waA.ap()[unit * 128:(unit + 1) * 128, :]; Tsrc = T_waA
                elif kind == "B":
                    s = nxt("B", slotB); src = waB.ap()[unit * 128:(unit + 1) * 128, :]; Tsrc = T_waB
                else:
                    s = nxt("C", slotC); src = waC.ap()[unit * 128:(unit + 1) * 128, :]; Tsrc = T_waC
                fw.dma("sp", s[:], src, [Tsrc], [s])
                return s

            plan = []
            def plan_ffn(l, which):
                for fc in range(NFC): plan.append(("A", uA(l, which, fc)))
                for dc in range(8): plan.append(("B", uB(l, which, dc)))
            if even_b:
                for dc in range(8): plan.append(("C", UC_BASE[lb] + 20 + dc))
            if odd_b:
                for dc in range(8): plan.append(("C", UC_BASE[lb] + 24 + dc))
            if lb is not None: plan_ffn(lb, 1)
            if la is not None:
                plan_ffn(la, 0)
                if la % 2 == 0:
                    order = list(range(12)) + [12, 16, 13, 17, 14, 18, 15, 19]
                else:
                    order = list(range(24))
                for u in order: plan.append(("C", UC_BASE[la] + u))
            full = plan * NTT
            loaded = []
            ptr = {"issue": 0, "use": 0}
            NS = {"A": NSA, "B": NSB, "C": NSC}

            def wget(kind):
                i = ptr["use"]
                assert full[i][0] == kind, (full[i], kind, i)
                lo = max(i - 1, 0)
                while ptr["issue"] < len(full) and ptr["issue"] - i <= 12:
                    kd, un = full[ptr["issue"]]
                    infl = sum(1 for jj in range(lo, ptr["issue"]) if full[jj][0] == kd)
                    if infl >= NS[kd]:
                        assert ptr["issue"] > i
                        break
                    loaded.append(wload(kd, un))
                    ptr["issue"] += 1
                ptr["use"] += 1
                return loaded[i]

            def gcol(l, i):
                return C_NORMG + (l * 6 + i) * 8

            def rstd_from(psT, n, extra_bias=0.0):
                fw.op("act", [psT], [lnv], lambda e: e.activation(out=lnv[:], in_=psT[:], func=AF.Ln,
                                                                   scale=1.0 / n, bias=EPS))
                fw.op("act", [lnv], [rstd], lambda e: e.activation(out=rstd[:], in_=lnv[:], func=AF.Exp,
                                                                    scale=-0.5, bias=extra_bias))

            def sumsq_mm(ps, srcs):
                n = len(srcs)
                for i, s in enumerate(srcs):
                    fw.op("pe", [cb, s], [ps], lambda e, s=s, i=i: e.matmul(ps[:], cb_ones(), s[:], start=(i == 0),
                                                                           stop=(i == n - 1)), inc=(i == n - 1))

            def prenorm(l, gi):
                for kc in range(8):
                    fw.op("act", [h[kc]], [sq[kc]], lambda e, kc=kc: e.activation(out=sq[kc][:], in_=h[kc][:], func=AF.Square))
                sumsq_mm(P[6], sq)
                rstd_from(P[6], D)
                gc = gcol(l, gi)
                for kc in range(8):
                    fw.op("dve", [h[kc], cf, rstd], [xn[kc]],
                          lambda e, kc=kc: e.scalar_tensor_tensor(out=xn[kc][:], in0=h[kc][:], scalar=cfc(gc + kc),
                                                                  in1=rstd[:], op0=ALU.mult, op1=ALU.mult))

            def post_residual(l, gi, res_scale):
                sumsq_mm(P[6], sq)
                rstd_from(P[6], D, extra_bias=math.log(res_scale))
                gc = gcol(l, gi)
                for dc in range(8):
                    tp = nxt("tmp", tmp)
                    fw.op("dve", [f[dc], cf, rstd], [tp],
                          lambda e, dc=dc, tp=tp: e.scalar_tensor_tensor(out=tp[:], in0=f[dc][:], scalar=cfc(gc + dc),
                                                                         in1=rstd[:], op0=ALU.mult, op1=ALU.mult))
                    fw.op("pool", [tp, h[dc]], [h[dc]],
                          lambda e, dc=dc, tp=tp: e.tensor_tensor(h[dc][:], tp[:], h[dc][:], op=ALU.add))

            def evac_f(ps, dc):
                fw.op("dve", [ps], [f[dc]], lambda e: e.tensor_copy(f[dc][:], ps[:]))
                fw.op("act", [f[dc]], [sq[dc]], lambda e: e.activation(out=sq[dc][:], in_=f[dc][:], func=AF.Square))

            def ffn(l, which):
                prenorm(l, 0 if which == 0 else 4)
                for fc in range(NFC):
                    wu = wget("A")
                    pa, pb = (P[0], P[1]) if fc % 2 == 0 else (P[2], P[3])
                    for half, ps in ((0, pa), (1, pb)):
                        for kc in range(8):
                            off = kc * 256 + half * 128
                            fw.op("pe", [wu, xn[kc]], [ps],
                                  lambda e, ps=ps, wu=wu, kc=kc, off=off: e.matmul(ps[:], wu[:, off:off + 128], xn[kc][:],
                                                                                  start=(kc == 0), stop=(kc == 7)),
                                  inc=(kc == 7))
                    s_ = nxt("sa", sa)
                    fw.op("act", [pa], [s_], lambda e, s_=s_, pa=pa: e.activation(out=s_[:], in_=pa[:], func=AF.Silu))
                    fw.op("dve", [s_, pb], [hm[fc]], lambda e, s_=s_, pb=pb, fc=fc: e.tensor_tensor(hm[fc][:], s_[:], pb[:], op=ALU.mult))
                for dc in range(8):
                    wo = wget("B")
                    ps = P[4 + dc % 2]
                    for fc in range(NFC):
                        fw.op("pe", [wo, hm[fc]], [ps],
                              lambda e, ps=ps, wo=wo, fc=fc: e.matmul(ps[:], wo[:, fc * 128:(fc + 1) * 128], hm[fc][:],
                                                                      start=(fc == 0), stop=(fc == NFC - 1)),
                              inc=(fc == NFC - 1))
                    evac_f(ps, dc)
                post_residual(l, 1 if which == 0 else 5, 0.5)

            def proj_fm(wu, ps, src=None):
                src = src or xn
                for kc in range(8):
                    fw.op("pe", [wu, src[kc]], [ps],
                          lambda e, kc=kc: e.matmul(ps[:], wu[:, kc * 128:(kc + 1) * 128], src[kc][:],
                                                    start=(kc == 0), stop=(kc == 7)), inc=(kc == 7))

            def proj_tm(nun, pss):
                for i in range(nun):
                    wu = wget("C")
                    for tb in range(4):
                        for kc in range(8):
                            fw.op("pe", [wu, xn[kc]], [pss[tb]],
                                  lambda e, i=i, wu=wu, tb=tb, kc=kc: e.matmul(
                                      pss[tb][:, i * 128:(i + 1) * 128], xn[kc][:, tb * 128:(tb + 1) * 128],
                                      wu[:, kc * 128:(kc + 1) * 128], start=(kc == 0), stop=(kc == 7)),
                                  inc=(kc == 7 and tb == 3))

            def out_proj_and_residual(l):
                for dc in range(8):
                    wu = wget("C")
                    ps = P[4 + dc % 2]
                    proj_fm(wu, ps, src=cat)
                    evac_f(ps, dc)
                post_residual(l, 3, 1.0)

            for tt in range(NTT):
                t0 = tt * TT
                src = hT_d if k > 0 else xT_d
                fw.dma("pool", h_t[:], src.ap()[:, t0:t0 + TT].rearrange("(kc p) t -> p kc t", p=128),
                       [T_h[tt]] if k > 0 else [], h)
                if even_b:
                    l = lb
                    for kc in range(4):
                        fw.dma("pool", cat[kc][:], locR[l][kc // 2].ap()[(kc % 2) * 128:(kc % 2 + 1) * 128, t0:t0 + TT],
                               [T_locR[l][kc // 2]], [cat[kc]])
                    fw.dma("pool", cbuf_t[:, :, 32:32 + TT],
                           cdr[l].ap()[:, t0:t0 + TT].rearrange("(cc p) t -> p cc t", p=128), [T_cdr[l][tt]], cbuf)
                    if tt > 0:
                        fw.dma("pool", cbuf_t[:, :, 0:32],
                               cdr[l].ap()[:, t0 - 32:t0].rearrange("(cc p) t -> p cc t", p=128), [T_cdr[l][tt - 1]], cbuf)
                    else:
                        fw.dma("pool", cbuf_t[:, :, 0:32],
                               locF[l][0].ap()[768:772, :].rearrange("cc (p i) -> p cc i", i=32),
                               [T_locF[l][0]], cbuf)
                        fw.op("pool", cbuf + [cf], cbuf,
                              lambda e: e.tensor_scalar(cbuf_t[:, :, 0:32], cbuf_t[:, :, 0:32], cfc(C_FLAG), None, op0=ALU.mult))
                    pc = C_CONVP + eb_ * 12
                    for cc in range(4):
                        ps = P[cc]
                        for kk in range(31):
                            fw.op("pe", [dg, cbuf[cc]], [ps],
                                  lambda e, ps=ps, cc=cc, kk=kk: e.matmul(ps[:], dg[:, (cc * 31 + kk) * 128:(cc * 31 + kk + 1) * 128],
                                                                          cbuf[cc][:, 2 + kk:2 + kk + TT], start=(kk == 0), stop=(kk == 30)),
                                  inc=(kk == 30))
                        fw.op("act", [ps, cf], [ysb[cc]], lambda e, ps=ps, cc=cc: e.activation(out=ysb[cc][:], in_=ps[:], func=AF.Identity,
                                                                                             bias=cfc(pc + cc)))
                        fw.op("act", [ysb[cc]], [sq[cc]], lambda e, cc=cc: e.activation(out=sq[cc][:], in_=ysb[cc][:], func=AF.Square))
                        fw.op("pool", [ysb[cc]], [ybf[cc]], lambda e, cc=cc: e.tensor_copy(ybf[cc][:], ysb[cc][:]))
                    sumsq_mm(P[4], ybf)
                    sumsq_mm(P[5], sq[0:4])
                    m2 = nxt("tmp", tmp)
                    var = nxt("tmp", tmp)
                    fw.op("dve", [P[4]], [mean], lambda e: e.tensor_scalar(mean[:], P[4][:], 1.0 / 512, None, op0=ALU.mult))
                    fw.op("dve", [mean], [m2], lambda e: e.tensor_tensor(m2[:], mean[:], mean[:], op=ALU.mult))
                    fw.op("dve", [P[5], m2], [var], lambda e: e.scalar_tensor_tensor(out=var[:], in0=P[5][:], scalar=1.0 / 512, in1=m2[:],
                                                                                   op0=ALU.mult, op1=ALU.subtract))
                    rstd_from(var, 1.0)
                    for cc in range(4):
                        t1 = nxt("tmp", tmp)
                        fw.op("dve", [ysb[cc], mean], [t1], lambda e, cc=cc, t1=t1: e.tensor_tensor(t1[:], ysb[cc][:], mean[:], op=ALU.subtract))
                        fw.op("pool", [t1, rstd], [t1], lambda e, t1=t1: e.tensor_tensor(t1[:], t1[:], rstd[:], op=ALU.mult))
                        fw.op("act", [t1, cf], [cat[4 + cc]],
                              lambda e, cc=cc, t1=t1: e.activation(out=cat[4 + cc][:], in_=t1[:], func=AF.Silu,
                                                                   scale=cfc(pc + 4 + cc), bias=cfc(pc + 8 + cc)))
                    out_proj_and_residual(l)
                if odd_b:
                    l = lb
                    o_ = l // 2
                    for oc in range(8):
                        fw.dma("pool", ob[oc][:], locR[l][oc // 4].ap()[(oc % 4) * 128:(oc % 4 + 1) * 128, t0:t0 + TT],
                               [T_locR[l][oc // 4]], [ob[oc]])
                    for hd in range(4):
                        for cc in range(2):
                            fw.op("act", [ob[2 * hd + cc]], [sq[cc]],
                                  lambda e, hd=hd, cc=cc: e.activation(out=sq[cc][:], in_=ob[2 * hd + cc][:], func=AF.Square))
                        sumsq_mm(P[hd % 2], sq[0:2])
                        rstd_from(P[hd % 2], 256)
                        for cc in range(2):
                            oc = 2 * hd + cc
                            t1 = nxt("tmp", tmp)
                            s32 = nxt("stg32", stg32)
                            fw.dma("pool", s32[:], rdr[l].ap()[oc * 128:(oc + 1) * 128, t0:t0 + TT], [T_rdr[l][tt]], [s32])
                            fw.op("dve", [ob[oc], cf, rstd], [t1],
                                  lambda e, oc=oc, cc=cc, t1=t1: e.scalar_tensor_tensor(out=t1[:], in0=ob[oc][:],
                                                                                        scalar=cfc(C_GLANG + o_ * 2 + cc),
                                                                                        in1=rstd[:], op0=ALU.mult, op1=ALU.mult))
                            fw.op("pool", [t1, s32], [cat[oc]], lambda e, oc=oc, t1=t1, s32=s32: e.tensor_tensor(cat[oc][:], t1[:], s32[:], op=ALU.mult))
                    out_proj_and_residual(l)
                if lb is not None:
                    ffn(lb, 1)
                if la is not None:
                    l = la
                    ffn(l, 0)
                    prenorm(l, 2)
                    sF = sendF[l].ap()
                    if l % 2 == 0:
                        for fc in range(8):
                            wu = wget("C")
                            ps = P[fc % 4]
                            proj_fm(wu, ps)
                            st = nxt("stg", stg)
                            sc_ = 0.125 if fc < 4 else 1.0
                            fw.op("act", [ps], [st], lambda e, ps=ps, st=st, sc_=sc_: e.activation(out=st[:], in_=ps[:], func=AF.Copy, scale=sc_))
                            f4 = fc % 4
                            row0 = (f4 // 2) * BE + (256 if fc >= 4 else 0) + (f4 % 2) * 128
                            fw.dma("pool", sF[row0:row0 + 128, t0:t0 + TT], st[:], [st], [T_sendF[l]])
                        proj_tm(4, P[0:4])
                        for tb in range(4):
                            st = nxt("stg", stg)
                            fw.op("dve" if tb % 2 else "act", [P[tb]], [st],
                                  (lambda e, tb=tb, st=st: e.tensor_copy(st[:], P[tb][:])) if tb % 2 else
                                  (lambda e, tb=tb, st=st: e.activation(out=st[:], in_=P[tb][:], func=AF.Copy)))
                            for hf in range(2):
                                vsec = sF[hf * BE + 512:hf * BE + 768, :].rearrange("r (t16 c) -> (r t16) c", c=256)
                                fw.dma("pool", vsec[t0 + tb * 128:t0 + (tb + 1) * 128, :], st[:, hf * 256:(hf + 1) * 256], [st], [T_sendF[l]])
                        for fc in range(4):
                            pu, pg = (P[4], P[5]) if fc % 2 == 0 else (P[6], P[7])
                            wuu = wget("C")
                            wug = wget("C")
                            proj_fm(wuu, pu)
                            proj_fm(wug, pg)
                            s_ = nxt("sa", sa)
                            st = nxt("stg", stg)
                            fw.op("act", [pg], [s_], lambda e, pg=pg, s_=s_: e.activation(out=s_[:], in_=pg[:], func=AF.Sigmoid))
                            fw.op("dve", [s_, pu], [st], lambda e, pu=pu, s_=s_, st=st: e.tensor_tensor(st[:], s_[:], pu[:], op=ALU.mult))
                            fw.dma("pool", cdr[l].ap()[fc * 128:(fc + 1) * 128, t0:t0 + TT], st[:], [st], [T_cdr[l][tt]])
                            if tt == NTT - 1:
                                for hf in range(2):
                                    fw.dma("pool", sF[hf * BE + 768 + fc:hf * BE + 769 + fc, :].rearrange("o (p i) -> (o p) i", i=32),
                                           st[:, TT - 32:TT], [st], [T_sendF[l]])
                    else:
                        o_ = l // 2
                        for fc in range(8):
                            wu = wget("C")
                            ps = P[fc % 4]
                            proj_fm(wu, ps)
                            st = nxt("stg", stg)
                            fw.op("act", [ps], [st], lambda e, ps=ps, st=st: e.activation(out=st[:], in_=ps[:], func=AF.Copy))
                            f4 = fc % 4
                            row0 = (f4 // 2) * BG + (256 if fc >= 4 else 0) + (f4 % 2) * 128
                            fw.dma("pool", sF[row0:row0 + 128, t0:t0 + TT], st[:], [st], [T_sendF[l]])
                        for hf in range(2):
                            vsec = sF[hf * BG + 512:hf * BG + 1024, :].rearrange("r (t8 c) -> (r t8) c", c=512)
                            pss = P[0:4] if hf == 0 else P[4:8]
                            proj_tm(4, pss)
                            for tb in range(4):
                                st = nxt("stg", stg)
                                fw.op("dve" if tb % 2 else "act", [pss[tb]], [st],
                                      (lambda e, tb=tb, st=st, pss=pss: e.tensor_copy(st[:], pss[tb][:])) if tb % 2 else
                                      (lambda e, tb=tb, st=st, pss=pss: e.activation(out=st[:], in_=pss[tb][:], func=AF.Copy)))
                                fw.dma("pool", vsec[t0 + tb * 128:t0 + (tb + 1) * 128, :], st[:], [st], [T_sendF[l]])
                        for fc in range(8):
                            wu = wget("C")
                            ps = P[fc % 4]
                            proj_fm(wu, ps)
                            s32 = nxt("stg32", stg32)
                            fw.op("act", [ps], [s32], lambda e, ps=ps, s32=s32: e.activation(out=s32[:], in_=ps[:], func=AF.Silu))
                            fw.dma("pool", rdr[l].ap()[fc * 128:(fc + 1) * 128, t0:t0 + TT], s32[:], [s32], [T_rdr[l][tt]])
                        psg = P[4]
                        for kc in range(8):
                            c0 = C_WGLR + o_ * 128 + kc * 16
                            fw.op("pe", [cb, xn[kc]], [psg],
                                  lambda e, kc=kc, c0=c0: e.matmul(psg[0:16, :], cb[:, c0:c0 + 16], xn[kc][:], start=(kc == 0), stop=(kc == 7)),
                                  inc=(kc == 7))
                        fw.op("act", [psg], [glr_aug], lambda e: e.activation(out=glr_aug[0:16, :], in_=psg[0:16, :], func=AF.Copy))
                        for tb in range(4):
                            ps = P[tb]
                            fw.op("pe", [glr_aug, cb], [ps],
                                  lambda e, tb=tb, ps=ps: e.matmul(ps[:], glr_aug[0:17, tb * 128:(tb + 1) * 128],
                                                                   cb[0:17, C_WGATE + o_ * 512:C_WGATE + (o_ + 1) * 512], start=True, stop=True))
                            s_ = nxt("sa", sa)
                            st = nxt("stg", stg)
                            fw.op("act", [ps], [s_], lambda e, ps=ps, s_=s_: e.activation(out=s_[:], in_=ps[:], func=AF.Exp, scale=-1.0))
                            fw.op("act", [s_], [st], lambda e, s_=s_, st=st: e.activation(out=st[:], in_=s_[:], func=AF.Ln, bias=1.0))
                            for hf in range(2):
                                gsec = sF[hf * BG + 1024:hf * BG + 1280, :].rearrange("r (t16 c) -> (r t16) c", c=256)
                                fw.dma("pool", gsec[t0 + tb * 128:t0 + (tb + 1) * 128, :], st[:, hf * 256:(hf + 1) * 256], [st], [T_sendF[l]])
                last = (k == 4) or (stop_after is not None and k == stop_after)
                dstd = out_d if last else hT_d
                fw.dma("pool", dstd.ap()[:, t0:t0 + TT].rearrange("(kc p) t -> p kc t", p=128), h_t[:],
                       h, [T_out] if last else [T_h[tt]])
            assert ptr["use"] == len(full), (ptr, len(full))
            fw.barrier()

    def attention(l):
        with ExitStack() as es:
            P = [fw.ptile(es, f"PA{i}", [128, TT], F32) for i in range(8)]
            QT_t, QT = fw.tiles(es, "QT", 2, [128, SEQ], BF16)
            KT_t, KT = fw.tiles(es, "KT", 2, [128, SEQ], BF16)
            V = fw.tile(es, "V", [128, 64, 256], BF16)
            OT_t, OT = fw.tiles(es, "OT", 2, [128, SEQ], BF16)
            E = [fw.tile(es, f"E{i}", [128, TT], F32) for i in range(2)]
            L1 = [fw.tile(es, f"L1{i}", [128, TT], BF16) for i in range(3)]
            X = [fw.tile(es, f"X{i}", [128, TT], F32) for i in range(2)]
            W = [fw.tile(es, f"W{i}", [128, TT], BF16) for i in range(3)]
            Cc = fw.tile(es, "Cc", [128, TT], F32)
            for s in range(2):
                lf = locF[l][s].ap()
                for hp in range(2):
                    fw.dma("pool", QT[hp][:, s * TOK:(s + 1) * TOK], lf[hp * 128:(hp + 1) * 128, :], [T_locF[l][s]], [QT[hp]])
                    fw.dma("pool", KT[hp][:, s * TOK:(s + 1) * TOK], lf[256 + hp * 128:256 + (hp + 1) * 128, :], [T_locF[l][s]], [KT[hp]])
                fw.dma("pool", V[:, s * 32:(s + 1) * 32, :],
                       lf[512:768, :].rearrange("y (t16 c) -> (y t16) c", c=256).rearrange("(blk p) c -> p blk c", p=128),
                       [T_locF[l][s]], [V])
            cnt = {"E": 0, "L1": 0, "X": 0, "W": 0, "z": 0, "x": 0, "s": 0, "o": 0}
            if ATT_NH < 4 or ATT_NG < 16:
                for hp in range(2):
                    fw.op("pool", [], [OT[hp]], lambda e, hp=hp: e.memset(OT[hp][:], 0.0))

            def nxt(key, lst):
                i = cnt[key]; cnt[key] += 1
                return lst[i % len(lst)]

            pairs = []
            for hl in range(ATT_NH):
                for g in range(ATT_NG):
                    jtop = 4 * g + 3
                    for jb in range(jtop, -1, -1):
                        pairs.append({"hl": hl, "g": g, "jb": jb, "jtop": jtop})
            Xs, Ss, Os = [P[0], P[1], P[2]], [P[3], P[4]], [P[5], P[6]]
            ob_i = [-1]

            def stA(i):
                p = pairs[i]
                hp, hh = p["hl"] // 2, p["hl"] % 2
                pl, ph = hh * 64, hh * 64 + 64
                k0, q0 = p["jb"] * 128, p["g"] * TT
                Xb = Xs[i % 3]; p["Xb"] = Xb
                fw.op("pe", [KT[hp], QT[hp]], [Xb],
                      lambda e: e.matmul(Xb[:], KT[hp][pl:ph, k0:k0 + 128], QT[hp][pl:ph, q0:q0 + TT], start=True, stop=True))

            def stBC(i):
                p = pairs[i]
                hp, hh = p["hl"] // 2, p["hl"] % 2
                pl, ph = hh * 64, hh * 64 + 64
                k0, q0 = p["jb"] * 128, p["g"] * TT
                dgn = p["jb"] - 4 * p["g"]
                Xb = p["Xb"]
                e_ = E[i % 2]; l1 = L1[i % 3]
                fw.op("act", [Xb], [e_], lambda e: e.activation(out=e_[:], in_=Xb[:], func=AF.Exp))
                fw.op("act", [e_], [l1], lambda e: e.activation(out=l1[:], in_=e_[:], func=AF.Ln, bias=1.0))
                if dgn >= 0:
                    mk = C_AMASK + dgn * 512
                    fw.op("pool", [l1, cb], [l1], lambda e: e.tensor_tensor(l1[:], l1[:], cb[:, mk:mk + 512], op=ALU.mult))
                fw.op("pe", [cb, l1], [Xb],
                      lambda e: e.matmul(Xb[:], cb[:, C_NEGU:C_NEGU + 128], l1[:], start=False, stop=True, skip_group_check=True))
                if p["jb"] > 0:
                    Sb = Ss[i % 2]; p["Sb"] = Sb
                    fw.op("pe", [cb, l1], [Sb], lambda e: e.matmul(Sb[:], cb_ones(), l1[:], start=True, stop=True))

            def stDE(i):
                p = pairs[i]
                hl = p["hl"]
                hp, hh = hl // 2, hl % 2
                pl, ph = hh * 64, hh * 64 + 64
                jb, jtop, q0 = p["jb"], p["jtop"], p["g"] * TT
                dgn = jb - 4 * p["g"]
                Xb = p["Xb"]
                w_ = W[i % 3]
                if jb == jtop:
                    ob_i[0] += 1
                    fw.op("act", [Xb], [w_], lambda e: e.activation(out=w_[:], in_=Xb[:], func=AF.Exp))
                    if jb > 0:
                        fw.op("dve", [p["Sb"]], [Cc], lambda e: e.tensor_copy(Cc[:], p["Sb"][:]))
                else:
                    x_ = X[i % 2]
                    fw.op("dve", [Xb, Cc], [x_], lambda e: e.tensor_tensor(x_[:], Xb[:], Cc[:], op=ALU.subtract))
                    if jb > 0:
                        fw.op("dve", [p["Sb"], Cc], [Cc], lambda e: e.tensor_tensor(Cc[:], Cc[:], p["Sb"][:], op=ALU.add))
                    fw.op("act", [x_], [w_], lambda e: e.activation(out=w_[:], in_=x_[:], func=AF.Exp))
                if dgn >= 0:
                    mk = C_AMASK + dgn * 512
                    fw.op("pool", [w_, cb], [w_], lambda e: e.tensor_tensor(w_[:], w_[:], cb[:, mk:mk + 512], op=ALU.mult))
                Ob = Os[ob_i[0] % 2]
                fw.op("pe", [V, w_], [Ob],
                      lambda e: e.matmul(Ob[pl:ph, :], V[:, jb, hl * 64:(hl + 1) * 64], w_[:], start=(jb == jtop), stop=(jb == 0)),
                      inc=(jb == 0))
                if jb == 0:
                    fw.op("dve", [Ob], [OT[hp]], lambda e: e.tensor_copy(OT[hp][pl:ph, q0:q0 + TT], Ob[pl:ph, :]))

            npairs = len(pairs)
            stA(0)
            for i in range(-1, npairs):
                if 0 <= i + 1 < npairs:
                    stBC(i + 1)
                if 0 <= i + 2 < npairs:
                    stA(i + 2)
                if i >= 0:
                    stDE(i)
            for hf in range(2):
                fw.dma("pool", sendR[l].ap()[hf * 256:(hf + 1) * 256, :].rearrange("(hp p) t -> p hp t", p=128),
                       OT_t[:, :, hf * TOK:(hf + 1) * TOK], OT, [T_sendR[l]])
            fw.barrier()

    def gla(l):
        with ExitStack() as es:
            Pb = [fw.ptile(es, f"PGb{i}", [128, TT], F32) for i in range(2)]
            Pa = [fw.ptile(es, f"PGa{i}", [128, TT], F32) for i in range(2)]
            Po = [fw.ptile(es, f"PGo{i}", [128, TT], F32) for i in range(2)]
            Ps = fw.ptile(es, "PGs", [128, TT], F32)
            Pt = fw.ptile(es, "PGt", [128, 1024], BF16)
            NSEG = 8
            SEGT = SEQ // NSEG
            QT = [fw.tile(es, f"gQ{i}", [128, 2, SEGT], BF16) for i in range(2)]
            KT = [fw.tile(es, f"gK{i}", [128, 2, SEGT], BF16) for i in range(2)]
            Vs = [fw.tile(es, f"gV{i}", [128, 8, 512], BF16) for i in range(2)]
            Gs = [fw.tile(es, f"gG{i}", [128, 8, 256], BF16) for i in range(2)]
            Os = [fw.tile(es, f"gO{i}", [128, 4, SEGT], BF16) for i in range(2)]
            S32 = [fw.tile(es, f"S32_{i}", [128, 256], F32) for i in range(2)]
            Sbf = [fw.tile(es, f"Sbf_{i}", [128, 256], BF16) for i in range(2)]
            tS = [fw.tile(es, f"tS_{i}", [128, 256], F32) for i in range(2)]
            ebt = [fw.tile(es, f"eb{i}", [128, 128], F32) for i in range(4)]
            enb = [fw.tile(es, f"enb{i}", [128, 128], F32) for i in range(4)]
            qt = [fw.tile(es, f"qt{i}", [128, 128], BF16) for i in range(4)]
            kt = [fw.tile(es, f"kt{i}", [128, 128], BF16) for i in range(4)]
            ktok = [fw.tile(es, f"ktok{i}", [128, 128], BF16) for i in range(4)]
            Am = [fw.tile(es, f"Am{i}", [128, 128], BF16) for i in range(4)]
            for i in range(2):
                fw.op("pool", [], [S32[i]], lambda e, i=i: e.memset(S32[i][:], 0.0))
                fw.op("pool", [], [Sbf[i]], lambda e, i=i: e.memset(Sbf[i][:], 0.0))
            sR = sendR[l].ap()
            it = [0]
            for sg in range(min(NSEG, GLA_NSEG)):
                hft = sg // 4
                loc = (sg % 4) * SEGT
                q_, k_, v_, g_, o_t = QT[sg % 2], KT[sg % 2], Vs[sg % 2], Gs[sg % 2], Os[sg % 2]
                lf = locF[l][hft].ap()
                Tl = T_locF[l][hft]
                fw.dma("pool", q_[:], lf[0:256, loc:loc + SEGT].rearrange("(h p) t -> p h t", p=128), [Tl], [q_])
                fw.dma("pool", k_[:], lf[256:512, loc:loc + SEGT].rearrange("(h p) t -> p h t", p=128), [Tl], [k_])
                fw.dma("pool", v_[:],
                       lf[512:1024, :].rearrange("y (t8 c) -> (y t8) c", c=512)[loc:loc + SEGT, :].rearrange("(ch p) c -> p ch c", p=128),
                       [Tl], [v_])
                fw.dma("pool", g_[:],
                       lf[1024:1280, :].rearrange("y (t16 c) -> (y t16) c", c=256)[loc:loc + SEGT, :].rearrange("(ch p) c -> p ch c", p=128),
                       [Tl], [g_])
                for ch in range(8):
                    c0 = ch * 128
                    for hh in range(2):
                        i4 = it[0] % 4; it[0] += 1
                        pb = Pb[i4 % 2]; pa = Pa[i4 % 2]; po = Po[i4 % 2]
                        eb_, en_, qt_, kt_, ktk, am = ebt[i4], enb[i4], qt[i4], kt[i4], ktok[i4], Am[i4]
                        fw.op("pe", [g_, cb], [pb], lambda e: e.matmul(pb[:, 0:128], g_[:, ch, hh * 128:(hh + 1) * 128],
                                                                       cb[:, C_TRIN:C_TRIN + 128], start=True, stop=True))
                        fw.op("act", [pb], [eb_], lambda e: e.activation(out=eb_[:], in_=pb[:, 0:128], func=AF.Exp))
                        fw.op("act", [pb], [en_], lambda e: e.activation(out=en_[:], in_=pb[:, 0:128], func=AF.Exp, scale=-1.0))
                        fw.op("dve", [q_, eb_], [qt_], lambda e: e.scalar_tensor_tensor(out=qt_[:], in0=q_[:, hh, c0:c0 + 128], scalar=128.0 ** -0.5,
                                                                                        in1=eb_[:], op0=ALU.mult, op1=ALU.mult))
                        fw.op("pool", [k_, en_], [kt_], lambda e: e.tensor_tensor(kt_[:], k_[:, hh, c0:c0 + 128], en_[:], op=ALU.mult))
                        ptv = Pt[:, i4 * 128:(i4 + 1) * 128]
                        fw.op("pe", [kt_, cb], [Pt], lambda e: e.transpose(ptv, kt_[:], cb_id()))
                        fw.op("act", [Pt], [ktk], lambda e: e.activation(out=ktk[:], in_=ptv, func=AF.Copy))
                        fw.op("pe", [kt_, qt_], [pa], lambda e: e.matmul(pa[:, 0:128], kt_[:], qt_[:], start=True, stop=True))
                        fw.op("dve", [pa, cb], [am], lambda e: e.tensor_tensor(am[:], pa[:, 0:128], cb[:, C_GMASK:C_GMASK + 128], op=ALU.mult))
                        for vc in range(2):
                            fw.op("pe", [v_, am], [po],
                                  lambda e, vc=vc: e.matmul(po[:, vc * 128:(vc + 1) * 128], v_[:, ch, hh * 256 + vc * 128:hh * 256 + (vc + 1) * 128],
                                                            am[:], start=True, stop=False), inc=False)
                            fw.op("pe", [Sbf[hh], qt_], [po],
                                  lambda e, vc=vc: e.matmul(po[:, vc * 128:(vc + 1) * 128], Sbf[hh][:, vc * 128:(vc + 1) * 128],
                                                            qt_[:], start=False, stop=True), inc=(vc == 1))
                        fw.op("act", [po], [o_t], lambda e: e.activation(out=o_t[:, hh * 2:hh * 2 + 2, c0:c0 + 128],
                                                                       in_=po[:, 0:256].rearrange("p (v t) -> p v t", v=2), func=AF.Copy))
                        fw.op("pe", [ktk, v_], [Ps], lambda e: e.matmul(Ps[:, hh * 256:(hh + 1) * 256], ktk[:], v_[:, ch, hh * 256:(hh + 1) * 256],
                                                                        start=True, stop=True))
                        fw.op("dve", [S32[hh], eb_], [tS[hh]], lambda e: e.tensor_scalar(tS[hh][:], S32[hh][:], eb_[:, 127:128], None, op0=ALU.mult))
                        fw.op("dve", [Ps, eb_, tS[hh]], [S32[hh]],
                              lambda e: e.scalar_tensor_tensor(out=S32[hh][:], in0=Ps[:, hh * 256:(hh + 1) * 256], scalar=eb_[:, 127:128],
                                                               in1=tS[hh][:], op0=ALU.mult, op1=ALU.add))
                        fw.op("pool", [S32[hh]], [Sbf[hh]], lambda e: e.tensor_copy(Sbf[hh][:], S32[hh][:]))
                fw.dma("pool", sR[hft * 512:(hft + 1) * 512, loc:loc + SEGT].rearrange("(c p) t -> p c t", p=128), o_t[:], [o_t], [T_sendR[l]])
            fw.barrier()

    if test in ("loop1", "loop2"):
        stop_after = int(test[-1])
        token_loop(stop_after)
        return nc, fw
    if test == "attn":
        attention(0)
        return nc, fw
    if test == "gla":
        gla(1)
        return nc, fw
    if stop_after is not None and stop_after < 0:
        with ExitStack() as es:
            ht = fw.tile(es, "pt_h", [128, 8, TT], F32)
            w16 = fw.tile(es, "pt_w", [128, 2048], BF16)
            fw.dma("sp", w16[:], waA.ap()[175 * 128:176 * 128, :], [T_waA], [w16])
            for tt in range(NTT):
                fw.dma("pool", ht[:], xT_d.ap()[:, tt * TT:(tt + 1) * TT].rearrange("(kc p) t -> p kc t", p=128), [], [ht])
                if tt == 0:
                    fw.op("dve", [w16, ht], [ht], lambda e: e.tensor_copy(ht[:, 0, :], w16[:, 0:TT]))
                fw.dma("pool", out_d.ap()[:, tt * TT:(tt + 1) * TT].rearrange("(kc p) t -> p kc t", p=128), ht[:], [ht], [T_out])
        fw.barrier()
        return nc, fw
    nloops = 5 if stop_after is None else stop_after + 1
    for k in range(nloops):
        token_loop(k)
        if k < 4 and not (stop_after is not None and k == stop_after):
            l = k
            allgather(sendF[l], gathF[l], T_sendF[l], T_gathF[l])
            fetch_blocks(gathF[l], T_gathF[l], locF[l], T_locF[l], BE if l % 2 == 0 else BG)
            fw.barrier()
            if l % 2 == 0:
                attention(l)
            else:
                gla(l)
            allgather(sendR[l], gathR[l], T_sendR[l], T_gathR[l])
            fetch_blocks(gathR[l], T_gathR[l], locR[l], T_locR[l], 256 if l % 2 == 0 else 512)
            fw.barrier()
    fw.barrier()
    return nc, fw


def _consts(norm_g, gla_w_in, gla_w_gate, gla_b_gate, gla_norm_g, conv_dw_w, conv_dw_b, conv_ln_g, conv_ln_b, j):
    c = np.zeros((128, NCONST), np.float32)
    ii = np.arange(128)
    c[:, C_ID:C_ID + 128] = np.eye(128, dtype=np.float32)
    c[:, C_ONES:C_ONES + 128] = 1.0
    c[:, C_NEGU:C_NEGU + 128] = -(ii[:, None] >= ii[None, :]).astype(np.float32)
    c[:, C_TRIN:C_TRIN + 128] = (ii[:, None] <= ii[None, :]).astype(np.float32) * (-1.0 / 16.0)
    c[:, C_GMASK:C_GMASK + 128] = (ii[:, None] <= ii[None, :]).astype(np.float32)
    qq = np.arange(512)
    for d in range(4):
        c[:, C_AMASK + d * 512:C_AMASK + (d + 1) * 512] = ((d * 128 + ii[:, None]) < qq[None, :]).astype(np.float32)
    for o in range(2):
        c[:, C_WGLR + o * 128:C_WGLR + (o + 1) * 128] = \
            gla_w_in[o][:, 3072:3088].reshape(8, 128, 16).transpose(1, 0, 2).reshape(128, 128)
        c[0:16, C_WGATE + o * 512:C_WGATE + (o + 1) * 512] = gla_w_gate[o]
        c[16, C_WGATE + o * 512:C_WGATE + (o + 1) * 512] = gla_b_gate[o]
        c[:, C_GLANG + o * 2:C_GLANG + o * 2 + 2] = gla_norm_g[o].reshape(2, 128).T
    c[:, C_NORMG:C_NORMG + 192] = norm_g.reshape(4, 6, 8, 128).transpose(3, 0, 1, 2).reshape(128, 192)
    for e in range(2):
        c[:, C_CONVW + e * 124:C_CONVW + (e + 1) * 124] = conv_dw_w[e].reshape(31, 4, 128).transpose(2, 1, 0).reshape(128, 124)
        for wi, arr in enumerate((conv_dw_b, conv_ln_g, conv_ln_b)):
            c[:, C_CONVP + e * 12 + wi * 4:C_CONVP + e * 12 + wi * 4 + 4] = arr[e].reshape(4, 128).T
    c[:, C_FLAG] = float(j)
    return c


def _weight_units(ffn1_w_in, ffn1_w_out, ffn2_w_in, ffn2_w_out, hyb_w_in, hyb_w_out, gla_w_in, gla_w_out):
    A = np.empty((176, 128, 2048), np.float32)
    B = np.empty((64, 128, 2816), np.float32)
    for l in range(4):
        for which, (wi, wo) in enumerate(((ffn1_w_in, ffn1_w_out), (ffn2_w_in, ffn2_w_out))):
            u = (l * 2 + which)
            A[u * 22:(u + 1) * 22] = wi[l].reshape(8, 128, 2, 22, 128).transpose(3, 1, 0, 2, 4).reshape(22, 128, 2048)
            B[u * 8:(u + 1) * 8] = wo[l].reshape(22, 128, 8, 128).transpose(2, 1, 0, 3).reshape(8, 128, 2816)
    C = np.empty((120, 128, 1024), np.float32)

    def units(w, nf):
        return w.reshape(8, 128, nf, 128).transpose(2, 1, 0, 3).reshape(nf, 128, 1024)
    for e in range(2):
        b0 = e * 28
        C[b0:b0 + 20] = units(hyb_w_in[e], 20)
        C[b0 + 20:b0 + 28] = units(hyb_w_out[e], 8)
    for o in range(2):
        b0 = 56 + o * 32
        C[b0:b0 + 24] = units(np.ascontiguousarray(gla_w_in[o][:, :3072]), 24)
        C[b0 + 24:b0 + 32] = units(gla_w_out[o], 8)
    return A, B, C


_CACHE = {}


def _run(inputs, stop_after=None):
    x = np.asarray(inputs["x"], np.float32)
    g = {k: np.asarray(v, np.float32) for k, v in inputs.items() if k != "x"}
    A, B, C = _weight_units(g["ffn1_w_in"], g["ffn1_w_out"], g["ffn2_w_in"], g["ffn2_w_out"],
                            g["hyb_w_in"], g["hyb_w_out"], g["gla_w_in"], g["gla_w_out"])
    in_maps = []
    for c in range(NCORES):
        b, j = c // 2, c % 2
        in_maps.append({
            "xT": np.ascontiguousarray(x[b, j * TOK:(j + 1) * TOK, :].T),
            "wA": np.ascontiguousarray(A[c * 22:(c + 1) * 22].reshape(22 * 128, 2048)),
            "wB": np.ascontiguousarray(B[c * 8:(c + 1) * 8].reshape(8 * 128, 2816)),
            "wC": np.ascontiguousarray(C[c * 15:(c + 1) * 15].reshape(15 * 128, 1024)),
            "consts": _consts(g["norm_g"], g["gla_w_in"], g["gla_w_gate"], g["gla_b_gate"], g["gla_norm_g"],
                              g["conv_dw_w"], g["conv_dw_b"], g["conv_ln_g"], g["conv_ln_b"], j),
        })
    key = stop_after
    if key not in _CACHE:
        _CACHE[key] = build_program(stop_after)[0]
    nc = _CACHE[key]
    res = run_bass_kernel_spmd(nc, in_maps, core_ids=list(range(NCORES)))
    out = np.empty((4, SEQ, D), np.float32)
    for c in range(NCORES):
        b, j = c // 2, c % 2
        out[b, j * TOK:(j + 1) * TOK, :] = res.results[c]["outT"].T
    return out


def kernel(**inputs):
    return _run(inputs)
```
